# Optimizing a Trainium2 kernel written in Bass

```python
import math
import jax, jax.numpy as jnp
from jax import lax
import numpy as np

D_MODEL = 1024
BATCH = 8
SEQ = 2048
DEPTH = 4

M_HEADS = 4
M_HEAD_DIM = 64
M_CHUNK = 64
M_WIDTH = M_HEADS * M_HEAD_DIM
SB_HEADS = 4
SB_HEAD_DIM = 64
SB_BLOCK = 128
SB_WIDTH = SB_HEADS * SB_HEAD_DIM
G_HEADS = 4
G_HEAD_DIM = 128
G_CHUNK = 64
G_CONV = 4
G_WIDTH = G_HEADS * G_HEAD_DIM
D_MIX = M_WIDTH + SB_WIDTH + G_WIDTH
IN_SPLITS = (M_WIDTH, M_WIDTH, M_WIDTH, M_WIDTH, M_HEADS, M_HEADS,
             SB_WIDTH, SB_WIDTH, SB_WIDTH,
             G_WIDTH, G_WIDTH, G_WIDTH, G_WIDTH, G_HEADS, G_HEADS)
D_IN = sum(IN_SPLITS)
N_EXPERTS = 32
TOP_K = 4
D_FF = D_MODEL
SWIGLU_LIMIT = 7.0
SWIGLU_ALPHA = 1.702
MOE_BLOCK = 256
PLE_DIM = 256
DEEPNORM_ALPHA = (2 * DEPTH) ** 0.25
DEEPNORM_BETA = (8 * DEPTH) ** -0.25
LN_EPS = 1e-5
NORM_EPS = 1e-6

kernel_name = 'hybrid_mlstm_stickbreak_gdn_moe_deepnorm'

F32 = jnp.float32


def layer_norm(x, g, b):
    xf = x.astype(F32)
    mu = jnp.mean(xf, -1, keepdims=True)
    var = jnp.mean(jnp.square(xf - mu), -1, keepdims=True)
    return ((xf - mu) * lax.rsqrt(var + LN_EPS) * g + b).astype(x.dtype)


def rms_normalize(x):
    return x * lax.rsqrt(jnp.mean(x * x, -1, keepdims=True) + NORM_EPS)


def l2_normalize(x):
    return x * lax.rsqrt(jnp.sum(x * x, -1, keepdims=True) + NORM_EPS)


def split_heads(t, heads):
    B, S, W = t.shape
    return t.reshape(B, S, heads, W // heads).astype(F32)


def to_chunks(t, size):
    B, S, H = t.shape[:3]
    t = t.reshape((B, S // size, size, H) + t.shape[3:])
    return jnp.moveaxis(t, (1, 3), (0, 2))


def from_chunks(t):
    t = jnp.moveaxis(t, (0, 2), (1, 3))
    B, N, L, H, d = t.shape
    return t.reshape(B, N * L, H, d)


def causal_depthwise_conv(x, w):
    C = x.shape[-1]
    return lax.conv_general_dilated(x, w[:, None, :], window_strides=(1,),
                                    padding=[(w.shape[0] - 1, 0)],
                                    dimension_numbers=('NWC', 'WIO', 'NWC'),
                                    feature_group_count=C)


def mlstm_mixer(q, k, v, o_pre, i_pre, f_pre, i_bias, f_bias, norm_w):
    q = to_chunks(split_heads(q, M_HEADS), M_CHUNK)
    k = to_chunks(split_heads(k, M_HEADS), M_CHUNK) * (M_HEAD_DIM ** -0.5)
    v = to_chunks(split_heads(v, M_HEADS), M_CHUNK)
    log_i = to_chunks(i_pre.astype(F32) + i_bias.astype(F32), M_CHUNK)
    log_f = to_chunks(jax.nn.log_sigmoid(f_pre.astype(F32) + f_bias.astype(F32)), M_CHUNK)
    b = jnp.cumsum(log_f, axis=-1)
    causal = jnp.tril(jnp.ones((M_CHUNK, M_CHUNK), bool))
    log_w = jnp.where(causal, b[..., :, None] - b[..., None, :] + log_i[..., None, :], -jnp.inf)
    m_intra = jnp.max(log_w, -1)
    log_w_end = b[..., -1:] - b + log_i
    m_end = jnp.max(log_w_end, -1)

    def step(carry, xs):
        C, n, m = carry
        qc, kc, vc, bc, lw, mi, lwe, me = xs
        m_t = jnp.maximum(bc + m[..., None], mi)
        inter = jnp.exp(bc + m[..., None] - m_t)
        s = jnp.einsum('bhtd,bhsd->bhts', qc, kc) * jnp.exp(lw - m_t[..., None])
        num = inter[..., None] * jnp.einsum('bhtd,bhde->bhte', qc, C) + jnp.einsum('bhts,bhse->bhte', s, vc)
        den = inter * jnp.einsum('bhtd,bhd->bht', qc, n) + jnp.sum(s, -1)
        h = num / jnp.maximum(jnp.abs(den), jnp.exp(-m_t))[..., None]
        m_new = jnp.maximum(bc[..., -1] + m, me)
        carry_scale = jnp.exp(bc[..., -1] + m - m_new)
        wk = jnp.exp(lwe - m_new[..., None])[..., None] * kc
        C = carry_scale[..., None, None] * C + jnp.einsum('bhsd,bhse->bhde', wk, vc)
        n = carry_scale[..., None] * n + jnp.sum(wk, -2)
        return (C, n, m_new), h

    N, B, H, L, d = q.shape
    init = (jnp.zeros((B, H, d, d), F32), jnp.zeros((B, H, d), F32), jnp.full((B, H), -jnp.inf, F32))
    _, h = lax.scan(step, init, (q, k, v, b, log_w, m_intra, log_w_end, m_end))
    h = rms_normalize(from_chunks(h))
    h = h.reshape(B, N * L, H * d) * norm_w
    return jax.nn.sigmoid(o_pre.astype(F32)) * h


def stick_breaking_mixer(q, k, v, norm_w):
    q = split_heads(q, SB_HEADS) * (SB_HEAD_DIM ** -0.5)
    k = split_heads(k, SB_HEADS)
    v = split_heads(v, SB_HEADS)
    B, S, H, d = q.shape
    outs = []
    for start in range(0, S, SB_BLOCK):
        end = start + SB_BLOCK
        z = jnp.einsum('bqhd,bkhd->bhqk', q[:, start:end], k[:, :end])
        before = jnp.arange(end)[None, :] < jnp.arange(start, end)[:, None]
        log_keep = jnp.where(before, jax.nn.log_sigmoid(-z), 0.0)
        log_rest = lax.cumsum(log_keep, axis=3, reverse=True) - log_keep
        a = jnp.where(before, jnp.exp(jax.nn.log_sigmoid(z) + log_rest), 0.0)
        outs.append(jnp.einsum('bhqk,bkhd->bqhd', a, v[:, :end]))
    o = rms_normalize(jnp.concatenate(outs, axis=1))
    return o.reshape(B, S, H * d) * norm_w


def gated_deltanet_mixer(q, k, v, z, a, b, conv_w, A_log, dt_bias, norm_w):
    qkv = jnp.concatenate([q, k, v], -1).astype(F32)
    qkv = jax.nn.silu(causal_depthwise_conv(qkv, conv_w.astype(F32)))
    q, k, v = jnp.split(qkv, 3, axis=-1)
    q = l2_normalize(split_heads(q, G_HEADS)) * (G_HEAD_DIM ** -0.5)
    k = l2_normalize(split_heads(k, G_HEADS))
    v = split_heads(v, G_HEADS)
    B, S, H, d = q.shape
    beta = jax.nn.sigmoid(b.astype(F32))
    g = -jnp.exp(A_log.astype(F32)) * jax.nn.softplus(a.astype(F32) + dt_bias.astype(F32))
    q, k, v = to_chunks(q, G_CHUNK), to_chunks(k, G_CHUNK), to_chunks(v, G_CHUNK)
    beta = to_chunks(beta, G_CHUNK)
    gam = jnp.cumsum(to_chunks(g, G_CHUNK), axis=-1)
    incl = jnp.tril(jnp.ones((G_CHUNK, G_CHUNK), bool))
    strict = jnp.tril(jnp.ones((G_CHUNK, G_CHUNK), bool), -1)
    decay = jnp.exp(jnp.where(incl, gam[..., :, None] - gam[..., None, :], -jnp.inf))
    m = jnp.where(strict, beta[..., :, None] * jnp.einsum('nbhtd,nbhsd->nbhts', k, k) * decay, 0.0)
    rhs = jnp.concatenate([v * beta[..., None], k * (beta * jnp.exp(gam))[..., None]], -1)
    sol = lax.linalg.triangular_solve(m + jnp.eye(G_CHUNK, dtype=F32), rhs, left_side=True,
                                      lower=True, unit_diagonal=True)
    u, w = sol[..., :d], sol[..., d:]
    qk = jnp.einsum('nbhtd,nbhsd->nbhts', q, k) * decay
    q_dec = q * jnp.exp(gam)[..., None]
    k_dec = k * jnp.exp(gam[..., -1:] - gam)[..., None]
    chunk_decay = jnp.exp(gam[..., -1])

    def step(state, xs):
        uc, wc, qkc, qdc, kdc, cd = xs
        v_new = uc - jnp.einsum('bhtd,bhde->bhte', wc, state)
        o = jnp.einsum('bhtd,bhde->bhte', qdc, state) + jnp.einsum('bhts,bhse->bhte', qkc, v_new)
        state = cd[..., None, None] * state + jnp.einsum('bhtd,bhte->bhde', kdc, v_new)
        return state, o

    _, o = lax.scan(step, jnp.zeros((B, H, d, d), F32), (u, w, qk, q_dec, k_dec, chunk_decay))
    o = rms_normalize(from_chunks(o)) * norm_w
    return o.reshape(B, S, H * d) * jax.nn.silu(z.astype(F32))


def moe_ffn(x, w_router, b_router, w_gu, b_gu, w_down, b_down):
    B, S, D = x.shape
    T = B * S
    xf = x.reshape(T, D)
    logits = xf.astype(F32) @ w_router.astype(F32) + b_router.astype(F32)
    top_vals, top_idx = lax.top_k(logits, TOP_K)
    gates = jax.nn.softmax(top_vals, axis=-1)
    A = T * TOP_K
    flat_e = top_idx.reshape(A)
    flat_tok = jnp.arange(A, dtype=jnp.int32) // TOP_K
    flat_gate = gates.reshape(A)
    order = jnp.argsort(flat_e)
    e_sorted = flat_e[order]
    counts = jnp.bincount(flat_e, length=N_EXPERTS)
    padded = (counts + MOE_BLOCK - 1) // MOE_BLOCK * MOE_BLOCK
    pad_end = jnp.cumsum(padded)
    pad_start = pad_end - padded
    grp_start = jnp.cumsum(counts) - counts
    dest = pad_start[e_sorted] + jnp.arange(A, dtype=jnp.int32) - grp_start[e_sorted]
    n_blocks = -(-A // MOE_BLOCK) + N_EXPERTS
    P = n_blocks * MOE_BLOCK
    slot_tok = jnp.zeros((P,), jnp.int32).at[dest].set(flat_tok[order])
    slot_gate = jnp.zeros((P,), F32).at[dest].set(flat_gate[order])
    block_e = jnp.minimum(jnp.searchsorted(pad_end, jnp.arange(n_blocks) * MOE_BLOCK, side='right'),
                          N_EXPERTS - 1)
    x_slots = xf[slot_tok].reshape(n_blocks, MOE_BLOCK, D)

    def expert_block(args):
        xb, e = args
        h = xb @ w_gu[e] + b_gu[e]
        gate, up = jnp.split(h, 2, axis=-1)
        gate = jnp.minimum(gate, SWIGLU_LIMIT)
        up = jnp.clip(up, -SWIGLU_LIMIT, SWIGLU_LIMIT)
        act = (up + 1.0) * gate * jax.nn.sigmoid(SWIGLU_ALPHA * gate)
        return act @ w_down[e] + b_down[e]

    y = lax.map(expert_block, (x_slots, block_e)).reshape(P, D)
    y = y * slot_gate[:, None].astype(y.dtype)
    out = jnp.zeros((T, D), y.dtype).at[slot_tok].add(y)
    return out.reshape(B, S, D).astype(x.dtype)


def decoder_layer(h, p_i, w_in, m_i_bias, m_f_bias, m_norm_w, sb_norm_w, g_conv_w, g_A_log,
                  g_dt_bias, g_norm_w, w_out, ln1_g, ln1_b, w_router, b_router, w_gu, b_gu,
                  w_down, b_down, w_ple_gate, w_ple_proj, ln2_g, ln2_b):
    u = h @ w_in
    offsets = [int(o) for o in np.cumsum(IN_SPLITS)[:-1]]
    (mq, mk, mv, mo, mi, mf, sq, sk, sv, gq, gk, gv, gz, ga, gb) = jnp.split(u, offsets, axis=-1)
    y_m = mlstm_mixer(mq, mk, mv, mo, mi, mf, m_i_bias, m_f_bias, m_norm_w)
    y_s = stick_breaking_mixer(sq, sk, sv, sb_norm_w)
    y_g = gated_deltanet_mixer(gq, gk, gv, gz, ga, gb, g_conv_w, g_A_log, g_dt_bias, g_norm_w)
    y = jnp.concatenate([y_m, y_s, y_g], axis=-1).astype(h.dtype)
    h = layer_norm(DEEPNORM_ALPHA * h + y @ w_out, ln1_g, ln1_b)
    ple = jax.nn.sigmoid(h @ w_ple_gate) * (p_i @ w_ple_proj)
    ffn = moe_ffn(h, w_router, b_router, w_gu, b_gu, w_down, b_down)
    return layer_norm(DEEPNORM_ALPHA * h + ffn + ple, ln2_g, ln2_b)


def setup_inputs(seed: int = 0) -> dict:
    key = jax.random.key(seed)
    ks = iter(jax.random.split(key, 32))
    L = DEPTH

    def normal(shape, scale):
        return jax.random.normal(next(ks), shape, F32) * scale

    x = normal((BATCH, SEQ, D_MODEL), 1.0)
    p = normal((DEPTH, BATCH, SEQ, PLE_DIM), 1.0)
    ln0_g = 1.0 + normal((D_MODEL,), 0.02)
    ln0_b = normal((D_MODEL,), 0.02)
    w_in = normal((L, D_MODEL, D_IN), D_MODEL ** -0.5)
    m_i_bias = -2.0 + normal((L, M_HEADS), 0.1)
    m_f_bias = jnp.linspace(3.0, 6.0, M_HEADS, dtype=F32)[None, :] + normal((L, M_HEADS), 0.1)
    m_norm_w = 1.0 + normal((L, M_WIDTH), 0.02)
    sb_norm_w = 1.0 + normal((L, SB_WIDTH), 0.02)
    g_conv_w = normal((L, G_CONV, 3 * G_WIDTH), G_CONV ** -0.5)
    g_A_log = jnp.log(jax.random.uniform(next(ks), (L, G_HEADS), F32, 1.0, 16.0))
    dt = jnp.exp(jax.random.uniform(next(ks), (L, G_HEADS), F32, math.log(1e-3), math.log(1e-1)))
    g_dt_bias = dt + jnp.log(-jnp.expm1(-dt))
    g_norm_w = 1.0 + normal((L, G_HEAD_DIM), 0.02)
    w_out = normal((L, D_MIX, D_MODEL), D_MIX ** -0.5 * DEEPNORM_BETA)
    ln1_g = 1.0 + normal((L, D_MODEL), 0.02)
    ln1_b = normal((L, D_MODEL), 0.02)
    w_router = normal((L, D_MODEL, N_EXPERTS), D_MODEL ** -0.5)
    b_router = normal((L, N_EXPERTS), 0.01)
    w_gu = normal((L, N_EXPERTS, D_MODEL, 2 * D_FF), D_MODEL ** -0.5)
    b_gu = normal((L, N_EXPERTS, 2 * D_FF), 0.01)
    w_down = normal((L, N_EXPERTS, D_FF, D_MODEL), D_FF ** -0.5 * DEEPNORM_BETA)
    b_down = normal((L, N_EXPERTS, D_MODEL), 0.01)
    w_ple_gate = normal((L, D_MODEL, D_MODEL), D_MODEL ** -0.5)
    w_ple_proj = normal((L, PLE_DIM, D_MODEL), PLE_DIM ** -0.5 * DEEPNORM_BETA)
    ln2_g = 1.0 + normal((L, D_MODEL), 0.02)
    ln2_b = normal((L, D_MODEL), 0.02)
    return {'x': x, 'p': p, 'ln0_g': ln0_g, 'ln0_b': ln0_b, 'w_in': w_in,
            'm_i_bias': m_i_bias, 'm_f_bias': m_f_bias, 'm_norm_w': m_norm_w,
            'sb_norm_w': sb_norm_w, 'g_conv_w': g_conv_w, 'g_A_log': g_A_log,
            'g_dt_bias': g_dt_bias, 'g_norm_w': g_norm_w, 'w_out': w_out,
            'ln1_g': ln1_g, 'ln1_b': ln1_b, 'w_router': w_router, 'b_router': b_router,
            'w_gu': w_gu, 'b_gu': b_gu, 'w_down': w_down, 'b_down': b_down,
            'w_ple_gate': w_ple_gate, 'w_ple_proj': w_ple_proj, 'ln2_g': ln2_g, 'ln2_b': ln2_b}


def reference(x, p, ln0_g, ln0_b, w_in, m_i_bias, m_f_bias, m_norm_w, sb_norm_w, g_conv_w,
              g_A_log, g_dt_bias, g_norm_w, w_out, ln1_g, ln1_b, w_router, b_router, w_gu,
              b_gu, w_down, b_down, w_ple_gate, w_ple_proj, ln2_g, ln2_b):
    h = layer_norm(x, ln0_g, ln0_b)
    for i in range(DEPTH):
        h = decoder_layer(h, p[i], w_in[i], m_i_bias[i], m_f_bias[i], m_norm_w[i], sb_norm_w[i],
                          g_conv_w[i], g_A_log[i], g_dt_bias[i], g_norm_w[i], w_out[i],
                          ln1_g[i], ln1_b[i], w_router[i], b_router[i], w_gu[i], b_gu[i],
                          w_down[i], b_down[i], w_ple_gate[i], w_ple_proj[i], ln2_g[i], ln2_b[i])
    return h
```

```python
import bisect
from contextlib import ExitStack

import numpy as np
import concourse.bass as bass
import concourse.mybir as mybir
from concourse.bass_utils import run_bass_kernel_spmd

F32 = mybir.dt.float32
BF16 = mybir.dt.bfloat16
AF = mybir.ActivationFunctionType
ALU = mybir.AluOpType
AX = mybir.AxisListType

S = 2048
D = 1024
NT = 16
DEPTH = 4
D_IN = 3856
NE = 32
ALPHA = (2 * DEPTH) ** 0.25
SEM_ROT = 30000


class Buf:
    __slots__ = ("name", "w", "r", "excl")

    def __init__(self, name):
        self.name = name
        self.w = None
        self.r = {}
        self.excl = False


class Op:
    __slots__ = ("idx", "eng", "method", "args", "kw", "deps", "is_dma", "key",
                 "needs_inc", "seq")


class Prog:
    ENGS = ("pe", "act", "dve", "pool", "sp")

    def __init__(self, nc):
        self.nc = nc
        self.ops = []
        self.dma_keys = {}
        self.st = ExitStack()
        self.nbuf = 0

    def sb(self, name, shape, dtype):
        return self.st.enter_context(self.nc.sbuf_tensor(name, list(shape), dtype))

    def ps(self, name, shape, dtype):
        return self.st.enter_context(self.nc.psum_tensor(name, list(shape), dtype))

    def buf(self, name=None):
        self.nbuf += 1
        return Buf(name or f"b{self.nbuf}")

    def add(self, eng, method, *args, R=(), W=(), key=None, **kw):
        op = Op()
        op.idx = len(self.ops)
        op.eng = eng
        op.method = method
        op.args = args
        op.kw = kw
        op.is_dma = method == "dma_start"
        op.key = key
        op.needs_inc = False
        op.seq = None
        deps = set()
        W = list(W) + [b for b in R if b.excl and b not in W]
        for b in R:
            if b.w is not None:
                deps.add(b.w)
        for b in W:
            if b.w is not None:
                deps.add(b.w)
            for ridx in b.r.values():
                deps.add(ridx)
        op.deps = deps
        rk = ("dma:" + key) if op.is_dma else eng
        for b in R:
            b.r[rk] = op.idx
        for b in W:
            b.w = op.idx
            b.r = {}
        if op.is_dma:
            self.dma_keys.setdefault(key, []).append(op.idx)
        self.ops.append(op)
        return op

    def pe(self, m, *a, **k): return self.add("pe", m, *a, **k)
    def act(self, m, *a, **k): return self.add("act", m, *a, **k)
    def dve(self, m, *a, **k): return self.add("dve", m, *a, **k)
    def pool(self, m, *a, **k): return self.add("pool", m, *a, **k)

    def dma(self, out, in_, R=(), W=(), key=None, eng="sp"):
        return self.add(eng, "dma_start", R=R, W=W, key=key, out=out, in_=in_)

    def _last(self):
        last = {}
        for op in self.ops:
            if op.is_dma:
                last["dma:" + op.key] = op.idx
            elif op.method is not None:
                last[op.eng] = op.idx
        return set(last.values())

    def barrier(self):
        deps = self._last()
        for e in self.ENGS:
            op = self.add(e, None)
            op.deps = set(deps)

    def emit(self):
        nc = self.nc
        ops = self.ops
        fin = self.add("sp", None)
        fin.deps = self._last()

        def skip(dop, op):
            return dop.eng == "pe" and op.eng == "pe" and not op.is_dma

        for op in ops:
            for d in op.deps:
                dop = ops[d]
                if dop.is_dma or skip(dop, op):
                    continue
                dop.needs_inc = True
        cnt = {e: 0 for e in self.ENGS}
        for op in ops:
            if op.needs_inc:
                cnt[op.eng] += 1
                op.seq = cnt[op.eng]
        sems = {}
        for e in self.ENGS:
            for ph in range(cnt[e] // SEM_ROT + 1):
                sems[f"s_{e}_{ph}"] = self.st.enter_context(nc.semaphore(f"s_{e}_{ph}"))
        for key in self.dma_keys:
            sems["d_" + key] = self.st.enter_context(nc.semaphore("d_" + key))
        per_eng = {e: [op for op in ops if op.eng == e] for e in self.ENGS}
        nw = [0]

        def emit_engine(ename, e):
            waited = {}
            for op in per_eng[ename]:
                need = {}
                for d in op.deps:
                    dop = ops[d]
                    if dop.is_dma:
                        lst = self.dma_keys[dop.key]
                        c = bisect.bisect_left(lst, op.idx)
                        sn = "d_" + dop.key
                        v = 16 * c
                    else:
                        if skip(dop, op):
                            continue
                        ph = (dop.seq - 1) // SEM_ROT
                        sn = f"s_{dop.eng}_{ph}"
                        v = (dop.seq - 1) % SEM_ROT + 1
                    if need.get(sn, 0) < v:
                        need[sn] = v
                for sn, v in need.items():
                    if waited.get(sn, 0) >= v:
                        continue
                    waited[sn] = v
                    e.wait_ge(sems[sn], v)
                    nw[0] += 1
                if op.method is None:
                    continue
                ins = getattr(e, op.method)(*op.args, **op.kw)
                if op.is_dma:
                    ins.then_inc(sems["d_" + op.key], 16)
                elif op.needs_inc:
                    ph = (op.seq - 1) // SEM_ROT
                    ins.then_inc(sems[f"s_{ename}_{ph}"], 1)

        block = self.st.enter_context(nc.Block())

        @block.tensor
        def _(e): emit_engine("pe", e)

        @block.scalar
        def _(e): emit_engine("act", e)

        @block.vector
        def _(e): emit_engine("dve", e)

        @block.gpsimd
        def _(e): emit_engine("pool", e)

        @block.sync
        def _(e): emit_engine("sp", e)

        self.st.close()
        return {"n_ops": len(ops), "n_waits": nw[0], "cnt": cnt}


ARENA_F32 = 27000

W_NAMES = ["w_in", "m_i_bias", "m_f_bias", "m_norm_w", "sb_norm_w", "g_conv_w", "g_A_log",
           "g_dt_bias", "g_norm_w", "w_out", "ln1_g", "ln1_b", "w_router", "b_router", "w_gu",
           "b_gu", "w_down", "b_down", "w_ple_gate", "w_ple_proj", "ln2_g", "ln2_b"]
W_SHAPES = {"w_in": [D, D_IN], "m_i_bias": [4], "m_f_bias": [4], "m_norm_w": [256],
            "sb_norm_w": [256], "g_conv_w": [4, 1536], "g_A_log": [4], "g_dt_bias": [4],
            "g_norm_w": [128], "w_out": [D, D], "ln1_g": [D], "ln1_b": [D],
            "w_router": [D, NE], "b_router": [NE], "w_gu": [NE, D, 2 * D], "b_gu": [NE, 2 * D],
            "w_down": [NE, D, D], "b_down": [NE, D], "w_ple_gate": [D, D],
            "w_ple_proj": [256, D], "ln2_g": [D], "ln2_b": [D]}


def build(layers, first, last, dbg=(), phases=("M", "S", "G", "O", "E")):
    nc = bass.Bass("TRN2", target_bir_lowering=False)

    def din(name, shape, dt=F32):
        return nc.dram_tensor(name, list(shape), dt, kind="ExternalInput").ap()

    x_d = din("x", [S, D])
    p_d = din("p", [len(layers), S, 256])
    ln0g_d = din("ln0_g", [D])
    ln0b_d = din("ln0_b", [D])
    need = set()
    if set(phases) & {"M", "S", "G"}:
        need |= {"w_in", "m_i_bias", "m_f_bias", "m_norm_w", "sb_norm_w", "g_conv_w", "g_A_log",
                 "g_dt_bias", "g_norm_w"}
    if "O" in phases:
        need |= {"w_out", "ln1_g", "ln1_b", "w_router", "b_router", "w_ple_gate", "w_ple_proj",
                 "b_down"}
    if "E" in phases:
        need |= {"w_gu", "b_gu", "w_down", "ln2_g", "ln2_b"}
    nl = len(layers)
    Wfull = {n: din(n, [nl] + W_SHAPES[n]) for n in W_NAMES if n in need}

    class _WL:
        def __getitem__(self, n):
            class _L:
                def __getitem__(s2, l):
                    return Wfull[n][l - layers[0]]
            return _L()
    Wd = _WL()
    cst_d = din("cst", [128, 5, 128])
    sel_d = din("sel", [4, 512])
    out_d = nc.dram_tensor("out", [S, D], F32, kind="ExternalOutput").ap()
    dbg_d = {}
    for name in dbg:
        if name in ("YT",):
            dbg_d[name] = nc.dram_tensor("dbg_" + name, [128, 8, S], BF16, kind="ExternalOutput").ap()
        else:
            dbg_d[name] = nc.dram_tensor("dbg_" + name, [S, D], F32, kind="ExternalOutput").ap()

    P = Prog(nc)
    X = P.sb("X", [128, NT, D], F32)
    XT = P.sb("XT", [128, 8, S], BF16)
    CST = P.sb("CST", [128, 5, 128], F32)
    SEL = P.sb("SEL", [4, 512], F32)
    AR = P.sb("AR", [128, ARENA_F32], F32)
    bX = [P.buf(f"X{i}") for i in range(NT)]
    bXT = [P.buf(f"XT{i}") for i in range(NT)]
    bC = P.buf("cst")
    ident = CST[:, 0, :]
    ones = CST[:, 1, :]
    TRI = CST[:, 2, :]
    UT1 = CST[:, 3, :]
    L1 = CST[:, 4, :]

    banks = [P.ps(f"bank{i}", [128, 512], F32) for i in range(8)]
    bbank = [P.buf(f"bank{i}") for i in range(8)]
    for b_ in bbank:
        b_.excl = True
    bctr = [0]

    def psum():
        i = bctr[0] % 6
        bctr[0] += 1
        return banks[i], bbank[i]

    actr = [0]

    def psacc():
        i = 6 + actr[0] % 2
        actr[0] += 1
        return banks[i], bbank[i]

    aoff = [0]

    def a_reset(off=0):
        aoff[0] = off

    def a_f32(shape, p0=0):
        n = int(np.prod(shape[1:]))
        v = AR[p0:p0 + shape[0], aoff[0]:aoff[0] + n]
        aoff[0] += n
        assert aoff[0] <= ARENA_F32, aoff[0]
        if len(shape) == 3:
            v = v.rearrange("p (a b) -> p a b", a=shape[1])
        return v, P.buf()

    def a_bf16(shape):
        n = int(np.prod(shape[1:]))
        nf = (n + 1) // 2
        v = AR[0:shape[0], aoff[0]:aoff[0] + nf].bitcast(BF16)[:, 0:n]
        aoff[0] += nf
        assert aoff[0] <= ARENA_F32, aoff[0]
        if len(shape) == 3:
            v = v.rearrange("p (a b) -> p a b", a=shape[1])
        return v, P.buf()

    def rsqrt_small(out, in_, scale, eps, b):
        P.act("activation", out=out, in_=in_, func=AF.Ln, scale=scale, bias=eps, R=[b], W=[b])
        P.act("activation", out=out, in_=out, func=AF.Exp, scale=-0.5, R=[b], W=[b])

    def layer_norm_tile(t, G, B, bG, bB, junk, bj, st, bst):
        xt = X[:, t, :]
        P.dve("reduce_sum", out=st[:, 0:1], in_=xt, axis=AX.X, R=[bX[t]], W=[bst])
        P.dve("tensor_scalar", out=st[:, 1:2], in0=st[:, 0:1], scalar1=-1.0 / D, scalar2=None,
              op0=ALU.mult, R=[bst], W=[bst])
        P.act("activation", out=junk, in_=xt, func=AF.Square, bias=st[:, 1:2], scale=1.0,
              accum_out=st[:, 2:3], R=[bX[t], bst], W=[bj, bst])
        rsqrt_small(st[:, 3:4], st[:, 2:3], 1.0 / D, 1e-5, bst)
        P.dve("tensor_scalar", out=xt, in0=xt, scalar1=st[:, 1:2], scalar2=st[:, 3:4],
              op0=ALU.add, op1=ALU.mult, R=[bX[t], bst], W=[bX[t]])
        P.pool("tensor_tensor", out=xt, in0=xt, in1=G, op=ALU.mult, R=[bX[t], bG], W=[bX[t]])
        P.dve("tensor_tensor", out=xt, in0=xt, in1=B, op=ALU.add, R=[bX[t], bB], W=[bX[t]])

    def transpose_tile(t, extra=None):
        for k in range(0, 8, 4):
            pt, pb = psum()
            for j in range(4):
                P.pe("transpose", out=pt[:, j * 128:(j + 1) * 128],
                     in_=X[:, t, (k + j) * 128:(k + j + 1) * 128], identity=ident,
                     R=[bX[t], bC], W=[pb])
            P.act("activation", out=XT[:, k:k + 4, t * 128:(t + 1) * 128],
                  in_=pt[:].rearrange("p (j c) -> p j c", j=4), func=AF.Copy,
                  R=[pb], W=[bXT[t]])
            if extra is not None:
                extra(k, pt, pb)

    wkey = [0]

    def load_w(dst, bdst, src_cols_list, slot):
        for src, c0, n in src_cols_list:
            P.dma(dst[:, :, c0:c0 + n], src.rearrange("(k p) c -> p k c", p=128),
                  W=[bdst], key=f"w{slot}", eng="pool")

    def proj_fm(Wt, bW, c0, ncols, evac, pbase=0, tbs=range(4)):
        for tb in tbs:
            pt, pb = psum()
            for k in range(8):
                P.pe("matmul", pt[pbase:pbase + ncols, :], lhsT=Wt[:, k, c0:c0 + ncols],
                     rhs=XT[:, k, tb * 512:(tb + 1) * 512], start=(k == 0), stop=(k == 7),
                     R=[bW] + bXT[tb * 4:tb * 4 + 4], W=[pb])
            evac(tb, pt[pbase:pbase + ncols, :], pb)

    def proj_tm(Wt, bW, c0, ncols, t):
        pt, pb = psum()
        for k in range(8):
            P.pe("matmul", pt[:, 0:ncols], lhsT=XT[:, k, t * 128:(t + 1) * 128],
                 rhs=Wt[:, k, c0:c0 + ncols], start=(k == 0), stop=(k == 7),
                 R=[bW, bXT[t]], W=[pb])
        return pt, pb

    def bcast_load(dst, bdst, src, key="c"):
        P.dma(dst, src.partition_broadcast(128), W=[bdst], key=key)

    def dump_X(name):
        if name in dbg_d:
            for t in range(NT):
                P.dma(dbg_d[name][t * 128:(t + 1) * 128, :], X[:, t, :], R=[bX[t]], key="dbg")

    P.dma(CST[:], cst_d, W=[bC], key="c")
    P.dma(SEL[:], sel_d, W=[bC], key="c")
    for t in range(NT):
        P.dma(X[:, t, :], x_d[t * 128:(t + 1) * 128, :], W=[bX[t]], key="x")
    a_reset()
    GATES, bGA = a_f32([128, NT, NE])
    AE = aoff[0]
    YT, _ = a_bf16([128, 8, S])
    bYT = [P.buf(f"YT{k}") for k in range(8)]
    A0 = aoff[0]
    if "YT" in dbg_d:
        for k in range(8):
            P.pool("memset", YT[:, k, :], 0.0, W=[bYT[k]])
    G_t, bG = a_f32([128, D])
    B_t, bB = a_f32([128, D])
    junk, bj = a_f32([128, D])
    st, bst = a_f32([128, 8])
    if first:
        bcast_load(G_t, bG, ln0g_d)
        bcast_load(B_t, bB, ln0b_d)
        for t in range(NT):
            layer_norm_tile(t, G_t, B_t, bG, bB, junk, bj, st, bst)
    for t in range(NT):
        transpose_tile(t)
    dump_X("h0")

    for l in layers:
        w_in = Wd["w_in"][l]
        P.barrier()
        a_reset(A0)
        qT, bq = a_bf16([64, S])
        kT, bk = a_bf16([64, S])
        vext, bv = a_bf16([128, NT, 80])
        Ytok, bY = a_f32([128, NT, 128])
        Wm = [a_bf16([128, 8, 256]) for _ in range(2)]
        wTt = [a_bf16([128, 512]) for _ in range(2)]
        oacc, boacc = a_f32([128, 4, 65])
        Gtok, bGtok = a_f32([128, NT, 4])
        emtok, bem = a_f32([128, NT, 4])
        sm, bsm = a_f32([128, 16])
        hh, bhh = a_f32([128, 64])
        osig, bos = a_f32([128, 64])
        NWm, bNWm = a_f32([128, 256])
        NWs, bNWs = a_f32([128, 256])
        gb4, bgb4 = a_f32([4, 4])
        A1 = aoff[0]
        gA, bgA = a_f32([4, S])
        gB, bgB = a_f32([4, S])
        gC, bgC = a_f32([4, S])
        MGrow, bMG = a_f32([128, S])
        DT = [a_f32([128, 512]) for _ in range(2)]
        a_reset(A1)
        e_t = [a_f32([128, 512]) for _ in range(2)]
        sp_t = [a_f32([128, 512]) for _ in range(2)]
        arg_t = [a_f32([128, 512]) for _ in range(2)]
        Suf, bSuf = a_f32([128, 512])
        bcast_load(NWm, bNWm, Wd["m_norm_w"][l])
        bcast_load(NWs, bNWs, Wd["sb_norm_w"][l])
        P.dma(gb4[:, 0:1], Wd["m_i_bias"][l].rearrange("(h o) -> h o", o=1), W=[bgb4], key="c")
        P.dma(gb4[:, 1:2], Wd["m_f_bias"][l].rearrange("(h o) -> h o", o=1), W=[bgb4], key="c")
        P.dve("tensor_scalar", out=gb4[:, 2:3], in0=gb4[:, 1:2], scalar1=-1.0, scalar2=None,
              op0=ALU.mult, R=[bgb4], W=[bgb4])
        wslot = [0]

        def next_w():
            i = wslot[0] % 2
            wslot[0] += 1
            return Wm[i][0], Wm[i][1], i

        if "M" in phases:
            Wt, bW, sl = next_w()
            load_w(Wt, bW, [(w_in[:, 1024:1032], 0, 8)], sl)

            def ev_i(tb, ps, pb):
                P.act("activation", out=gA[:, tb * 512:(tb + 1) * 512], in_=ps, func=AF.Identity,
                      bias=gb4[:, 0:1], scale=1.0, R=[pb, bgb4], W=[bgA])
            proj_fm(Wt, bW, 0, 4, ev_i)

            def ev_f(tb, ps, pb):
                P.act("activation", out=gB[:, tb * 512:(tb + 1) * 512], in_=ps, func=AF.Exp,
                      bias=gb4[:, 2:3], scale=-1.0, R=[pb, bgb4], W=[bgB])
            proj_fm(Wt, bW, 4, 4, ev_f)
            P.act("activation", out=gB, in_=gB, func=AF.Ln, bias=1.0, scale=1.0, R=[bgB], W=[bgB])
            P.dve("tensor_tensor_scan", out=gC, data0=gB, data1=gB, initial=0.0,
                  op0=ALU.add, op1=ALU.max, R=[bgB], W=[bgC])
            P.dve("tensor_tensor", out=gA, in0=gA, in1=gC, op=ALU.add, R=[bgA, bgC], W=[bgA])
            P.dve("tensor_tensor_scan", out=gB, data0=gA, data1=gA, initial=-1e30,
                  op0=ALU.max, op1=ALU.max, R=[bgA], W=[bgB])
            P.dve("tensor_tensor", out=gC, in0=gC, in1=gB, op=ALU.subtract, R=[bgC, bgB], W=[bgC])
            pt, pb = psum()
            for j in range(NT):
                P.pe("transpose", out=pt[:, j * 4:(j + 1) * 4], in_=gA[:, j * 128:(j + 1) * 128],
                     identity=ident[0:4, 0:4], R=[bgA, bC], W=[pb])
                P.pe("transpose", out=pt[:, 64 + j * 4:64 + (j + 1) * 4],
                     in_=gC[:, j * 128:(j + 1) * 128], identity=ident[0:4, 0:4],
                     R=[bgC, bC], W=[pb])
            P.dve("tensor_copy", out=Gtok, in_=pt[:, 0:64].rearrange("p (a b) -> p a b", a=NT),
                  R=[pb], W=[bGtok])
            P.act("activation", out=emtok, in_=pt[:, 64:128].rearrange("p (a b) -> p a b", a=NT),
                  func=AF.Exp, R=[pb], W=[bem])
            P.dve("memset", vext[:, :, 64:66], 1.0, W=[bv])

        def epilogue(qb, pacc, pab, h, hslot, width, NW, bNW, is_m):
            P.act("activation", out=oacc[:, :, 0:width],
                  in_=pacc[:, 0:4 * width].rearrange("p (a b) -> p a b", a=4), func=AF.Copy,
                  R=[pab], W=[boacc])
            for tt in range(4):
                t = 4 * qb + tt
                num = oacc[:, tt, 0:64]
                if is_m:
                    den = oacc[:, tt, 64:65]
                    P.dve("tensor_scalar", out=sm[:, 0:1], in0=den, scalar1=-1.0, scalar2=None,
                          op0=ALU.mult, R=[boacc], W=[bsm])
                    P.dve("tensor_tensor", out=sm[:, 1:2], in0=sm[:, 0:1], in1=den, op=ALU.max,
                          R=[bsm, boacc], W=[bsm])
                    P.dve("tensor_tensor", out=sm[:, 2:3], in0=sm[:, 1:2], in1=emtok[:, t, h:h + 1],
                          op=ALU.max, R=[bsm, bem], W=[bsm])
                    P.dve("reciprocal", out=sm[:, 3:4], in_=sm[:, 2:3], R=[bsm], W=[bsm])
                    P.dve("tensor_scalar", out=hh, in0=num, scalar1=sm[:, 3:4], scalar2=None,
                          op0=ALU.mult, R=[boacc, bsm], W=[bhh])
                    src, bsrc = hh, bhh
                else:
                    src, bsrc = num, boacc
                P.act("activation", out=osig, in_=src, func=AF.Square, accum_out=sm[:, 4:5],
                      R=[bsrc], W=[bos, bsm])
                rsqrt_small(sm[:, 5:6], sm[:, 4:5], 1.0 / 64, 1e-6, bsm)
                dst = Ytok[:, t, hslot * 64:(hslot + 1) * 64]
                P.dve("scalar_tensor_tensor", out=dst, in0=src, scalar=sm[:, 5:6],
                      in1=NW[:, h * 64:(h + 1) * 64], op0=ALU.mult, op1=ALU.mult,
                      R=[bsrc, bsm, bNW], W=[bY])
                if is_m:
                    pt, pb = proj_tm(Wcur[0], Wcur[1], 192, 64, t)
                    P.act("activation", out=osig, in_=pt[:, 0:64], func=AF.Sigmoid, R=[pb], W=[bos])
                    P.pool("tensor_tensor", out=dst, in0=dst, in1=osig, op=ALU.mult,
                           R=[bY, bos], W=[bY])

        def flush_Y(chunk, Ytok=Ytok, bY=bY):
            for t0 in range(0, NT, 4):
                pt, pb = psum()
                for j in range(4):
                    P.pe("transpose", out=pt[:, j * 128:(j + 1) * 128], in_=Ytok[:, t0 + j, :],
                         identity=ident, R=[bY, bC], W=[pb])
                P.act("activation", out=YT[:, chunk, t0 * 128:(t0 + 4) * 128], in_=pt[:],
                      func=AF.Copy, R=[pb], W=[bYT[chunk]])

        Wcur = [None, None]
        if "M" in phases:
            for h in range(4):
                Wt, bW, sl = next_w()
                Wcur[0], Wcur[1] = Wt, bW
                load_w(Wt, bW, [(w_in[:, h * 64:(h + 1) * 64], 0, 64),
                                (w_in[:, 256 + h * 64:256 + (h + 1) * 64], 64, 64),
                                (w_in[:, 512 + h * 64:512 + (h + 1) * 64], 128, 64),
                                (w_in[:, 768 + h * 64:768 + (h + 1) * 64], 192, 64)], sl)

                def ev_q(tb, ps, pb):
                    P.act("activation", out=qT[:, tb * 512:(tb + 1) * 512], in_=ps, func=AF.Copy,
                          R=[pb], W=[bq])
                proj_fm(Wt, bW, 0, 64, ev_q)

                def ev_k(tb, ps, pb):
                    P.act("activation", out=kT[:, tb * 512:(tb + 1) * 512], in_=ps, func=AF.Copy,
                          scale=0.125, R=[pb], W=[bk])
                proj_fm(Wt, bW, 64, 64, ev_k)
                for t in range(NT):
                    pt, pb = proj_tm(Wt, bW, 128, 64, t)
                    P.dve("tensor_copy", out=vext[:, t, 0:64], in_=pt[:, 0:64], R=[pb], W=[bv])
                for tb in range(4):
                    pt, pb = psum()
                    P.pe("matmul", pt[:, :], lhsT=SEL[0:4, h * 128:(h + 1) * 128],
                         rhs=gB[:, tb * 512:(tb + 1) * 512], start=True, stop=True,
                         R=[bC, bgB], W=[pb])
                    P.dve("tensor_copy", out=MGrow[:, tb * 512:(tb + 1) * 512], in_=pt[:, :],
                          R=[pb], W=[bMG])
                pi = 0
                for qb in range(4):
                    pacc, pab = psacc()
                    for kb in range(4 * qb + 4):
                        r = kb - 4 * qb
                        c0 = max(r, 0) * 128
                        n = 512 - c0
                        q0 = qb * 512 + c0
                        pz, pzb = psum()
                        P.pe("matmul", pz[:, 0:n], lhsT=kT[:, kb * 128:(kb + 1) * 128],
                             rhs=qT[:, q0:q0 + n], start=True, stop=True, R=[bk, bq], W=[pzb])
                        dt_, bdt = DT[pi % 2]
                        w_, bw_ = wTt[pi % 2]
                        pi += 1
                        P.act("activation", out=dt_[:, 0:n], in_=MGrow[:, q0:q0 + n], func=AF.Exp,
                              scale=-1.0, bias=Gtok[:, kb, h:h + 1], R=[bMG, bGtok], W=[bdt])
                        if r >= 0:
                            P.pool("tensor_tensor", out=dt_[:, 0:128], in0=dt_[:, 0:128], in1=TRI,
                                   op=ALU.mult, R=[bdt, bC], W=[bdt])
                        P.dve("tensor_tensor", out=w_[:, 0:n], in0=pz[:, 0:n], in1=dt_[:, 0:n],
                              op=ALU.mult, R=[pzb, bdt], W=[bw_])
                        for tt in range(max(r, 0), 4):
                            cc = (tt - max(r, 0)) * 128
                            P.pe("matmul", pacc[:, tt * 65:(tt + 1) * 65], lhsT=w_[:, cc:cc + 128],
                                 rhs=vext[:, kb, 0:65], start=(kb == 0 and tt == 0), stop=(kb == 4 * qb + 3 and tt == 3),
                                 R=[bw_, bv], W=[pab])
                    epilogue(qb, pacc, pab, h, h % 2, 65, NWm, bNWm, True)
                if h % 2 == 1:
                    flush_Y(h // 2)

        if "S" in phases:
            P.barrier()
            for h in range(4):
                Wt, bW, sl = next_w()
                load_w(Wt, bW, [(w_in[:, 1032 + h * 64:1032 + (h + 1) * 64], 0, 64),
                                (w_in[:, 1288 + h * 64:1288 + (h + 1) * 64], 64, 64),
                                (w_in[:, 1544 + h * 64:1544 + (h + 1) * 64], 128, 64)], sl)

                def ev_q(tb, ps, pb):
                    P.act("activation", out=qT[:, tb * 512:(tb + 1) * 512], in_=ps, func=AF.Copy,
                          scale=0.125, R=[pb], W=[bq])
                proj_fm(Wt, bW, 0, 64, ev_q)

                def ev_k(tb, ps, pb):
                    P.act("activation", out=kT[:, tb * 512:(tb + 1) * 512], in_=ps, func=AF.Copy,
                          R=[pb], W=[bk])
                proj_fm(Wt, bW, 64, 64, ev_k)
                for t in range(NT):
                    pt, pb = proj_tm(Wt, bW, 128, 64, t)
                    P.dve("tensor_copy", out=vext[:, t, 0:64], in_=pt[:, 0:64], R=[pb], W=[bv])
                pi = 0
                for qb in range(4):
                    pacc, pab = psacc()
                    P.pool("memset", Suf, 0.0, W=[bSuf])
                    for kb in range(4 * qb + 3, -1, -1):
                        r = kb - 4 * qb
                        c0 = max(r, 0) * 128
                        n = 512 - c0
                        q0 = qb * 512 + c0
                        e_, be_ = e_t[pi % 2]
                        s_, bs_ = sp_t[pi % 2]
                        a_, ba_ = arg_t[pi % 2]
                        w_, bw_ = wTt[pi % 2]
                        pi += 1
                        pz, pzb = psum()
                        P.pe("matmul", pz[:, 0:n], lhsT=kT[:, kb * 128:(kb + 1) * 128],
                             rhs=qT[:, q0:q0 + n], start=True, stop=True, R=[bk, bq], W=[pzb])
                        P.act("activation", out=e_[:, 0:n], in_=pz[:, 0:n], func=AF.Exp,
                              R=[pzb], W=[be_])
                        P.act("activation", out=s_[:, 0:n], in_=e_[:, 0:n], func=AF.Ln, bias=1.0,
                              scale=1.0, R=[be_], W=[bs_])
                        if r >= 0:
                            P.pool("tensor_tensor", out=s_[:, 0:128], in0=s_[:, 0:128], in1=UT1,
                                   op=ALU.mult, R=[bs_, bC], W=[bs_])
                        p2, p2b = psum()
                        P.pe("matmul", p2[:, 0:n], lhsT=L1, rhs=s_[:, 0:n], start=True, stop=True,
                             R=[bC, bs_], W=[p2b])
                        p3, p3b = psum()
                        P.pe("matmul", p3[:, 0:n], lhsT=ones, rhs=s_[:, 0:n], start=True, stop=True,
                             R=[bC, bs_], W=[p3b])
                        P.dve("tensor_tensor", out=a_[:, 0:n], in0=pz[:, 0:n], in1=s_[:, 0:n],
                              op=ALU.subtract, R=[pzb, bs_], W=[ba_])
                        P.dve("tensor_tensor", out=a_[:, 0:n], in0=a_[:, 0:n], in1=p2[:, 0:n],
                              op=ALU.subtract, R=[ba_, p2b], W=[ba_])
                        P.pool("tensor_tensor", out=a_[:, 0:n], in0=a_[:, 0:n], in1=Suf[:, c0:512],
                               op=ALU.subtract, R=[ba_, bSuf], W=[ba_])
                        P.act("activation", out=w_[:, 0:n], in_=a_[:, 0:n], func=AF.Exp,
                              R=[ba_], W=[bw_])
                        if r >= 0:
                            P.pool("tensor_tensor", out=w_[:, 0:128], in0=w_[:, 0:128], in1=UT1,
                                   op=ALU.mult, R=[bw_, bC], W=[bw_])
                        P.dve("tensor_tensor", out=Suf[:, c0:512], in0=Suf[:, c0:512],
                              in1=p3[:, 0:n], op=ALU.add, R=[bSuf, p3b], W=[bSuf])
                        for tt in range(max(r, 0), 4):
                            cc = (tt - max(r, 0)) * 128
                            P.pe("matmul", pacc[:, tt * 64:(tt + 1) * 64], lhsT=w_[:, cc:cc + 128],
                                 rhs=vext[:, kb, 0:64], start=(kb == 4 * qb + 3 and tt == 3), stop=(kb == 0 and tt == 3),
                                 R=[bw_, bv], W=[pab])
                    epilogue(qb, pacc, pab, h, h % 2, 64, NWs, bNWs, False)
                if h % 2 == 1:
                    flush_Y(2 + h // 2)


        if "G" in phases:
            P.barrier()
            a_reset(A0)
            cin, bcin = a_f32([128, S + 4])
            acc, bacc = a_f32([128, S])
            V32, bV32 = a_f32([128, S])
            Uv = acc.rearrange("p (a b) -> p a b", a=NT)
            Yv = V32.rearrange("p (a b) -> p a b", a=NT)
            qTn, bqn = a_bf16([128, S])
            kTn, bkn = a_bf16([128, S])
            KDEC, bKD = a_bf16([128, NT, 128])
            QKT, bQK = a_bf16([128, NT, 128])
            WTt, bWT = a_bf16([128, NT, 128])
            Wgs = [a_bf16([128, 8, 128]) for _ in range(3)]
            sqb, bsqb = a_f32([128, 512])
            rn, brn = a_f32([128, 512])
            g2d, bg2 = a_f32([128, 64])
            be2d, bbe = a_f32([128, 64])
            gam, bgam = a_f32([128, 64])
            glast, bgl = a_f32([128, 64])
            egam, beg = a_f32([128, 64])
            kds, bkds = a_f32([128, 64])
            cdv, bcd = a_f32([128, 64])
            bwv, bbw = a_f32([128, 64])
            g3 = g2d.rearrange("p (a b) -> p a b", a=NT)
            be3 = be2d.rearrange("p (a b) -> p a b", a=NT)
            DTB, bDTB = a_f32([128, 4])
            NEA, bNEA = a_f32([128, 4])
            t4, bt4 = a_f32([128, 4])
            convw, bcw = a_f32([128, 12, 4])
            GNW, bGNW = a_f32([128, 128])
            TG, bTG = a_f32([128, 128])
            dec, bdec = a_f32([128, 256])
            Mm, bMm = a_f32([128, 128])
            MT, bMT = a_f32([128, 128])
            Pm, bPm = a_f32([128, 128])
            Xs = [a_f32([128, 128]) for _ in range(2)]
            XTs = [a_f32([128, 128]) for _ in range(2)]
            RHS, bRHS = a_f32([128, 256])
            S_f, bSf = a_f32([128, 128])
            S_b, bSb = a_bf16([128, 128])
            vnb, bvn = a_bf16([128, 128])
            otmp, bot = a_f32([128, 128])
            zs, bzs = a_f32([128, 128])
            sm2, bsm2 = a_f32([128, 8])
            gslot = [0]

            def next_g():
                i = gslot[0] % 3
                gslot[0] += 1
                return Wgs[i][0], Wgs[i][1], 4 + i

            bcast_load(DTB, bDTB, Wd["g_dt_bias"][l])
            bcast_load(NEA, bNEA, Wd["g_A_log"][l])
            bcast_load(GNW, bGNW, Wd["g_norm_w"][l])
            P.act("activation", out=NEA, in_=NEA, func=AF.Exp, R=[bNEA], W=[bNEA])
            P.dve("tensor_scalar", out=NEA, in0=NEA, scalar1=-1.0, scalar2=None, op0=ALU.mult,
                  R=[bNEA], W=[bNEA])
            P.dma(acc[0:4, 0:1536], Wd["g_conv_w"][l], W=[bacc], key="c")
            pt, pb = psum()
            for c in range(12):
                P.pe("transpose", out=pt[:, c * 4:(c + 1) * 4], in_=acc[0:4, c * 128:(c + 1) * 128],
                     identity=ident[0:4, 0:4], R=[bacc, bC], W=[pb])
            P.dve("tensor_copy", out=convw, in_=pt[:, 0:48].rearrange("p (a b) -> p a b", a=12),
                  R=[pb], W=[bcw])
            P.dve("memset", cin[:, 0:3], 0.0, W=[bcin])
            Wt, bW, sl = next_g()
            load_w(Wt, bW, [(w_in[:, 3848:3856], 0, 8)], sl)
            for t in range(NT):
                pt, pb = proj_tm(Wt, bW, 0, 8, t)
                P.dve("tensor_tensor", out=t4, in0=pt[:, 0:4], in1=DTB, op=ALU.add, R=[pb, bDTB], W=[bt4])
                P.act("activation", out=t4, in_=t4, func=AF.Exp, R=[bt4], W=[bt4])
                P.act("activation", out=t4, in_=t4, func=AF.Ln, bias=1.0, scale=1.0, R=[bt4], W=[bt4])
                P.dve("tensor_tensor", out=g3[:, t, :], in0=t4, in1=NEA, op=ALU.mult, R=[bt4, bNEA], W=[bg2])
                P.act("activation", out=be3[:, t, :], in_=pt[:, 4:8], func=AF.Sigmoid, R=[pb], W=[bbe])
            pt, pb = psum()
            P.pe("matmul", pt[:, 0:64], lhsT=TRI, rhs=g2d, start=True, stop=True, R=[bC, bg2], W=[pb])
            P.pe("matmul", pt[:, 64:128], lhsT=ones, rhs=g2d, start=True, stop=True, R=[bC, bg2], W=[pb])
            P.dve("tensor_copy", out=gam, in_=pt[:, 0:64], R=[pb], W=[bgam])
            P.dve("tensor_copy", out=glast, in_=pt[:, 64:128], R=[pb], W=[bgl])
            P.act("activation", out=egam, in_=gam, func=AF.Exp, R=[bgam], W=[beg])
            P.act("activation", out=cdv, in_=glast, func=AF.Exp, R=[bgl], W=[bcd])
            P.dve("tensor_tensor", out=kds, in0=glast, in1=gam, op=ALU.subtract, R=[bgl, bgam], W=[bkds])
            P.act("activation", out=kds, in_=kds, func=AF.Exp, R=[bkds], W=[bkds])
            P.dve("tensor_tensor", out=bwv, in0=be2d, in1=egam, op=ALU.mult, R=[bbe, beg], W=[bbw])

            def l2n(tb, dst_fn):
                tsl = slice(tb * 512, (tb + 1) * 512)
                P.pool("tensor_tensor", out=sqb, in0=acc[:, tsl], in1=acc[:, tsl], op=ALU.mult,
                       R=[bacc], W=[bsqb])
                pt, pb = psum()
                P.pe("matmul", pt[:, :], lhsT=ones, rhs=sqb, start=True, stop=True, R=[bC, bsqb], W=[pb])
                P.act("activation", out=rn, in_=pt[:, :], func=AF.Ln, bias=1e-6, scale=1.0, R=[pb], W=[brn])
                P.act("activation", out=rn, in_=rn, func=AF.Exp, scale=-0.5, R=[brn], W=[brn])
                dst_fn(tsl)

            for h in range(4):
                for nm, col0, cc in (("q", 1800 + h * 128, h), ("v", 2824 + h * 128, 8 + h),
                                     ("k", 2312 + h * 128, 4 + h)):
                    Wt, bW, sl = next_g()
                    load_w(Wt, bW, [(w_in[:, col0:col0 + 128], 0, 128)], sl)

                    def ev_c(tb, ps, pb):
                        P.act("activation", out=cin[:, 3 + tb * 512:3 + (tb + 1) * 512], in_=ps,
                              func=AF.Copy, R=[pb], W=[bcin])
                    proj_fm(Wt, bW, 0, 128, ev_c)
                    P.dve("tensor_scalar", out=acc, in0=cin[:, 3:3 + S], scalar1=convw[:, cc, 3:4],
                          scalar2=None, op0=ALU.mult, R=[bcin, bcw], W=[bacc])
                    for j in (2, 1, 0):
                        P.dve("scalar_tensor_tensor", out=acc, in0=cin[:, j:j + S],
                              scalar=convw[:, cc, j:j + 1], in1=acc, op0=ALU.mult, op1=ALU.add,
                              R=[bcin, bcw, bacc], W=[bacc])
                    P.act("activation", out=acc, in_=acc, func=AF.Silu, R=[bacc], W=[bacc])
                    if nm == "q":
                        for tb in range(4):
                            l2n(tb, lambda tsl: P.dve(
                                "scalar_tensor_tensor", out=qTn[:, tsl], in0=acc[:, tsl],
                                scalar=128 ** -0.5, in1=rn, op0=ALU.mult, op1=ALU.mult,
                                R=[bacc, brn], W=[bqn]))
                    elif nm == "v":
                        P.pool("tensor_copy", out=V32, in_=acc, R=[bacc], W=[bV32])
                    else:
                        for tb in range(4):
                            def kdst(tsl, tb=tb):
                                P.dve("tensor_tensor", out=cin[:, 3 + tb * 512:3 + (tb + 1) * 512],
                                      in0=acc[:, tsl], in1=rn, op=ALU.mult, R=[bacc, brn], W=[bcin])
                                P.act("activation", out=kTn[:, tsl], in_=cin[:, 3 + tb * 512:3 + (tb + 1) * 512],
                                      func=AF.Copy, R=[bcin], W=[bkn])
                            l2n(tb, kdst)
                for c in range(NT):
                    csl = slice(c * 128, (c + 1) * 128)
                    ch = slice(c * 4 + h, c * 4 + h + 1)
                    pk, pkb = psum()
                    P.pe("transpose", out=pk[:, 0:128], in_=cin[:, 3 + c * 128:3 + (c + 1) * 128],
                         identity=ident, R=[bcin, bC], W=[pkb])
                    P.pe("transpose", out=pk[:, 128:256], in_=V32[:, csl], identity=ident,
                         R=[bV32, bC], W=[pkb])
                    P.dve("tensor_scalar", out=RHS[:, 0:128], in0=pk[:, 128:256], scalar1=be2d[:, ch],
                          scalar2=None, op0=ALU.mult, R=[pkb, bbe], W=[bRHS])
                    P.act("activation", out=RHS[:, 128:256], in_=pk[:, 0:128], func=AF.Copy,
                          scale=bwv[:, ch], R=[pkb, bbw], W=[bRHS])
                    P.dve("tensor_scalar", out=KDEC[:, c, :], in0=pk[:, 0:128], scalar1=kds[:, ch],
                          scalar2=None, op0=ALU.mult, R=[pkb, bkds], W=[bKD])
                    P.dve("tensor_scalar", out=TG, in0=TRI, scalar1=g2d[:, ch], scalar2=None,
                          op0=ALU.mult, R=[bC, bg2], W=[bTG])
                    pa, pab_ = psum()
                    P.pe("matmul", pa[:, 0:128], lhsT=TG, rhs=L1, start=True, stop=True, R=[bTG, bC], W=[pab_])
                    P.pe("matmul", pa[:, 128:256], lhsT=L1, rhs=TG, start=True, stop=True, R=[bTG, bC], W=[pab_])
                    P.act("activation", out=dec, in_=pa[:, 0:256], func=AF.Exp, R=[pab_], W=[bdec])
                    P.pool("tensor_tensor", out=dec[:, 0:128], in0=dec[:, 0:128], in1=L1, op=ALU.mult,
                           R=[bdec, bC], W=[bdec])
                    P.pool("tensor_tensor", out=dec[:, 128:256], in0=dec[:, 128:256], in1=TRI, op=ALU.mult,
                           R=[bdec, bC], W=[bdec])
                    pkk, pkkb = psum()
                    P.pe("matmul", pkk[:, 0:128], lhsT=kTn[:, csl], rhs=kTn[:, csl], start=True, stop=True,
                         R=[bkn], W=[pkkb])
                    P.pe("matmul", pkk[:, 128:256], lhsT=kTn[:, csl], rhs=qTn[:, csl], start=True, stop=True,
                         R=[bkn, bqn], W=[pkkb])
                    P.dve("scalar_tensor_tensor", out=Mm, in0=pkk[:, 0:128], scalar=be2d[:, ch],
                          in1=dec[:, 0:128], op0=ALU.mult, op1=ALU.mult, R=[pkkb, bbe, bdec], W=[bMm])
                    P.dve("tensor_tensor", out=QKT[:, c, :], in0=pkk[:, 128:256], in1=dec[:, 128:256],
                          op=ALU.mult, R=[pkkb, bdec], W=[bQK])
                    pm, pmb = psum()
                    P.pe("transpose", out=pm[:, 0:128], in_=Mm, identity=ident, R=[bMm, bC], W=[pmb])
                    P.act("activation", out=MT, in_=pm[:, 0:128], func=AF.Copy, R=[pmb], W=[bMT])
                    P.dve("tensor_tensor", out=Pm, in0=ident, in1=pm[:, 0:128], op=ALU.subtract,
                          R=[bC, pmb], W=[bPm])
                    Xc, bXc, XcT, bXcT = Mm, bMm, MT, bMT
                    for lvl in range(1, 7):
                        px, pxb = psum()
                        P.pe("matmul", px[:, 0:128], lhsT=XcT, rhs=Xc, start=True, stop=True,
                             R=[bXc, bXcT], W=[pxb])
                        if lvl < 6:
                            P.pe("matmul", px[:, 128:256], lhsT=Xc, rhs=XcT, start=True, stop=True,
                                 R=[bXc, bXcT], W=[pxb])
                        Xn, bXn = Xs[lvl % 2]
                        XnT, bXnT = XTs[lvl % 2]
                        P.act("activation", out=Xn, in_=px[:, 0:128], func=AF.Copy, R=[pxb], W=[bXn])
                        if lvl < 6:
                            P.dve("tensor_copy", out=XnT, in_=px[:, 128:256], R=[pxb], W=[bXnT])
                        pp, ppb = psum()
                        P.pe("matmul", pp[:, 0:128], lhsT=Xn, rhs=Pm, start=True, stop=True,
                             R=[bXn, bPm], W=[ppb])
                        P.dve("tensor_tensor", out=Pm, in0=Pm, in1=pp[:, 0:128], op=ALU.add,
                              R=[bPm, ppb], W=[bPm])
                        Xc, bXc, XcT, bXcT = Xn, bXn, XnT, bXnT
                    pu, pub = psum()
                    P.pe("matmul", pu[:, 0:128], lhsT=Pm, rhs=RHS[:, 0:128], start=True, stop=True,
                         R=[bPm, bRHS], W=[pub])
                    P.pe("matmul", pu[:, 128:256], lhsT=RHS[:, 128:256], rhs=Pm, start=True, stop=True,
                         R=[bPm, bRHS], W=[pub])
                    P.act("activation", out=Uv[:, c, :], in_=pu[:, 0:128], func=AF.Copy, R=[pub], W=[bacc])
                    P.dve("tensor_copy", out=WTt[:, c, :], in_=pu[:, 128:256], R=[pub], W=[bWT])
                Wt, bW, sl = next_g()
                load_w(Wt, bW, [(w_in[:, 3336 + h * 128:3336 + (h + 1) * 128], 0, 128)], sl)
                P.dve("memset", S_f, 0.0, W=[bSf])
                P.dve("memset", S_b, 0.0, W=[bSb])
                for c in range(NT):
                    csl = slice(c * 128, (c + 1) * 128)
                    ch = slice(c * 4 + h, c * 4 + h + 1)
                    p1, p1b = psum()
                    P.pe("matmul", p1[:, 0:128], lhsT=WTt[:, c, :], rhs=S_b, start=True, stop=True,
                         R=[bWT, bSb], W=[p1b])
                    P.pe("matmul", p1[:, 128:256], lhsT=qTn[:, csl], rhs=S_b, start=True, stop=True,
                         R=[bqn, bSb], W=[p1b])
                    P.dve("tensor_tensor", out=vnb, in0=Uv[:, c, :], in1=p1[:, 0:128], op=ALU.subtract,
                          R=[bacc, p1b], W=[bvn])
                    p2, p2b = psum()
                    P.pe("matmul", p2[:, 0:128], lhsT=QKT[:, c, :], rhs=vnb, start=True, stop=True,
                         R=[bQK, bvn], W=[p2b])
                    P.pe("matmul", p2[:, 128:256], lhsT=KDEC[:, c, :], rhs=vnb, start=True, stop=True,
                         R=[bKD, bvn], W=[p2b])
                    P.dve("scalar_tensor_tensor", out=S_f, in0=S_f, scalar=cdv[:, ch], in1=p2[:, 128:256],
                          op0=ALU.mult, op1=ALU.add, R=[bSf, bcd, p2b], W=[bSf])
                    P.act("activation", out=S_b, in_=S_f, func=AF.Copy, R=[bSf], W=[bSb])
                    P.act("activation", out=otmp, in_=p1[:, 128:256], func=AF.Copy, scale=egam[:, ch],
                          R=[p1b, beg], W=[bot])
                    P.dve("tensor_tensor", out=otmp, in0=otmp, in1=p2[:, 0:128], op=ALU.add,
                          R=[bot, p2b], W=[bot])
                    P.act("activation", out=zs, in_=otmp, func=AF.Square, accum_out=sm2[:, 0:1],
                          R=[bot], W=[bzs, bsm2])
                    rsqrt_small(sm2[:, 1:2], sm2[:, 0:1], 1.0 / 128, 1e-6, bsm2)
                    pz, pzb = proj_tm(Wt, bW, 0, 128, c)
                    P.act("activation", out=zs, in_=pz[:, 0:128], func=AF.Silu, R=[pzb], W=[bzs])
                    P.dve("scalar_tensor_tensor", out=Yv[:, c, :], in0=otmp, scalar=sm2[:, 1:2], in1=GNW,
                          op0=ALU.mult, op1=ALU.mult, R=[bot, bsm2, bGNW], W=[bV32])
                    P.pool("tensor_tensor", out=Yv[:, c, :], in0=Yv[:, c, :], in1=zs, op=ALU.mult,
                           R=[bV32, bzs], W=[bV32])
                flush_Y(4 + h, Yv, bV32)

        if "YT" in dbg_d:
            P.dma(dbg_d["YT"], YT, R=bYT, key="dbg")

        if "O" in phases:
            P.barrier()
            a_reset(A0)
            Wo, bWo = a_bf16([128, 8, D])
            Wg, bWg = a_bf16([128, 8, D])
            Wp, bWp = a_bf16([128, 2, D])
            G1, bG1 = a_f32([128, D])
            B1, bB1 = a_f32([128, D])
            junk1, bj1 = a_f32([128, D])
            st1, bst1 = a_f32([128, 8])
            WR, bWR = a_f32([128, 8, NE])
            BR, bBR = a_f32([128, NE])
            BD, bBD = a_f32([32, D])
            hT32, bh32 = a_f32([128, 8, 128])
            ptile, bpt = a_f32([128, 256])
            pT, bpT = a_bf16([128, 2, 128])
            lg, blg = a_f32([128, NE])
            msk, bmsk = a_f32([128, NE])
            mx8, bmx = a_f32([128, 16])
            gT, bgT = a_f32([32, 128])
            sig = [a_f32([128, 512]) for _ in range(2)]
            P.dma(Wo, Wd["w_out"][l].rearrange("(k p) c -> p k c", p=128), W=[bWo], key="wo", eng="pool")
            P.dma(Wg, Wd["w_ple_gate"][l].rearrange("(k p) c -> p k c", p=128), W=[bWg], key="wo", eng="pool")
            P.dma(Wp, Wd["w_ple_proj"][l].rearrange("(k p) c -> p k c", p=128), W=[bWp], key="wo", eng="pool")
            P.dma(WR, Wd["w_router"][l].rearrange("(k p) c -> p k c", p=128), W=[bWR], key="c")
            P.dma(BD, Wd["b_down"][l], W=[bBD], key="c")
            bcast_load(BR, bBR, Wd["b_router"][l])
            bcast_load(G1, bG1, Wd["ln1_g"][l])
            bcast_load(B1, bB1, Wd["ln1_b"][l])
            for t in range(NT):
                tsl = slice(t * 128, (t + 1) * 128)
                for nb in range(2):
                    nsl = slice(nb * 512, (nb + 1) * 512)
                    pt, pb = psum()
                    for k in range(8):
                        P.pe("matmul", pt[:, :], lhsT=YT[:, k, tsl], rhs=Wo[:, k, nsl], start=(k == 0),
                             stop=(k == 7), R=[bYT[k], bWo], W=[pb])
                    P.dve("scalar_tensor_tensor", out=X[:, t, nsl], in0=X[:, t, nsl], scalar=ALPHA,
                          in1=pt[:, :], op0=ALU.mult, op1=ALU.add, R=[bX[t], pb], W=[bX[t]])
                layer_norm_tile(t, G1, B1, bG1, bB1, junk1, bj1, st1, bst1)
                if "h1" in dbg_d:
                    P.dma(dbg_d["h1"][tsl, :], X[:, t, :], R=[bX[t]], key="dbg")

                def extra(k, pt, pb):
                    P.dve("tensor_copy", out=hT32[:, k:k + 4, :],
                          in_=pt[:].rearrange("p (j c) -> p j c", j=4), R=[pb], W=[bh32])
                transpose_tile(t, extra)
                pt, pb = psum()
                for k in range(8):
                    P.pe("matmul", pt[:, 0:NE], lhsT=hT32[:, k, :], rhs=WR[:, k, :], start=(k == 0),
                         stop=(k == 7), R=[bh32, bWR], W=[pb])
                P.dve("tensor_tensor", out=lg, in0=pt[:, 0:NE], in1=BR, op=ALU.add, R=[pb, bBR], W=[blg])
                P.dve("max", out=mx8[:, 0:8], in_=lg, R=[blg], W=[bmx])
                P.dve("tensor_scalar", out=msk, in0=lg, scalar1=mx8[:, 3:4], scalar2=None,
                      op0=ALU.is_ge, R=[blg, bmx], W=[bmsk])
                P.dve("tensor_scalar", out=mx8[:, 8:9], in0=mx8[:, 0:1], scalar1=-1.0, scalar2=None,
                      op0=ALU.mult, R=[bmx], W=[bmx])
                P.act("activation", out=lg, in_=lg, func=AF.Exp, bias=mx8[:, 8:9], scale=1.0,
                      R=[blg, bmx], W=[blg])
                P.dve("tensor_tensor", out=lg, in0=lg, in1=msk, op=ALU.mult, R=[blg, bmsk], W=[blg])
                P.dve("reduce_sum", out=mx8[:, 9:10], in_=lg, axis=AX.X, R=[blg], W=[bmx])
                P.dve("reciprocal", out=mx8[:, 10:11], in_=mx8[:, 9:10], R=[bmx], W=[bmx])
                P.dve("tensor_scalar", out=GATES[:, t, :], in0=lg, scalar1=mx8[:, 10:11], scalar2=None,
                      op0=ALU.mult, R=[blg, bmx], W=[bGA])
                pt, pb = psum()
                P.pe("transpose", out=pt[0:NE, 0:128], in_=GATES[:, t, :], identity=ident,
                     R=[bGA, bC], W=[pb])
                P.act("activation", out=gT, in_=pt[0:NE, 0:128], func=AF.Copy, R=[pb], W=[bgT])
                P.dma(ptile, p_d[l - layers[0], tsl, :], W=[bpt], key="pt")
                pt, pb = psum()
                for j in range(2):
                    P.pe("transpose", out=pt[:, j * 128:(j + 1) * 128], in_=ptile[:, j * 128:(j + 1) * 128],
                         identity=ident, R=[bpt, bC], W=[pb])
                P.act("activation", out=pT, in_=pt[:, 0:256].rearrange("p (j c) -> p j c", j=2),
                      func=AF.Copy, R=[pb], W=[bpT])
                for nb in range(2):
                    nsl = slice(nb * 512, (nb + 1) * 512)
                    sg_, bsg_ = sig[nb]
                    pg, pgb = psum()
                    for k in range(8):
                        P.pe("matmul", pg[:, :], lhsT=XT[:, k, tsl], rhs=Wg[:, k, nsl], start=(k == 0),
                             stop=(k == 7), R=[bXT[t], bWg], W=[pgb])
                    pp, ppb = psum()
                    for k in range(2):
                        P.pe("matmul", pp[:, :], lhsT=pT[:, k, :], rhs=Wp[:, k, nsl], start=(k == 0),
                             stop=(k == 1), R=[bpT, bWp], W=[ppb])
                    pbd, pbdb = psum()
                    P.pe("matmul", pbd[:, :], lhsT=gT, rhs=BD[:, nsl], start=True, stop=True,
                         R=[bgT, bBD], W=[pbdb])
                    P.act("activation", out=sg_, in_=pg[:, :], func=AF.Sigmoid, R=[pgb], W=[bsg_])
                    P.dve("tensor_tensor", out=sg_, in0=sg_, in1=pp[:, :], op=ALU.mult, R=[bsg_, ppb], W=[bsg_])
                    P.dve("scalar_tensor_tensor", out=X[:, t, nsl], in0=X[:, t, nsl], scalar=ALPHA,
                          in1=sg_, op0=ALU.mult, op1=ALU.add, R=[bX[t], bsg_], W=[bX[t]])
                    P.dve("tensor_tensor", out=X[:, t, nsl], in0=X[:, t, nsl], in1=pbd[:, :], op=ALU.add,
                          R=[bX[t], pbdb], W=[bX[t]])

        if "E" in phases:
            P.barrier()
            a_reset(AE)
            WG = [a_bf16([128, 8, 1024]) for _ in range(2)]
            WDn = [a_bf16([128, 4, 1024]) for _ in range(2)]
            actb = [a_bf16([128, 4, 512]) for _ in range(2)]
            gm_t = [a_f32([128, 512]) for _ in range(2)]
            sg_t = [a_f32([128, 512]) for _ in range(2)]
            um_t = [a_f32([128, 512]) for _ in range(2)]
            BGU, bBGU = a_f32([32, 2 * D])
            bguT, bbguT = a_f32([128, 16, NE])
            G2, bG2 = a_f32([128, D])
            B2, bB2 = a_f32([128, D])
            junk2, bj2 = a_f32([128, D])
            st2, bst2 = a_f32([128, 8])
            P.dma(BGU, Wd["b_gu"][l], W=[bBGU], key="c")
            bcast_load(G2, bG2, Wd["ln2_g"][l])
            bcast_load(B2, bB2, Wd["ln2_b"][l])
            pt, pb = psum()
            for c in range(16):
                P.pe("transpose", out=pt[:, c * NE:(c + 1) * NE], in_=BGU[:, c * 128:(c + 1) * 128],
                     identity=ident[0:NE, 0:NE], R=[bBGU, bC], W=[pb])
            P.dve("tensor_copy", out=bguT, in_=pt[:, :].rearrange("p (a b) -> p a b", a=16), R=[pb], W=[bbguT])
            hi = 0
            ei = 0
            for e in range(NE):
                for g in range(2):
                    wg_, bwg_ = WG[hi % 2]
                    wd_, bwd_ = WDn[hi % 2]
                    sl = hi % 2
                    hi += 1
                    wgu = Wd["w_gu"][l][e]
                    P.dma(wg_[:, :, 0:512], wgu[:, g * 512:(g + 1) * 512].rearrange("(k p) c -> p k c", p=128),
                          W=[bwg_], key=f"e{sl}", eng="pool")
                    P.dma(wg_[:, :, 512:1024], wgu[:, D + g * 512:D + (g + 1) * 512].rearrange("(k p) c -> p k c", p=128),
                          W=[bwg_], key=f"e{sl}", eng="pool")
                    P.dma(wd_, Wd["w_down"][l][e][g * 512:(g + 1) * 512, :].rearrange("(k p) c -> p k c", p=128),
                          W=[bwd_], key=f"e{sl}", eng="pool")
                    for tb in range(4):
                        a_, ba_ = actb[ei % 2]
                        for fc in range(4):
                            gm, bgm = gm_t[ei % 2]
                            sg, bsg = sg_t[ei % 2]
                            um, bum = um_t[ei % 2]
                            ei += 1
                            jg = g * 4 + fc
                            ju = 8 + g * 4 + fc
                            pg, pgb = psum()
                            for k in range(8):
                                P.pe("matmul", pg[:, :], lhsT=wg_[:, k, fc * 128:(fc + 1) * 128],
                                     rhs=XT[:, k, tb * 512:(tb + 1) * 512], start=(k == 0), stop=(k == 7),
                                     R=[bwg_] + bXT[tb * 4:tb * 4 + 4], W=[pgb])
                            pu, pub = psum()
                            for k in range(8):
                                P.pe("matmul", pu[:, :], lhsT=wg_[:, k, 512 + fc * 128:512 + (fc + 1) * 128],
                                     rhs=XT[:, k, tb * 512:(tb + 1) * 512], start=(k == 0), stop=(k == 7),
                                     R=[bwg_] + bXT[tb * 4:tb * 4 + 4], W=[pub])
                            P.dve("tensor_scalar", out=gm, in0=pg[:, :], scalar1=bguT[:, jg, e:e + 1], scalar2=7.0,
                                  op0=ALU.add, op1=ALU.min, R=[pgb, bbguT], W=[bgm])
                            P.act("activation", out=sg, in_=gm, func=AF.Sigmoid, scale=1.702, R=[bgm], W=[bsg])
                            P.dve("tensor_scalar", out=um, in0=pu[:, :], scalar1=bguT[:, ju, e:e + 1], scalar2=7.0,
                                  op0=ALU.add, op1=ALU.min, R=[pub, bbguT], W=[bum])
                            P.pool("tensor_tensor", out=sg, in0=sg, in1=gm, op=ALU.mult, R=[bsg, bgm], W=[bsg])
                            P.dve("tensor_scalar", out=um, in0=um, scalar1=-7.0, scalar2=1.0,
                                  op0=ALU.max, op1=ALU.add, R=[bum], W=[bum])
                            P.pool("tensor_tensor", out=a_[:, fc, :], in0=um, in1=sg, op=ALU.mult, R=[bum, bsg], W=[ba_])
                        for tt in range(4):
                            t = tb * 4 + tt
                            for nb in range(2):
                                nsl = slice(nb * 512, (nb + 1) * 512)
                                py, pyb = psum()
                                for fc in range(4):
                                    P.pe("matmul", py[:, :], lhsT=a_[:, fc, tt * 128:(tt + 1) * 128],
                                         rhs=wd_[:, fc, nsl], start=(fc == 0), stop=(fc == 3),
                                         R=[ba_, bwd_], W=[pyb])
                                P.dve("scalar_tensor_tensor", out=X[:, t, nsl], in0=py[:, :],
                                      scalar=GATES[:, t, e:e + 1], in1=X[:, t, nsl], op0=ALU.mult,
                                      op1=ALU.add, R=[pyb, bGA, bX[t]], W=[bX[t]])
            for t in range(NT):
                layer_norm_tile(t, G2, B2, bG2, bB2, junk2, bj2, st2, bst2)
                if l != layers[-1] or not last:
                    transpose_tile(t)
            dump_X("h2")

    for t in range(NT):
        P.dma(out_d[t * 128:(t + 1) * 128, :], X[:, t, :], R=[bX[t]], key="out")
    info = P.emit()
    return nc, info


def make_consts():
    j = np.arange(128)[:, None]
    t = np.arange(128)[None, :]
    cst = np.zeros((128, 5, 128), np.float32)
    cst[:, 0, :] = np.eye(128)
    cst[:, 1, :] = 1.0
    cst[:, 2, :] = (j <= t)
    cst[:, 3, :] = (j < t)
    cst[:, 4, :] = (j > t)
    sel = np.zeros((4, 512), np.float32)
    for h in range(4):
        sel[h, h * 128:(h + 1) * 128] = 1.0
    return cst, sel


_CACHE = {}


def run_prog(inputs, xs, layers, first, last, dbg=(), phases=("M", "S", "G", "O", "E"), trace=False, wl0=0):
    key = (tuple(layers), first, last, tuple(dbg), tuple(phases))
    if key not in _CACHE:
        _CACHE[key] = build(layers, first, last, dbg, phases)
    nc, info = _CACHE[key]
    cst, sel = make_consts()
    l0, l1 = layers[0] + wl0, layers[-1] + 1 + wl0
    names = [n for n in W_NAMES]
    in_maps = []
    import concourse.bass as _b
    declared = set(t for t in ["w_in", "m_i_bias", "m_f_bias", "m_norm_w", "sb_norm_w", "g_conv_w",
                               "g_A_log", "g_dt_bias", "g_norm_w"] if set(phases) & {"M", "S", "G"})
    if "O" in phases:
        declared |= {"w_out", "ln1_g", "ln1_b", "w_router", "b_router", "w_ple_gate", "w_ple_proj",
                     "b_down"}
    if "E" in phases:
        declared |= {"w_gu", "b_gu", "w_down", "ln2_g", "ln2_b"}
    wsl = {n: np.ascontiguousarray(np.asarray(inputs[n])[l0:l1]) for n in declared}
    for b in range(8):
        m = {"x": np.ascontiguousarray(xs[b]),
             "p": np.ascontiguousarray(np.asarray(inputs["p"])[l0:l1, b]),
             "ln0_g": np.asarray(inputs["ln0_g"]), "ln0_b": np.asarray(inputs["ln0_b"]),
             "cst": cst, "sel": sel}
        m.update(wsl)
        in_maps.append(m)
    res = run_bass_kernel_spmd(nc, in_maps, core_ids=list(range(8)), trace=trace)
    return res


FUSED = False


def kernel(**inputs):
    xs = np.asarray(inputs["x"], dtype=np.float32)
    if FUSED:
        res = run_prog(inputs, xs, list(range(DEPTH)), True, True)
        return np.stack([np.asarray(r["out"]) for r in res.results], axis=0).astype(np.float32)
    for l in range(DEPTH):
        res = run_prog(inputs, xs, [0], l == 0, True, wl0=l)
        xs = np.stack([np.asarray(r["out"]) for r in res.results], axis=0).astype(np.float32)
    return xs
```

```python
import bisect
from contextlib import ExitStack

import numpy as np
import concourse.bass as bass
import concourse.mybir as mybir
from concourse.bass_utils import run_bass_kernel_spmd

F32 = mybir.dt.float32
BF16 = mybir.dt.bfloat16
AF = mybir.ActivationFunctionType
ALU = mybir.AluOpType
AX = mybir.AxisListType

S = 2048
D = 1024
NT = 16
DEPTH = 4
D_IN = 3856
NE = 32
ALPHA = (2 * DEPTH) ** 0.25
SEM_ROT = 30000
SCHEDULE = True


class Buf:
    __slots__ = ("name", "w", "r", "excl")

    def __init__(self, name):
        self.name = name
        self.w = None
        self.r = {}
        self.excl = False


class Op:
    __slots__ = ("idx", "eng", "method", "args", "kw", "deps", "is_dma", "key",
                 "needs_inc", "seq")


class Prog:
    ENGS = ("pe", "act", "dve", "pool", "sp")

    def __init__(self, nc):
        self.nc = nc
        self.ops = []
        self.dma_keys = {}
        self.st = ExitStack()
        self.nbuf = 0
        self.fences = []

    def sb(self, name, shape, dtype):
        return self.st.enter_context(self.nc.sbuf_tensor(name, list(shape), dtype))

    def ps(self, name, shape, dtype):
        return self.st.enter_context(self.nc.psum_tensor(name, list(shape), dtype))

    def buf(self, name=None):
        self.nbuf += 1
        return Buf(name or f"b{self.nbuf}")

    def add(self, eng, method, *args, R=(), W=(), key=None, **kw):
        op = Op()
        op.idx = len(self.ops)
        op.eng = eng
        op.method = method
        op.args = args
        op.kw = kw
        op.is_dma = method == "dma_start"
        op.key = key
        op.needs_inc = False
        op.seq = None
        deps = set()
        W = list(W) + [b for b in R if b.excl and b not in W]
        for b in R:
            if b.w is not None:
                deps.add(b.w)
        for b in W:
            if b.w is not None:
                deps.add(b.w)
            for ridx in b.r.values():
                deps.add(ridx)
        for d in list(deps):
            dop = self.ops[d]
            if dop.is_dma:
                deps.add(self.dma_keys[dop.key][-1])
        op.deps = deps
        rk = ("dma:" + key) if op.is_dma else eng
        for b in R:
            b.r[rk] = op.idx
        for b in W:
            b.w = op.idx
            b.r = {}
        if op.is_dma:
            self.dma_keys.setdefault(key, []).append(op.idx)
        self.ops.append(op)
        return op

    def pe(self, m, *a, **k): return self.add("pe", m, *a, **k)
    def act(self, m, *a, **k): return self.add("act", m, *a, **k)
    def dve(self, m, *a, **k): return self.add("dve", m, *a, **k)
    def pool(self, m, *a, **k): return self.add("pool", m, *a, **k)

    def dma(self, out, in_, R=(), W=(), key=None, eng="sp"):
        return self.add(eng, "dma_start", R=R, W=W, key=key, out=out, in_=in_)

    def _last(self):
        last = {}
        for op in self.ops:
            if op.is_dma:
                last["dma:" + op.key] = op.idx
            elif op.method is not None:
                last[op.eng] = op.idx
        return set(last.values())

    def barrier(self):
        deps = self._last()
        for e in self.ENGS:
            op = self.add(e, None)
            op.deps = set(deps)
        self.fences.append(len(self.ops))

    @staticmethod
    def _dur(op):
        if op.method is None:
            return 0.0, 0.0
        if op.is_dma:
            out = op.kw["out"]
            n = 1
            for d in out.shape:
                n *= d
            return 0.1, 2.0 + n * 4 / 150e3
        out = op.kw.get("out", op.args[0] if op.args else None)
        n = 1
        for d in out.shape[1:]:
            n *= d
        if op.eng == "pe":
            lhs = op.kw.get("lhsT", op.kw.get("in_"))
            passes = 4 if lhs.dtype == F32 else 1
            d = max(n, 64) * passes / 2.0e3 + 0.03
            return d, d + 0.1
        d = n / 0.9e3 + 0.12
        return d, d + 0.25

    def schedule(self, window=32):
        ops = self.ops
        n = len(ops)
        users = [[] for _ in range(n)]
        for op in ops:
            for d in op.deps:
                users[d].append(op.idx)
        finish = [0.0] * n
        ready = [0.0] * n
        remaining = [0] * n
        scheduled = [False] * n
        free = {e: 0.0 for e in self.ENGS}
        order = []
        bounds = [0] + [f for f in self.fences if f < n] + [n]
        for si in range(len(bounds) - 1):
            lo, hi = bounds[si], bounds[si + 1]
            if lo >= hi:
                continue
            pending = {e: [] for e in self.ENGS}
            for j in range(lo, hi):
                op = ops[j]
                pending[op.eng].append(j)
                r = 0
                rt = 0.0
                for d in op.deps:
                    if scheduled[d]:
                        if finish[d] > rt:
                            rt = finish[d]
                    else:
                        r += 1
                remaining[j] = r
                ready[j] = rt
            left = hi - lo
            while left:
                best = None
                for e in self.ENGS:
                    pend = pending[e]
                    if not pend:
                        continue
                    fe = free[e]
                    lim = min(window, len(pend))
                    dma_seen = False
                    for i in range(lim):
                        j = pend[i]
                        op = ops[j]
                        if op.method is None:
                            if i == 0 and remaining[j] == 0:
                                st = max(fe, ready[j])
                                if best is None or (st, j) < (best[0], best[1]):
                                    best = (st, j, e, i)
                            break
                        if op.is_dma:
                            if dma_seen:
                                continue
                            dma_seen = True
                        if remaining[j] == 0:
                            st = max(fe, ready[j])
                            if best is None or (st, j) < (best[0], best[1]):
                                best = (st, j, e, i)
                st, j, e, i = best
                op = ops[j]
                busy, lat = self._dur(op)
                free[e] = st + busy
                finish[j] = st + lat
                scheduled[j] = True
                pending[e].pop(i)
                order.append(j)
                left -= 1
                fj = finish[j]
                for u in users[j]:
                    if lo <= u < hi:
                        remaining[u] -= 1
                        if ready[u] < fj:
                            ready[u] = fj
            m = max(free.values())
            for e in self.ENGS:
                free[e] = m
        return order

    def emit(self):
        nc = self.nc
        ops = self.ops
        fin = self.add("sp", None)
        fin.deps = self._last()
        self.fences.append(fin.idx)
        order = self.schedule() if SCHEDULE else list(range(len(ops)))
        assert sorted(order) == list(range(len(ops)))
        sched = [ops[j] for j in order]

        def skip(dop, op):
            return dop.eng == "pe" and op.eng == "pe" and not op.is_dma

        for op in ops:
            for d in op.deps:
                dop = ops[d]
                if dop.is_dma or skip(dop, op):
                    continue
                dop.needs_inc = True
        cnt = {e: 0 for e in self.ENGS}
        for op in sched:
            if op.needs_inc:
                cnt[op.eng] += 1
                op.seq = cnt[op.eng]
        sems = {}
        for e in self.ENGS:
            for ph in range(cnt[e] // SEM_ROT + 1):
                sems[f"s_{e}_{ph}"] = self.st.enter_context(nc.semaphore(f"s_{e}_{ph}"))
        for key in self.dma_keys:
            sems["d_" + key] = self.st.enter_context(nc.semaphore("d_" + key))
        per_eng = {e: [op for op in sched if op.eng == e] for e in self.ENGS}
        nw = [0]

        def emit_engine(ename, e):
            waited = {}
            for op in per_eng[ename]:
                need = {}
                for d in op.deps:
                    dop = ops[d]
                    if dop.is_dma:
                        lst = self.dma_keys[dop.key]
                        c = bisect.bisect_left(lst, op.idx)
                        sn = "d_" + dop.key
                        v = 16 * c
                    else:
                        if skip(dop, op):
                            continue
                        ph = (dop.seq - 1) // SEM_ROT
                        sn = f"s_{dop.eng}_{ph}"
                        v = (dop.seq - 1) % SEM_ROT + 1
                    if need.get(sn, 0) < v:
                        need[sn] = v
                for sn, v in need.items():
                    if waited.get(sn, 0) >= v:
                        continue
                    waited[sn] = v
                    e.wait_ge(sems[sn], v)
                    nw[0] += 1
                if op.method is None:
                    continue
                ins = getattr(e, op.method)(*op.args, **op.kw)
                if op.is_dma:
                    ins.then_inc(sems["d_" + op.key], 16)
                elif op.needs_inc:
                    ph = (op.seq - 1) // SEM_ROT
                    ins.then_inc(sems[f"s_{ename}_{ph}"], 1)

        block = self.st.enter_context(nc.Block())

        @block.tensor
        def _(e): emit_engine("pe", e)

        @block.scalar
        def _(e): emit_engine("act", e)

        @block.vector
        def _(e): emit_engine("dve", e)

        @block.gpsimd
        def _(e): emit_engine("pool", e)

        @block.sync
        def _(e): emit_engine("sp", e)

        self.st.close()
        return {"n_ops": len(ops), "n_waits": nw[0], "cnt": cnt}


ARENA_F32 = 27000

W_NAMES = ["w_in", "m_i_bias", "m_f_bias", "m_norm_w", "sb_norm_w", "g_conv_w", "g_A_log",
           "g_dt_bias", "g_norm_w", "w_out", "ln1_g", "ln1_b", "w_router", "b_router", "w_gu",
           "b_gu", "w_down", "b_down", "w_ple_gate", "w_ple_proj", "ln2_g", "ln2_b"]
W_SHAPES = {"w_in": [D, D_IN], "m_i_bias": [4], "m_f_bias": [4], "m_norm_w": [256],
            "sb_norm_w": [256], "g_conv_w": [4, 1536], "g_A_log": [4], "g_dt_bias": [4],
            "g_norm_w": [128], "w_out": [D, D], "ln1_g": [D], "ln1_b": [D],
            "w_router": [D, NE], "b_router": [NE], "w_gu": [NE, D, 2 * D], "b_gu": [NE, 2 * D],
            "w_down": [NE, D, D], "b_down": [NE, D], "w_ple_gate": [D, D],
            "w_ple_proj": [256, D], "ln2_g": [D], "ln2_b": [D]}


def build(layers, first, last, dbg=(), phases=("M", "S", "G", "O", "E")):
    nc = bass.Bass("TRN2", target_bir_lowering=False)

    def din(name, shape, dt=F32):
        return nc.dram_tensor(name, list(shape), dt, kind="ExternalInput").ap()

    x_d = din("x", [S, D])
    p_d = din("p", [len(layers), S, 256])
    ln0g_d = din("ln0_g", [D])
    ln0b_d = din("ln0_b", [D])
    need = set()
    if set(phases) & {"M", "S", "G"}:
        need |= {"w_in", "m_i_bias", "m_f_bias", "m_norm_w", "sb_norm_w", "g_conv_w", "g_A_log",
                 "g_dt_bias", "g_norm_w"}
    if "O" in phases:
        need |= {"w_out", "ln1_g", "ln1_b", "w_router", "b_router", "w_ple_gate", "w_ple_proj",
                 "b_down"}
    if "E" in phases:
        need |= {"w_gu", "b_gu", "w_down", "ln2_g", "ln2_b"}
    nl = len(layers)
    Wfull = {n: din(n, [nl] + W_SHAPES[n]) for n in W_NAMES if n in need}

    class _WL:
        def __getitem__(self, n):
            class _L:
                def __getitem__(s2, l):
                    return Wfull[n][l - layers[0]]
            return _L()
    Wd = _WL()
    cst_d = din("cst", [128, 5, 128])
    sel_d = din("sel", [4, 512])
    out_d = nc.dram_tensor("out", [S, D], F32, kind="ExternalOutput").ap()
    dbg_d = {}
    for name in dbg:
        if name in ("YT",):
            dbg_d[name] = nc.dram_tensor("dbg_" + name, [128, 8, S], BF16, kind="ExternalOutput").ap()
        else:
            dbg_d[name] = nc.dram_tensor("dbg_" + name, [S, D], F32, kind="ExternalOutput").ap()

    P = Prog(nc)
    X = P.sb("X", [128, NT, D], F32)
    XT = P.sb("XT", [128, 8, S], BF16)
    CST = P.sb("CST", [128, 5, 128], F32)
    SEL = P.sb("SEL", [4, 512], F32)
    AR = P.sb("AR", [128, ARENA_F32], F32)
    bX = [P.buf(f"X{i}") for i in range(NT)]
    bXT = [P.buf(f"XT{i}") for i in range(NT)]
    bC = P.buf("cst")
    ident = CST[:, 0, :]
    ones = CST[:, 1, :]
    TRI = CST[:, 2, :]
    UT1 = CST[:, 3, :]
    L1 = CST[:, 4, :]

    banks = [P.ps(f"bank{i}", [128, 512], F32) for i in range(8)]
    bbank = [P.buf(f"bank{i}") for i in range(8)]
    for b_ in bbank:
        b_.excl = True
    bctr = [0]

    def psum():
        i = bctr[0] % 6
        bctr[0] += 1
        return banks[i], bbank[i]

    actr = [0]

    def psacc():
        i = 6 + actr[0] % 2
        actr[0] += 1
        return banks[i], bbank[i]

    aoff = [0]

    def a_reset(off=0):
        aoff[0] = off

    def a_f32(shape, p0=0):
        n = int(np.prod(shape[1:]))
        v = AR[p0:p0 + shape[0], aoff[0]:aoff[0] + n]
        aoff[0] += n
        assert aoff[0] <= ARENA_F32, aoff[0]
        if len(shape) == 3:
            v = v.rearrange("p (a b) -> p a b", a=shape[1])
        return v, P.buf()

    def a_bf16(shape):
        n = int(np.prod(shape[1:]))
        nf = (n + 1) // 2
        v = AR[0:shape[0], aoff[0]:aoff[0] + nf].bitcast(BF16)[:, 0:n]
        aoff[0] += nf
        assert aoff[0] <= ARENA_F32, aoff[0]
        if len(shape) == 3:
            v = v.rearrange("p (a b) -> p a b", a=shape[1])
        return v, P.buf()

    def rsqrt_small(out, in_, scale, eps, b):
        P.act("activation", out=out, in_=in_, func=AF.Ln, scale=scale, bias=eps, R=[b], W=[b])
        P.act("activation", out=out, in_=out, func=AF.Exp, scale=-0.5, R=[b], W=[b])

    def layer_norm_tile(t, G, B, bG, bB, junk, bj, st, bst):
        xt = X[:, t, :]
        P.dve("reduce_sum", out=st[:, 0:1], in_=xt, axis=AX.X, R=[bX[t]], W=[bst])
        P.dve("tensor_scalar", out=st[:, 1:2], in0=st[:, 0:1], scalar1=-1.0 / D, scalar2=None,
              op0=ALU.mult, R=[bst], W=[bst])
        P.act("activation", out=junk, in_=xt, func=AF.Square, bias=st[:, 1:2], scale=1.0,
              accum_out=st[:, 2:3], R=[bX[t], bst], W=[bj, bst])
        rsqrt_small(st[:, 3:4], st[:, 2:3], 1.0 / D, 1e-5, bst)
        P.dve("tensor_scalar", out=xt, in0=xt, scalar1=st[:, 1:2], scalar2=st[:, 3:4],
              op0=ALU.add, op1=ALU.mult, R=[bX[t], bst], W=[bX[t]])
        P.pool("tensor_tensor", out=xt, in0=xt, in1=G, op=ALU.mult, R=[bX[t], bG], W=[bX[t]])
        P.dve("tensor_tensor", out=xt, in0=xt, in1=B, op=ALU.add, R=[bX[t], bB], W=[bX[t]])

    def transpose_tile(t, extra=None):
        for k in range(0, 8, 4):
            pt, pb = psum()
            for j in range(4):
                P.pe("transpose", out=pt[:, j * 128:(j + 1) * 128],
                     in_=X[:, t, (k + j) * 128:(k + j + 1) * 128], identity=ident,
                     R=[bX[t], bC], W=[pb])
            P.act("activation", out=XT[:, k:k + 4, t * 128:(t + 1) * 128],
                  in_=pt[:].rearrange("p (j c) -> p j c", j=4), func=AF.Copy,
                  R=[pb], W=[bXT[t]])
            if extra is not None:
                extra(k, pt, pb)

    wkey = [0]

    def load_w(dst, bdst, src_cols_list, slot):
        for src, c0, n in src_cols_list:
            P.dma(dst[:, :, c0:c0 + n], src.rearrange("(k p) c -> p k c", p=128),
                  W=[bdst], key=f"w{slot}", eng="pool")

    def proj_fm(Wt, bW, c0, ncols, evac, pbase=0, tbs=range(4)):
        for tb in tbs:
            pt, pb = psum()
            for k in range(8):
                P.pe("matmul", pt[pbase:pbase + ncols, :], lhsT=Wt[:, k, c0:c0 + ncols],
                     rhs=XT[:, k, tb * 512:(tb + 1) * 512], start=(k == 0), stop=(k == 7),
                     R=[bW] + bXT[tb * 4:tb * 4 + 4], W=[pb])
            evac(tb, pt[pbase:pbase + ncols, :], pb)

    def proj_tm(Wt, bW, c0, ncols, t):
        pt, pb = psum()
        for k in range(8):
            P.pe("matmul", pt[:, 0:ncols], lhsT=XT[:, k, t * 128:(t + 1) * 128],
                 rhs=Wt[:, k, c0:c0 + ncols], start=(k == 0), stop=(k == 7),
                 R=[bW, bXT[t]], W=[pb])
        return pt, pb

    def bcast_load(dst, bdst, src, key="c"):
        P.dma(dst, src.partition_broadcast(128), W=[bdst], key=key)

    def dump_X(name):
        if name in dbg_d:
            for t in range(NT):
                P.dma(dbg_d[name][t * 128:(t + 1) * 128, :], X[:, t, :], R=[bX[t]], key="dbg")

    P.dma(CST[:], cst_d, W=[bC], key="c")
    P.dma(SEL[:], sel_d, W=[bC], key="c")
    for t in range(NT):
        P.dma(X[:, t, :], x_d[t * 128:(t + 1) * 128, :], W=[bX[t]], key="x")
    a_reset()
    GATES, bGA = a_f32([128, NT, NE])
    AE = aoff[0]
    YT, _ = a_bf16([128, 8, S])
    bYT = [P.buf(f"YT{k}") for k in range(8)]
    A0 = aoff[0]
    if "YT" in dbg_d:
        for k in range(8):
            P.pool("memset", YT[:, k, :], 0.0, W=[bYT[k]])
    G_t, bG = a_f32([128, D])
    B_t, bB = a_f32([128, D])
    junk, bj = a_f32([128, D])
    st, bst = a_f32([128, 8])
    if first:
        bcast_load(G_t, bG, ln0g_d)
        bcast_load(B_t, bB, ln0b_d)
        for t in range(NT):
            layer_norm_tile(t, G_t, B_t, bG, bB, junk, bj, st, bst)
    for t in range(NT):
        transpose_tile(t)
    dump_X("h0")

    for l in layers:
        w_in = Wd["w_in"][l]
        P.barrier()
        a_reset(A0)
        qT, bq = a_bf16([64, S])
        kT, bk = a_bf16([64, S])
        vext, bv = a_bf16([128, NT, 80])
        Ytok, bY = a_f32([128, NT, 128])
        Wm = [a_bf16([128, 8, 256]) for _ in range(2)]
        wTt = [a_bf16([128, 512]) for _ in range(2)]
        oacc, boacc = a_f32([128, 4, 65])
        Gtok, bGtok = a_f32([128, NT, 4])
        emtok, bem = a_f32([128, NT, 4])
        sm, bsm = a_f32([128, 16])
        hh, bhh = a_f32([128, 64])
        osig, bos = a_f32([128, 64])
        NWm, bNWm = a_f32([128, 256])
        NWs, bNWs = a_f32([128, 256])
        gb4, bgb4 = a_f32([4, 4])
        A1 = aoff[0]
        gA, bgA = a_f32([4, S])
        gB, bgB = a_f32([4, S])
        gC, bgC = a_f32([4, S])
        MGrow, bMG = a_f32([128, S])
        DT = [a_f32([128, 512]) for _ in range(2)]
        a_reset(A1)
        e_t = [a_f32([128, 512]) for _ in range(2)]
        sp_t = [a_f32([128, 512]) for _ in range(2)]
        arg_t = [a_f32([128, 512]) for _ in range(2)]
        Suf, bSuf = a_f32([128, 512])
        bcast_load(NWm, bNWm, Wd["m_norm_w"][l])
        bcast_load(NWs, bNWs, Wd["sb_norm_w"][l])
        P.dma(gb4[:, 0:1], Wd["m_i_bias"][l].rearrange("(h o) -> h o", o=1), W=[bgb4], key="c")
        P.dma(gb4[:, 1:2], Wd["m_f_bias"][l].rearrange("(h o) -> h o", o=1), W=[bgb4], key="c")
        P.dve("tensor_scalar", out=gb4[:, 2:3], in0=gb4[:, 1:2], scalar1=-1.0, scalar2=None,
              op0=ALU.mult, R=[bgb4], W=[bgb4])
        wslot = [0]

        def next_w():
            i = wslot[0] % 2
            wslot[0] += 1
            return Wm[i][0], Wm[i][1], i

        if "M" in phases:
            Wt, bW, sl = next_w()
            load_w(Wt, bW, [(w_in[:, 1024:1032], 0, 8)], sl)

            def ev_i(tb, ps, pb):
                P.act("activation", out=gA[:, tb * 512:(tb + 1) * 512], in_=ps, func=AF.Identity,
                      bias=gb4[:, 0:1], scale=1.0, R=[pb, bgb4], W=[bgA])
            proj_fm(Wt, bW, 0, 4, ev_i)

            def ev_f(tb, ps, pb):
                P.act("activation", out=gB[:, tb * 512:(tb + 1) * 512], in_=ps, func=AF.Exp,
                      bias=gb4[:, 2:3], scale=-1.0, R=[pb, bgb4], W=[bgB])
            proj_fm(Wt, bW, 4, 4, ev_f)
            P.act("activation", out=gB, in_=gB, func=AF.Ln, bias=1.0, scale=1.0, R=[bgB], W=[bgB])
            P.dve("tensor_tensor_scan", out=gC, data0=gB, data1=gB, initial=0.0,
                  op0=ALU.add, op1=ALU.max, R=[bgB], W=[bgC])
            P.dve("tensor_tensor", out=gA, in0=gA, in1=gC, op=ALU.add, R=[bgA, bgC], W=[bgA])
            P.dve("tensor_tensor_scan", out=gB, data0=gA, data1=gA, initial=-1e30,
                  op0=ALU.max, op1=ALU.max, R=[bgA], W=[bgB])
            P.dve("tensor_tensor", out=gC, in0=gC, in1=gB, op=ALU.subtract, R=[bgC, bgB], W=[bgC])
            pt, pb = psum()
            for j in range(NT):
                P.pe("transpose", out=pt[:, j * 4:(j + 1) * 4], in_=gA[:, j * 128:(j + 1) * 128],
                     identity=ident[0:4, 0:4], R=[bgA, bC], W=[pb])
                P.pe("transpose", out=pt[:, 64 + j * 4:64 + (j + 1) * 4],
                     in_=gC[:, j * 128:(j + 1) * 128], identity=ident[0:4, 0:4],
                     R=[bgC, bC], W=[pb])
            P.dve("tensor_copy", out=Gtok, in_=pt[:, 0:64].rearrange("p (a b) -> p a b", a=NT),
                  R=[pb], W=[bGtok])
            P.act("activation", out=emtok, in_=pt[:, 64:128].rearrange("p (a b) -> p a b", a=NT),
                  func=AF.Exp, R=[pb], W=[bem])
            P.dve("memset", vext[:, :, 64:66], 1.0, W=[bv])

        def epilogue(qb, pacc, pab, h, hslot, width, NW, bNW, is_m):
            P.act("activation", out=oacc[:, :, 0:width],
                  in_=pacc[:, 0:4 * width].rearrange("p (a b) -> p a b", a=4), func=AF.Copy,
                  R=[pab], W=[boacc])
            for tt in range(4):
                t = 4 * qb + tt
                num = oacc[:, tt, 0:64]
                if is_m:
                    den = oacc[:, tt, 64:65]
                    P.dve("tensor_scalar", out=sm[:, 0:1], in0=den, scalar1=-1.0, scalar2=None,
                          op0=ALU.mult, R=[boacc], W=[bsm])
                    P.dve("tensor_tensor", out=sm[:, 1:2], in0=sm[:, 0:1], in1=den, op=ALU.max,
                          R=[bsm, boacc], W=[bsm])
                    P.dve("tensor_tensor", out=sm[:, 2:3], in0=sm[:, 1:2], in1=emtok[:, t, h:h + 1],
                          op=ALU.max, R=[bsm, bem], W=[bsm])
                    P.dve("reciprocal", out=sm[:, 3:4], in_=sm[:, 2:3], R=[bsm], W=[bsm])
                    P.dve("tensor_scalar", out=hh, in0=num, scalar1=sm[:, 3:4], scalar2=None,
                          op0=ALU.mult, R=[boacc, bsm], W=[bhh])
                    src, bsrc = hh, bhh
                else:
                    src, bsrc = num, boacc
                P.act("activation", out=osig, in_=src, func=AF.Square, accum_out=sm[:, 4:5],
                      R=[bsrc], W=[bos, bsm])
                rsqrt_small(sm[:, 5:6], sm[:, 4:5], 1.0 / 64, 1e-6, bsm)
                dst = Ytok[:, t, hslot * 64:(hslot + 1) * 64]
                P.dve("scalar_tensor_tensor", out=dst, in0=src, scalar=sm[:, 5:6],
                      in1=NW[:, h * 64:(h + 1) * 64], op0=ALU.mult, op1=ALU.mult,
                      R=[bsrc, bsm, bNW], W=[bY])
                if is_m:
                    pt, pb = proj_tm(Wcur[0], Wcur[1], 192, 64, t)
                    P.act("activation", out=osig, in_=pt[:, 0:64], func=AF.Sigmoid, R=[pb], W=[bos])
                    P.pool("tensor_tensor", out=dst, in0=dst, in1=osig, op=ALU.mult,
                           R=[bY, bos], W=[bY])

        def flush_Y(chunk, Ytok=Ytok, bY=bY):
            for t0 in range(0, NT, 4):
                pt, pb = psum()
                for j in range(4):
                    P.pe("transpose", out=pt[:, j * 128:(j + 1) * 128], in_=Ytok[:, t0 + j, :],
                         identity=ident, R=[bY, bC], W=[pb])
                P.act("activation", out=YT[:, chunk, t0 * 128:(t0 + 4) * 128], in_=pt[:],
                      func=AF.Copy, R=[pb], W=[bYT[chunk]])

        Wcur = [None, None]
        if "M" in phases:
            for h in range(4):
                Wt, bW, sl = next_w()
                Wcur[0], Wcur[1] = Wt, bW
                load_w(Wt, bW, [(w_in[:, h * 64:(h + 1) * 64], 0, 64),
                                (w_in[:, 256 + h * 64:256 + (h + 1) * 64], 64, 64),
                                (w_in[:, 512 + h * 64:512 + (h + 1) * 64], 128, 64),
                                (w_in[:, 768 + h * 64:768 + (h + 1) * 64], 192, 64)], sl)

                def ev_q(tb, ps, pb):
                    P.act("activation", out=qT[:, tb * 512:(tb + 1) * 512], in_=ps, func=AF.Copy,
                          R=[pb], W=[bq])
                proj_fm(Wt, bW, 0, 64, ev_q)

                def ev_k(tb, ps, pb):
                    P.act("activation", out=kT[:, tb * 512:(tb + 1) * 512], in_=ps, func=AF.Copy,
                          scale=0.125, R=[pb], W=[bk])
                proj_fm(Wt, bW, 64, 64, ev_k)
                for t in range(NT):
                    pt, pb = proj_tm(Wt, bW, 128, 64, t)
                    P.dve("tensor_copy", out=vext[:, t, 0:64], in_=pt[:, 0:64], R=[pb], W=[bv])
                for tb in range(4):
                    pt, pb = psum()
                    P.pe("matmul", pt[:, :], lhsT=SEL[0:4, h * 128:(h + 1) * 128],
                         rhs=gB[:, tb * 512:(tb + 1) * 512], start=True, stop=True,
                         R=[bC, bgB], W=[pb])
                    P.dve("tensor_copy", out=MGrow[:, tb * 512:(tb + 1) * 512], in_=pt[:, :],
                          R=[pb], W=[bMG])
                pi = 0
                for qb in range(4):
                    pacc, pab = psacc()
                    for kb in range(4 * qb + 4):
                        r = kb - 4 * qb
                        c0 = max(r, 0) * 128
                        n = 512 - c0
                        q0 = qb * 512 + c0
                        pz, pzb = psum()
                        P.pe("matmul", pz[:, 0:n], lhsT=kT[:, kb * 128:(kb + 1) * 128],
                             rhs=qT[:, q0:q0 + n], start=True, stop=True, R=[bk, bq], W=[pzb])
                        dt_, bdt = DT[pi % 2]
                        w_, bw_ = wTt[pi % 2]
                        pi += 1
                        P.act("activation", out=dt_[:, 0:n], in_=MGrow[:, q0:q0 + n], func=AF.Exp,
                              scale=-1.0, bias=Gtok[:, kb, h:h + 1], R=[bMG, bGtok], W=[bdt])
                        if r >= 0:
                            P.pool("tensor_tensor", out=dt_[:, 0:128], in0=dt_[:, 0:128], in1=TRI,
                                   op=ALU.mult, R=[bdt, bC], W=[bdt])
                        P.dve("tensor_tensor", out=w_[:, 0:n], in0=pz[:, 0:n], in1=dt_[:, 0:n],
                              op=ALU.mult, R=[pzb, bdt], W=[bw_])
                        for tt in range(max(r, 0), 4):
                            cc = (tt - max(r, 0)) * 128
                            P.pe("matmul", pacc[:, tt * 65:(tt + 1) * 65], lhsT=w_[:, cc:cc + 128],
                                 rhs=vext[:, kb, 0:65], start=(kb == 0 and tt == 0), stop=(kb == 4 * qb + 3 and tt == 3),
                                 R=[bw_, bv], W=[pab])
                    epilogue(qb, pacc, pab, h, h % 2, 65, NWm, bNWm, True)
                if h % 2 == 1:
                    flush_Y(h // 2)

        if "S" in phases:
            P.barrier()
            for h in range(4):
                Wt, bW, sl = next_w()
                load_w(Wt, bW, [(w_in[:, 1032 + h * 64:1032 + (h + 1) * 64], 0, 64),
                                (w_in[:, 1288 + h * 64:1288 + (h + 1) * 64], 64, 64),
                                (w_in[:, 1544 + h * 64:1544 + (h + 1) * 64], 128, 64)], sl)

                def ev_q(tb, ps, pb):
                    P.act("activation", out=qT[:, tb * 512:(tb + 1) * 512], in_=ps, func=AF.Copy,
                          scale=0.125, R=[pb], W=[bq])
                proj_fm(Wt, bW, 0, 64, ev_q)

                def ev_k(tb, ps, pb):
                    P.act("activation", out=kT[:, tb * 512:(tb + 1) * 512], in_=ps, func=AF.Copy,
                          R=[pb], W=[bk])
                proj_fm(Wt, bW, 64, 64, ev_k)
                for t in range(NT):
                    pt, pb = proj_tm(Wt, bW, 128, 64, t)
                    P.dve("tensor_copy", out=vext[:, t, 0:64], in_=pt[:, 0:64], R=[pb], W=[bv])
                pi = 0
                for qb in range(4):
                    pacc, pab = psacc()
                    P.pool("memset", Suf, 0.0, W=[bSuf])
                    for kb in range(4 * qb + 3, -1, -1):
                        r = kb - 4 * qb
                        c0 = max(r, 0) * 128
                        n = 512 - c0
                        q0 = qb * 512 + c0
                        e_, be_ = e_t[pi % 2]
                        s_, bs_ = sp_t[pi % 2]
                        a_, ba_ = arg_t[pi % 2]
                        w_, bw_ = wTt[pi % 2]
                        pi += 1
                        pz, pzb = psum()
                        P.pe("matmul", pz[:, 0:n], lhsT=kT[:, kb * 128:(kb + 1) * 128],
                             rhs=qT[:, q0:q0 + n], start=True, stop=True, R=[bk, bq], W=[pzb])
                        P.act("activation", out=e_[:, 0:n], in_=pz[:, 0:n], func=AF.Exp,
                              R=[pzb], W=[be_])
                        P.act("activation", out=s_[:, 0:n], in_=e_[:, 0:n], func=AF.Ln, bias=1.0,
                              scale=1.0, R=[be_], W=[bs_])
                        if r >= 0:
                            P.pool("tensor_tensor", out=s_[:, 0:128], in0=s_[:, 0:128], in1=UT1,
                                   op=ALU.mult, R=[bs_, bC], W=[bs_])
                        p2, p2b = psum()
                        P.pe("matmul", p2[:, 0:n], lhsT=L1, rhs=s_[:, 0:n], start=True, stop=True,
                             R=[bC, bs_], W=[p2b])
                        p3, p3b = psum()
                        P.pe("matmul", p3[:, 0:n], lhsT=ones, rhs=s_[:, 0:n], start=True, stop=True,
                             R=[bC, bs_], W=[p3b])
                        P.dve("tensor_tensor", out=a_[:, 0:n], in0=pz[:, 0:n], in1=s_[:, 0:n],
                              op=ALU.subtract, R=[pzb, bs_], W=[ba_])
                        P.dve("tensor_tensor", out=a_[:, 0:n], in0=a_[:, 0:n], in1=p2[:, 0:n],
                              op=ALU.subtract, R=[ba_, p2b], W=[ba_])
                        P.pool("tensor_tensor", out=a_[:, 0:n], in0=a_[:, 0:n], in1=Suf[:, c0:512],
                               op=ALU.subtract, R=[ba_, bSuf], W=[ba_])
                        P.act("activation", out=w_[:, 0:n], in_=a_[:, 0:n], func=AF.Exp,
                              R=[ba_], W=[bw_])
                        if r >= 0:
                            P.pool("tensor_tensor", out=w_[:, 0:128], in0=w_[:, 0:128], in1=UT1,
                                   op=ALU.mult, R=[bw_, bC], W=[bw_])
                        P.dve("tensor_tensor", out=Suf[:, c0:512], in0=Suf[:, c0:512],
                              in1=p3[:, 0:n], op=ALU.add, R=[bSuf, p3b], W=[bSuf])
                        for tt in range(max(r, 0), 4):
                            cc = (tt - max(r, 0)) * 128
                            P.pe("matmul", pacc[:, tt * 64:(tt + 1) * 64], lhsT=w_[:, cc:cc + 128],
                                 rhs=vext[:, kb, 0:64], start=(kb == 4 * qb + 3 and tt == 3), stop=(kb == 0 and tt == 3),
                                 R=[bw_, bv], W=[pab])
                    epilogue(qb, pacc, pab, h, h % 2, 64, NWs, bNWs, False)
                if h % 2 == 1:
                    flush_Y(2 + h // 2)


        if "G" in phases:
            P.barrier()
            a_reset(A0)
            cin, bcin = a_f32([128, S + 4])
            acc, bacc = a_f32([128, S])
            V32, bV32 = a_f32([128, S])
            Uv = acc.rearrange("p (a b) -> p a b", a=NT)
            Yv = V32.rearrange("p (a b) -> p a b", a=NT)
            qTn, bqn = a_bf16([128, S])
            kTn, bkn = a_bf16([128, S])
            KDEC, bKD = a_bf16([128, NT, 128])
            QKT, bQK = a_bf16([128, NT, 128])
            WTt, bWT = a_bf16([128, NT, 128])
            Wgs = [a_bf16([128, 8, 128]) for _ in range(3)]
            sqb, bsqb = a_f32([128, 512])
            rn, brn = a_f32([128, 512])
            g2d, bg2 = a_f32([128, 64])
            be2d, bbe = a_f32([128, 64])
            gam, bgam = a_f32([128, 64])
            glast, bgl = a_f32([128, 64])
            egam, beg = a_f32([128, 64])
            kds, bkds = a_f32([128, 64])
            cdv, bcd = a_f32([128, 64])
            bwv, bbw = a_f32([128, 64])
            g3 = g2d.rearrange("p (a b) -> p a b", a=NT)
            be3 = be2d.rearrange("p (a b) -> p a b", a=NT)
            DTB, bDTB = a_f32([128, 4])
            NEA, bNEA = a_f32([128, 4])
            t4, bt4 = a_f32([128, 4])
            convw, bcw = a_f32([128, 12, 4])
            GNW, bGNW = a_f32([128, 128])
            TG, bTG = a_f32([128, 128])
            dec, bdec = a_f32([128, 256])
            Mm, bMm = a_f32([128, 128])
            MT, bMT = a_f32([128, 128])
            Pm, bPm = a_f32([128, 128])
            Xs = [a_f32([128, 128]) for _ in range(2)]
            XTs = [a_f32([128, 128]) for _ in range(2)]
            RHS, bRHS = a_f32([128, 256])
            S_f, bSf = a_f32([128, 128])
            S_b, bSb = a_bf16([128, 128])
            vnb, bvn = a_bf16([128, 128])
            otmp, bot = a_f32([128, 128])
            zs, bzs = a_f32([128, 128])
            sm2, bsm2 = a_f32([128, 8])
            gslot = [0]

            def next_g():
                i = gslot[0] % 3
                gslot[0] += 1
                return Wgs[i][0], Wgs[i][1], 4 + i

            bcast_load(DTB, bDTB, Wd["g_dt_bias"][l])
            bcast_load(NEA, bNEA, Wd["g_A_log"][l])
            bcast_load(GNW, bGNW, Wd["g_norm_w"][l])
            P.act("activation", out=NEA, in_=NEA, func=AF.Exp, R=[bNEA], W=[bNEA])
            P.dve("tensor_scalar", out=NEA, in0=NEA, scalar1=-1.0, scalar2=None, op0=ALU.mult,
                  R=[bNEA], W=[bNEA])
            P.dma(acc[0:4, 0:1536], Wd["g_conv_w"][l], W=[bacc], key="c")
            pt, pb = psum()
            for c in range(12):
                P.pe("transpose", out=pt[:, c * 4:(c + 1) * 4], in_=acc[0:4, c * 128:(c + 1) * 128],
                     identity=ident[0:4, 0:4], R=[bacc, bC], W=[pb])
            P.dve("tensor_copy", out=convw, in_=pt[:, 0:48].rearrange("p (a b) -> p a b", a=12),
                  R=[pb], W=[bcw])
            P.dve("memset", cin[:, 0:3], 0.0, W=[bcin])
            Wt, bW, sl = next_g()
            load_w(Wt, bW, [(w_in[:, 3848:3856], 0, 8)], sl)
            for t in range(NT):
                pt, pb = proj_tm(Wt, bW, 0, 8, t)
                P.dve("tensor_tensor", out=t4, in0=pt[:, 0:4], in1=DTB, op=ALU.add, R=[pb, bDTB], W=[bt4])
                P.act("activation", out=t4, in_=t4, func=AF.Exp, R=[bt4], W=[bt4])
                P.act("activation", out=t4, in_=t4, func=AF.Ln, bias=1.0, scale=1.0, R=[bt4], W=[bt4])
                P.dve("tensor_tensor", out=g3[:, t, :], in0=t4, in1=NEA, op=ALU.mult, R=[bt4, bNEA], W=[bg2])
                P.act("activation", out=be3[:, t, :], in_=pt[:, 4:8], func=AF.Sigmoid, R=[pb], W=[bbe])
            pt, pb = psum()
            P.pe("matmul", pt[:, 0:64], lhsT=TRI, rhs=g2d, start=True, stop=True, R=[bC, bg2], W=[pb])
            P.pe("matmul", pt[:, 64:128], lhsT=ones, rhs=g2d, start=True, stop=True, R=[bC, bg2], W=[pb])
            P.dve("tensor_copy", out=gam, in_=pt[:, 0:64], R=[pb], W=[bgam])
            P.dve("tensor_copy", out=glast, in_=pt[:, 64:128], R=[pb], W=[bgl])
            P.act("activation", out=egam, in_=gam, func=AF.Exp, R=[bgam], W=[beg])
            P.act("activation", out=cdv, in_=glast, func=AF.Exp, R=[bgl], W=[bcd])
            P.dve("tensor_tensor", out=kds, in0=glast, in1=gam, op=ALU.subtract, R=[bgl, bgam], W=[bkds])
            P.act("activation", out=kds, in_=kds, func=AF.Exp, R=[bkds], W=[bkds])
            P.dve("tensor_tensor", out=bwv, in0=be2d, in1=egam, op=ALU.mult, R=[bbe, beg], W=[bbw])

            def l2n(tb, dst_fn):
                tsl = slice(tb * 512, (tb + 1) * 512)
                P.pool("tensor_tensor", out=sqb, in0=acc[:, tsl], in1=acc[:, tsl], op=ALU.mult,
                       R=[bacc], W=[bsqb])
                pt, pb = psum()
                P.pe("matmul", pt[:, :], lhsT=ones, rhs=sqb, start=True, stop=True, R=[bC, bsqb], W=[pb])
                P.act("activation", out=rn, in_=pt[:, :], func=AF.Ln, bias=1e-6, scale=1.0, R=[pb], W=[brn])
                P.act("activation", out=rn, in_=rn, func=AF.Exp, scale=-0.5, R=[brn], W=[brn])
                dst_fn(tsl)

            for h in range(4):
                for nm, col0, cc in (("q", 1800 + h * 128, h), ("v", 2824 + h * 128, 8 + h),
                                     ("k", 2312 + h * 128, 4 + h)):
                    Wt, bW, sl = next_g()
                    load_w(Wt, bW, [(w_in[:, col0:col0 + 128], 0, 128)], sl)

                    def ev_c(tb, ps, pb):
                        P.act("activation", out=cin[:, 3 + tb * 512:3 + (tb + 1) * 512], in_=ps,
                              func=AF.Copy, R=[pb], W=[bcin])
                    proj_fm(Wt, bW, 0, 128, ev_c)
                    P.dve("tensor_scalar", out=acc, in0=cin[:, 3:3 + S], scalar1=convw[:, cc, 3:4],
                          scalar2=None, op0=ALU.mult, R=[bcin, bcw], W=[bacc])
                    for j in (2, 1, 0):
                        P.dve("scalar_tensor_tensor", out=acc, in0=cin[:, j:j + S],
                              scalar=convw[:, cc, j:j + 1], in1=acc, op0=ALU.mult, op1=ALU.add,
                              R=[bcin, bcw, bacc], W=[bacc])
                    P.act("activation", out=acc, in_=acc, func=AF.Silu, R=[bacc], W=[bacc])
                    if nm == "q":
                        for tb in range(4):
                            l2n(tb, lambda tsl: P.dve(
                                "scalar_tensor_tensor", out=qTn[:, tsl], in0=acc[:, tsl],
                                scalar=128 ** -0.5, in1=rn, op0=ALU.mult, op1=ALU.mult,
                                R=[bacc, brn], W=[bqn]))
                    elif nm == "v":
                        P.pool("tensor_copy", out=V32, in_=acc, R=[bacc], W=[bV32])
                    else:
                        for tb in range(4):
                            def kdst(tsl, tb=tb):
                                P.dve("tensor_tensor", out=cin[:, 3 + tb * 512:3 + (tb + 1) * 512],
                                      in0=acc[:, tsl], in1=rn, op=ALU.mult, R=[bacc, brn], W=[bcin])
                                P.act("activation", out=kTn[:, tsl], in_=cin[:, 3 + tb * 512:3 + (tb + 1) * 512],
                                      func=AF.Copy, R=[bcin], W=[bkn])
                            l2n(tb, kdst)
                for c in range(NT):
                    csl = slice(c * 128, (c + 1) * 128)
                    ch = slice(c * 4 + h, c * 4 + h + 1)
                    pk, pkb = psum()
                    P.pe("transpose", out=pk[:, 0:128], in_=cin[:, 3 + c * 128:3 + (c + 1) * 128],
                         identity=ident, R=[bcin, bC], W=[pkb])
                    P.pe("transpose", out=pk[:, 128:256], in_=V32[:, csl], identity=ident,
                         R=[bV32, bC], W=[pkb])
                    P.dve("tensor_scalar", out=RHS[:, 0:128], in0=pk[:, 128:256], scalar1=be2d[:, ch],
                          scalar2=None, op0=ALU.mult, R=[pkb, bbe], W=[bRHS])
                    P.act("activation", out=RHS[:, 128:256], in_=pk[:, 0:128], func=AF.Copy,
                          scale=bwv[:, ch], R=[pkb, bbw], W=[bRHS])
                    P.dve("tensor_scalar", out=KDEC[:, c, :], in0=pk[:, 0:128], scalar1=kds[:, ch],
                          scalar2=None, op0=ALU.mult, R=[pkb, bkds], W=[bKD])
                    P.dve("tensor_scalar", out=TG, in0=TRI, scalar1=g2d[:, ch], scalar2=None,
                          op0=ALU.mult, R=[bC, bg2], W=[bTG])
                    pa, pab_ = psum()
                    P.pe("matmul", pa[:, 0:128], lhsT=TG, rhs=L1, start=True, stop=True, R=[bTG, bC], W=[pab_])
                    P.pe("matmul", pa[:, 128:256], lhsT=L1, rhs=TG, start=True, stop=True, R=[bTG, bC], W=[pab_])
                    P.act("activation", out=dec, in_=pa[:, 0:256], func=AF.Exp, R=[pab_], W=[bdec])
                    P.pool("tensor_tensor", out=dec[:, 0:128], in0=dec[:, 0:128], in1=L1, op=ALU.mult,
                           R=[bdec, bC], W=[bdec])
                    P.pool("tensor_tensor", out=dec[:, 128:256], in0=dec[:, 128:256], in1=TRI, op=ALU.mult,
                           R=[bdec, bC], W=[bdec])
                    pkk, pkkb = psum()
                    P.pe("matmul", pkk[:, 0:128], lhsT=kTn[:, csl], rhs=kTn[:, csl], start=True, stop=True,
                         R=[bkn], W=[pkkb])
                    P.pe("matmul", pkk[:, 128:256], lhsT=kTn[:, csl], rhs=qTn[:, csl], start=True, stop=True,
                         R=[bkn, bqn], W=[pkkb])
                    P.dve("scalar_tensor_tensor", out=Mm, in0=pkk[:, 0:128], scalar=be2d[:, ch],
                          in1=dec[:, 0:128], op0=ALU.mult, op1=ALU.mult, R=[pkkb, bbe, bdec], W=[bMm])
                    P.dve("tensor_tensor", out=QKT[:, c, :], in0=pkk[:, 128:256], in1=dec[:, 128:256],
                          op=ALU.mult, R=[pkkb, bdec], W=[bQK])
                    pm, pmb = psum()
                    P.pe("transpose", out=pm[:, 0:128], in_=Mm, identity=ident, R=[bMm, bC], W=[pmb])
                    P.act("activation", out=MT, in_=pm[:, 0:128], func=AF.Copy, R=[pmb], W=[bMT])
                    P.dve("tensor_tensor", out=Pm, in0=ident, in1=pm[:, 0:128], op=ALU.subtract,
                          R=[bC, pmb], W=[bPm])
                    Xc, bXc, XcT, bXcT = Mm, bMm, MT, bMT
                    for lvl in range(1, 7):
                        px, pxb = psum()
                        P.pe("matmul", px[:, 0:128], lhsT=XcT, rhs=Xc, start=True, stop=True,
                             R=[bXc, bXcT], W=[pxb])
                        if lvl < 6:
                            P.pe("matmul", px[:, 128:256], lhsT=Xc, rhs=XcT, start=True, stop=True,
                                 R=[bXc, bXcT], W=[pxb])
                        Xn, bXn = Xs[lvl % 2]
                        XnT, bXnT = XTs[lvl % 2]
                        P.act("activation", out=Xn, in_=px[:, 0:128], func=AF.Copy, R=[pxb], W=[bXn])
                        if lvl < 6:
                            P.dve("tensor_copy", out=XnT, in_=px[:, 128:256], R=[pxb], W=[bXnT])
                        pp, ppb = psum()
                        P.pe("matmul", pp[:, 0:128], lhsT=Xn, rhs=Pm, start=True, stop=True,
                             R=[bXn, bPm], W=[ppb])
                        P.dve("tensor_tensor", out=Pm, in0=Pm, in1=pp[:, 0:128], op=ALU.add,
                              R=[bPm, ppb], W=[bPm])
                        Xc, bXc, XcT, bXcT = Xn, bXn, XnT, bXnT
                    pu, pub = psum()
                    P.pe("matmul", pu[:, 0:128], lhsT=Pm, rhs=RHS[:, 0:128], start=True, stop=True,
                         R=[bPm, bRHS], W=[pub])
                    P.pe("matmul", pu[:, 128:256], lhsT=RHS[:, 128:256], rhs=Pm, start=True, stop=True,
                         R=[bPm, bRHS], W=[pub])
                    P.act("activation", out=Uv[:, c, :], in_=pu[:, 0:128], func=AF.Copy, R=[pub], W=[bacc])
                    P.dve("tensor_copy", out=WTt[:, c, :], in_=pu[:, 128:256], R=[pub], W=[bWT])
                Wt, bW, sl = next_g()
                load_w(Wt, bW, [(w_in[:, 3336 + h * 128:3336 + (h + 1) * 128], 0, 128)], sl)
                P.dve("memset", S_f, 0.0, W=[bSf])
                P.dve("memset", S_b, 0.0, W=[bSb])
                for c in range(NT):
                    csl = slice(c * 128, (c + 1) * 128)
                    ch = slice(c * 4 + h, c * 4 + h + 1)
                    p1, p1b = psum()
                    P.pe("matmul", p1[:, 0:128], lhsT=WTt[:, c, :], rhs=S_b, start=True, stop=True,
                         R=[bWT, bSb], W=[p1b])
                    P.pe("matmul", p1[:, 128:256], lhsT=qTn[:, csl], rhs=S_b, start=True, stop=True,
                         R=[bqn, bSb], W=[p1b])
                    P.dve("tensor_tensor", out=vnb, in0=Uv[:, c, :], in1=p1[:, 0:128], op=ALU.subtract,
                          R=[bacc, p1b], W=[bvn])
                    p2, p2b = psum()
                    P.pe("matmul", p2[:, 0:128], lhsT=QKT[:, c, :], rhs=vnb, start=True, stop=True,
                         R=[bQK, bvn], W=[p2b])
                    P.pe("matmul", p2[:, 128:256], lhsT=KDEC[:, c, :], rhs=vnb, start=True, stop=True,
                         R=[bKD, bvn], W=[p2b])
                    P.dve("scalar_tensor_tensor", out=S_f, in0=S_f, scalar=cdv[:, ch], in1=p2[:, 128:256],
                          op0=ALU.mult, op1=ALU.add, R=[bSf, bcd, p2b], W=[bSf])
                    P.act("activation", out=S_b, in_=S_f, func=AF.Copy, R=[bSf], W=[bSb])
                    P.act("activation", out=otmp, in_=p1[:, 128:256], func=AF.Copy, scale=egam[:, ch],
                          R=[p1b, beg], W=[bot])
                    P.dve("tensor_tensor", out=otmp, in0=otmp, in1=p2[:, 0:128], op=ALU.add,
                          R=[bot, p2b], W=[bot])
                    P.act("activation", out=zs, in_=otmp, func=AF.Square, accum_out=sm2[:, 0:1],
                          R=[bot], W=[bzs, bsm2])
                    rsqrt_small(sm2[:, 1:2], sm2[:, 0:1], 1.0 / 128, 1e-6, bsm2)
                    pz, pzb = proj_tm(Wt, bW, 0, 128, c)
                    P.act("activation", out=zs, in_=pz[:, 0:128], func=AF.Silu, R=[pzb], W=[bzs])
                    P.dve("scalar_tensor_tensor", out=Yv[:, c, :], in0=otmp, scalar=sm2[:, 1:2], in1=GNW,
                          op0=ALU.mult, op1=ALU.mult, R=[bot, bsm2, bGNW], W=[bV32])
                    P.pool("tensor_tensor", out=Yv[:, c, :], in0=Yv[:, c, :], in1=zs, op=ALU.mult,
                           R=[bV32, bzs], W=[bV32])
                flush_Y(4 + h, Yv, bV32)

        if "YT" in dbg_d:
            P.dma(dbg_d["YT"], YT, R=bYT, key="dbg")

        if "O" in phases:
            P.barrier()
            a_reset(A0)
            Wo, bWo = a_bf16([128, 8, D])
            Wg, bWg = a_bf16([128, 8, D])
            Wp, bWp = a_bf16([128, 2, D])
            G1, bG1 = a_f32([128, D])
            B1, bB1 = a_f32([128, D])
            junk1, bj1 = a_f32([128, D])
            st1, bst1 = a_f32([128, 8])
            WR, bWR = a_f32([128, 8, NE])
            BR, bBR = a_f32([128, NE])
            BD, bBD = a_f32([32, D])
            hT32, bh32 = a_f32([128, 8, 128])
            ptile, bpt = a_f32([128, 256])
            pT, bpT = a_bf16([128, 2, 128])
            lg, blg = a_f32([128, NE])
            msk, bmsk = a_f32([128, NE])
            mx8, bmx = a_f32([128, 16])
            gT, bgT = a_f32([32, 128])
            sig = [a_f32([128, 512]) for _ in range(2)]
            P.dma(Wo, Wd["w_out"][l].rearrange("(k p) c -> p k c", p=128), W=[bWo], key="wo", eng="pool")
            P.dma(Wg, Wd["w_ple_gate"][l].rearrange("(k p) c -> p k c", p=128), W=[bWg], key="wo", eng="pool")
            P.dma(Wp, Wd["w_ple_proj"][l].rearrange("(k p) c -> p k c", p=128), W=[bWp], key="wo", eng="pool")
            P.dma(WR, Wd["w_router"][l].rearrange("(k p) c -> p k c", p=128), W=[bWR], key="c")
            P.dma(BD, Wd["b_down"][l], W=[bBD], key="c")
            bcast_load(BR, bBR, Wd["b_router"][l])
            bcast_load(G1, bG1, Wd["ln1_g"][l])
            bcast_load(B1, bB1, Wd["ln1_b"][l])
            for t in range(NT):
                tsl = slice(t * 128, (t + 1) * 128)
                for nb in range(2):
                    nsl = slice(nb * 512, (nb + 1) * 512)
                    pt, pb = psum()
                    for k in range(8):
                        P.pe("matmul", pt[:, :], lhsT=YT[:, k, tsl], rhs=Wo[:, k, nsl], start=(k == 0),
                             stop=(k == 7), R=[bYT[k], bWo], W=[pb])
                    P.dve("scalar_tensor_tensor", out=X[:, t, nsl], in0=X[:, t, nsl], scalar=ALPHA,
                          in1=pt[:, :], op0=ALU.mult, op1=ALU.add, R=[bX[t], pb], W=[bX[t]])
                layer_norm_tile(t, G1, B1, bG1, bB1, junk1, bj1, st1, bst1)
                if "h1" in dbg_d:
                    P.dma(dbg_d["h1"][tsl, :], X[:, t, :], R=[bX[t]], key="dbg")

                def extra(k, pt, pb):
                    P.dve("tensor_copy", out=hT32[:, k:k + 4, :],
                          in_=pt[:].rearrange("p (j c) -> p j c", j=4), R=[pb], W=[bh32])
                transpose_tile(t, extra)
                pt, pb = psum()
                for k in range(8):
                    P.pe("matmul", pt[:, 0:NE], lhsT=hT32[:, k, :], rhs=WR[:, k, :], start=(k == 0),
                         stop=(k == 7), R=[bh32, bWR], W=[pb])
                P.dve("tensor_tensor", out=lg, in0=pt[:, 0:NE], in1=BR, op=ALU.add, R=[pb, bBR], W=[blg])
                P.dve("max", out=mx8[:, 0:8], in_=lg, R=[blg], W=[bmx])
                P.dve("tensor_scalar", out=msk, in0=lg, scalar1=mx8[:, 3:4], scalar2=None,
                      op0=ALU.is_ge, R=[blg, bmx], W=[bmsk])
                P.dve("tensor_scalar", out=mx8[:, 8:9], in0=mx8[:, 0:1], scalar1=-1.0, scalar2=None,
                      op0=ALU.mult, R=[bmx], W=[bmx])
                P.act("activation", out=lg, in_=lg, func=AF.Exp, bias=mx8[:, 8:9], scale=1.0,
                      R=[blg, bmx], W=[blg])
                P.dve("tensor_tensor", out=lg, in0=lg, in1=msk, op=ALU.mult, R=[blg, bmsk], W=[blg])
                P.dve("reduce_sum", out=mx8[:, 9:10], in_=lg, axis=AX.X, R=[blg], W=[bmx])
                P.dve("reciprocal", out=mx8[:, 10:11], in_=mx8[:, 9:10], R=[bmx], W=[bmx])
                P.dve("tensor_scalar", out=GATES[:, t, :], in0=lg, scalar1=mx8[:, 10:11], scalar2=None,
                      op0=ALU.mult, R=[blg, bmx], W=[bGA])
                pt, pb = psum()
                P.pe("transpose", out=pt[0:NE, 0:128], in_=GATES[:, t, :], identity=ident,
                     R=[bGA, bC], W=[pb])
                P.act("activation", out=gT, in_=pt[0:NE, 0:128], func=AF.Copy, R=[pb], W=[bgT])
                P.dma(ptile, p_d[l - layers[0], tsl, :], W=[bpt], key="pt")
                pt, pb = psum()
                for j in range(2):
                    P.pe("transpose", out=pt[:, j * 128:(j + 1) * 128], in_=ptile[:, j * 128:(j + 1) * 128],
                         identity=ident, R=[bpt, bC], W=[pb])
                P.act("activation", out=pT, in_=pt[:, 0:256].rearrange("p (j c) -> p j c", j=2),
                      func=AF.Copy, R=[pb], W=[bpT])
                for nb in range(2):
                    nsl = slice(nb * 512, (nb + 1) * 512)
                    sg_, bsg_ = sig[nb]
                    pg, pgb = psum()
                    for k in range(8):
                        P.pe("matmul", pg[:, :], lhsT=XT[:, k, tsl], rhs=Wg[:, k, nsl], start=(k == 0),
                             stop=(k == 7), R=[bXT[t], bWg], W=[pgb])
                    pp, ppb = psum()
                    for k in range(2):
                        P.pe("matmul", pp[:, :], lhsT=pT[:, k, :], rhs=Wp[:, k, nsl], start=(k == 0),
                             stop=(k == 1), R=[bpT, bWp], W=[ppb])
                    pbd, pbdb = psum()
                    P.pe("matmul", pbd[:, :], lhsT=gT, rhs=BD[:, nsl], start=True, stop=True,
                         R=[bgT, bBD], W=[pbdb])
                    P.act("activation", out=sg_, in_=pg[:, :], func=AF.Sigmoid, R=[pgb], W=[bsg_])
                    P.dve("tensor_tensor", out=sg_, in0=sg_, in1=pp[:, :], op=ALU.mult, R=[bsg_, ppb], W=[bsg_])
                    P.dve("scalar_tensor_tensor", out=X[:, t, nsl], in0=X[:, t, nsl], scalar=ALPHA,
                          in1=sg_, op0=ALU.mult, op1=ALU.add, R=[bX[t], bsg_], W=[bX[t]])
                    P.dve("tensor_tensor", out=X[:, t, nsl], in0=X[:, t, nsl], in1=pbd[:, :], op=ALU.add,
                          R=[bX[t], pbdb], W=[bX[t]])

        if "E" in phases:
            P.barrier()
            a_reset(AE)
            WG = [a_bf16([128, 8, 1024]) for _ in range(2)]
            WDn = [a_bf16([128, 4, 1024]) for _ in range(2)]
            actb = [a_bf16([128, 4, 512]) for _ in range(2)]
            gm_t = [a_f32([128, 512]) for _ in range(2)]
            sg_t = [a_f32([128, 512]) for _ in range(2)]
            um_t = [a_f32([128, 512]) for _ in range(2)]
            BGU, bBGU = a_f32([32, 2 * D])
            bguT, bbguT = a_f32([128, 16, NE])
            G2, bG2 = a_f32([128, D])
            B2, bB2 = a_f32([128, D])
            junk2, bj2 = a_f32([128, D])
            st2, bst2 = a_f32([128, 8])
            P.dma(BGU, Wd["b_gu"][l], W=[bBGU], key="c")
            bcast_load(G2, bG2, Wd["ln2_g"][l])
            bcast_load(B2, bB2, Wd["ln2_b"][l])
            pt, pb = psum()
            for c in range(16):
                P.pe("transpose", out=pt[:, c * NE:(c + 1) * NE], in_=BGU[:, c * 128:(c + 1) * 128],
                     identity=ident[0:NE, 0:NE], R=[bBGU, bC], W=[pb])
            P.dve("tensor_copy", out=bguT, in_=pt[:, :].rearrange("p (a b) -> p a b", a=16), R=[pb], W=[bbguT])
            hi = 0
            ei = 0
            for e in range(NE):
                for g in range(2):
                    wg_, bwg_ = WG[hi % 2]
                    wd_, bwd_ = WDn[hi % 2]
                    sl = hi % 2
                    hi += 1
                    wgu = Wd["w_gu"][l][e]
                    P.dma(wg_[:, :, 0:512], wgu[:, g * 512:(g + 1) * 512].rearrange("(k p) c -> p k c", p=128),
                          W=[bwg_], key=f"e{sl}", eng="pool")
                    P.dma(wg_[:, :, 512:1024], wgu[:, D + g * 512:D + (g + 1) * 512].rearrange("(k p) c -> p k c", p=128),
                          W=[bwg_], key=f"e{sl}", eng="pool")
                    P.dma(wd_, Wd["w_down"][l][e][g * 512:(g + 1) * 512, :].rearrange("(k p) c -> p k c", p=128),
                          W=[bwd_], key=f"e{sl}", eng="pool")
                    for tb in range(4):
                        a_, ba_ = actb[ei % 2]
                        for fc in range(4):
                            gm, bgm = gm_t[ei % 2]
                            sg, bsg = sg_t[ei % 2]
                            um, bum = um_t[ei % 2]
                            ei += 1
                            jg = g * 4 + fc
                            ju = 8 + g * 4 + fc
                            pg, pgb = psum()
                            for k in range(8):
                                P.pe("matmul", pg[:, :], lhsT=wg_[:, k, fc * 128:(fc + 1) * 128],
                                     rhs=XT[:, k, tb * 512:(tb + 1) * 512], start=(k == 0), stop=(k == 7),
                                     R=[bwg_] + bXT[tb * 4:tb * 4 + 4], W=[pgb])
                            pu, pub = psum()
                            for k in range(8):
                                P.pe("matmul", pu[:, :], lhsT=wg_[:, k, 512 + fc * 128:512 + (fc + 1) * 128],
                                     rhs=XT[:, k, tb * 512:(tb + 1) * 512], start=(k == 0), stop=(k == 7),
                                     R=[bwg_] + bXT[tb * 4:tb * 4 + 4], W=[pub])
                            P.dve("tensor_scalar", out=gm, in0=pg[:, :], scalar1=bguT[:, jg, e:e + 1], scalar2=7.0,
                                  op0=ALU.add, op1=ALU.min, R=[pgb, bbguT], W=[bgm])
                            P.act("activation", out=sg, in_=gm, func=AF.Sigmoid, scale=1.702, R=[bgm], W=[bsg])
                            P.dve("tensor_scalar", out=um, in0=pu[:, :], scalar1=bguT[:, ju, e:e + 1], scalar2=7.0,
                                  op0=ALU.add, op1=ALU.min, R=[pub, bbguT], W=[bum])
                            P.pool("tensor_tensor", out=sg, in0=sg, in1=gm, op=ALU.mult, R=[bsg, bgm], W=[bsg])
                            P.dve("tensor_scalar", out=um, in0=um, scalar1=-7.0, scalar2=1.0,
                                  op0=ALU.max, op1=ALU.add, R=[bum], W=[bum])
                            P.pool("tensor_tensor", out=a_[:, fc, :], in0=um, in1=sg, op=ALU.mult, R=[bum, bsg], W=[ba_])
                        for tt in range(4):
                            t = tb * 4 + tt
                            for nb in range(2):
                                nsl = slice(nb * 512, (nb + 1) * 512)
                                py, pyb = psum()
                                for fc in range(4):
                                    P.pe("matmul", py[:, :], lhsT=a_[:, fc, tt * 128:(tt + 1) * 128],
                                         rhs=wd_[:, fc, nsl], start=(fc == 0), stop=(fc == 3),
                                         R=[ba_, bwd_], W=[pyb])
                                P.dve("scalar_tensor_tensor", out=X[:, t, nsl], in0=py[:, :],
                                      scalar=GATES[:, t, e:e + 1], in1=X[:, t, nsl], op0=ALU.mult,
                                      op1=ALU.add, R=[pyb, bGA, bX[t]], W=[bX[t]])
            for t in range(NT):
                layer_norm_tile(t, G2, B2, bG2, bB2, junk2, bj2, st2, bst2)
                if l != layers[-1] or not last:
                    transpose_tile(t)
            dump_X("h2")

    for t in range(NT):
        P.dma(out_d[t * 128:(t + 1) * 128, :], X[:, t, :], R=[bX[t]], key="out")
    info = P.emit()
    return nc, info


def make_consts():
    j = np.arange(128)[:, None]
    t = np.arange(128)[None, :]
    cst = np.zeros((128, 5, 128), np.float32)
    cst[:, 0, :] = np.eye(128)
    cst[:, 1, :] = 1.0
    cst[:, 2, :] = (j <= t)
    cst[:, 3, :] = (j < t)
    cst[:, 4, :] = (j > t)
    sel = np.zeros((4, 512), np.float32)
    for h in range(4):
        sel[h, h * 128:(h + 1) * 128] = 1.0
    return cst, sel


_CACHE = {}


def run_prog(inputs, xs, layers, first, last, dbg=(), phases=("M", "S", "G", "O", "E"), trace=False, wl0=0):
    key = (tuple(layers), first, last, tuple(dbg), tuple(phases))
    if key not in _CACHE:
        _CACHE[key] = build(layers, first, last, dbg, phases)
    nc, info = _CACHE[key]
    cst, sel = make_consts()
    l0, l1 = layers[0] + wl0, layers[-1] + 1 + wl0
    names = [n for n in W_NAMES]
    in_maps = []
    import concourse.bass as _b
    declared = set(t for t in ["w_in", "m_i_bias", "m_f_bias", "m_norm_w", "sb_norm_w", "g_conv_w",
                               "g_A_log", "g_dt_bias", "g_norm_w"] if set(phases) & {"M", "S", "G"})
    if "O" in phases:
        declared |= {"w_out", "ln1_g", "ln1_b", "w_router", "b_router", "w_ple_gate", "w_ple_proj",
                     "b_down"}
    if "E" in phases:
        declared |= {"w_gu", "b_gu", "w_down", "ln2_g", "ln2_b"}
    wsl = {n: np.ascontiguousarray(np.asarray(inputs[n])[l0:l1]) for n in declared}
    for b in range(8):
        m = {"x": np.ascontiguousarray(xs[b]),
             "p": np.ascontiguousarray(np.asarray(inputs["p"])[l0:l1, b]),
             "ln0_g": np.asarray(inputs["ln0_g"]), "ln0_b": np.asarray(inputs["ln0_b"]),
             "cst": cst, "sel": sel}
        m.update(wsl)
        in_maps.append(m)
    res = run_bass_kernel_spmd(nc, in_maps, core_ids=list(range(8)), trace=trace)
    return res


FUSED = True


def kernel(**inputs):
    xs = np.asarray(inputs["x"], dtype=np.float32)
    if FUSED:
        res = run_prog(inputs, xs, list(range(DEPTH)), True, True)
        return np.stack([np.asarray(r["out"]) for r in res.results], axis=0).astype(np.float32)
    for l in range(DEPTH):
        res = run_prog(inputs, xs, [0], l == 0, True, wl0=l)
        xs = np.stack([np.asarray(r["out"]) for r in res.results], axis=0).astype(np.float32)
    return xs
```

```python
import bisect
from contextlib import ExitStack

import numpy as np
import concourse.bass as bass
import concourse.mybir as mybir
from concourse.bass_utils import run_bass_kernel_spmd

F32 = mybir.dt.float32
BF16 = mybir.dt.bfloat16
AF = mybir.ActivationFunctionType
ALU = mybir.AluOpType
AX = mybir.AxisListType

S = 2048
D = 1024
NT = 16
DEPTH = 4
D_IN = 3856
NE = 32
ALPHA = (2 * DEPTH) ** 0.25
SEM_ROT = 30000
SCHEDULE = True
WINDOW = 32


class Buf:
    __slots__ = ("name", "w", "r", "excl")

    def __init__(self, name):
        self.name = name
        self.w = None
        self.r = {}
        self.excl = False


class Op:
    __slots__ = ("idx", "eng", "method", "args", "kw", "deps", "is_dma", "key",
                 "needs_inc", "seq")


class Prog:
    ENGS = ("pe", "act", "dve", "pool", "sp")

    def __init__(self, nc):
        self.nc = nc
        self.ops = []
        self.dma_keys = {}
        self.st = ExitStack()
        self.nbuf = 0
        self.fences = []

    def sb(self, name, shape, dtype):
        return self.st.enter_context(self.nc.sbuf_tensor(name, list(shape), dtype))

    def ps(self, name, shape, dtype):
        return self.st.enter_context(self.nc.psum_tensor(name, list(shape), dtype))

    def buf(self, name=None):
        self.nbuf += 1
        return Buf(name or f"b{self.nbuf}")

    def add(self, eng, method, *args, R=(), W=(), key=None, **kw):
        op = Op()
        op.idx = len(self.ops)
        op.eng = eng
        op.method = method
        op.args = args
        op.kw = kw
        op.is_dma = method == "dma_start"
        op.key = key
        op.needs_inc = False
        op.seq = None
        deps = set()
        W = list(W) + [b for b in R if b.excl and b not in W]
        for b in R:
            if b.w is not None:
                deps.add(b.w)
        for b in W:
            if b.w is not None:
                deps.add(b.w)
            for ridx in b.r.values():
                deps.add(ridx)
        for d in list(deps):
            dop = self.ops[d]
            if dop.is_dma:
                deps.add(self.dma_keys[dop.key][-1])
        op.deps = deps
        rk = ("dma:" + key) if op.is_dma else eng
        for b in R:
            b.r[rk] = op.idx
        for b in W:
            b.w = op.idx
            b.r = {}
        if op.is_dma:
            self.dma_keys.setdefault(key, []).append(op.idx)
        self.ops.append(op)
        return op

    def pe(self, m, *a, **k): return self.add("pe", m, *a, **k)
    def act(self, m, *a, **k): return self.add("act", m, *a, **k)
    def dve(self, m, *a, **k): return self.add("dve", m, *a, **k)
    def pool(self, m, *a, **k): return self.add("pool", m, *a, **k)

    def dma(self, out, in_, R=(), W=(), key=None, eng="sp"):
        return self.add(eng, "dma_start", R=R, W=W, key=key, out=out, in_=in_)

    def _last(self):
        last = {}
        for op in self.ops:
            if op.is_dma:
                last["dma:" + op.key] = op.idx
            elif op.method is not None:
                last[op.eng] = op.idx
        return set(last.values())

    def barrier(self):
        deps = self._last()
        for e in self.ENGS:
            op = self.add(e, None)
            op.deps = set(deps)
        self.fences.append(len(self.ops))

    @staticmethod
    def _dur(op):
        if op.method is None:
            return 0.0, 0.0
        if op.is_dma:
            out = op.kw["out"]
            n = 1
            for d in out.shape:
                n *= d
            return 0.1, 2.0 + n * 4 / 150e3
        out = op.kw.get("out", op.args[0] if op.args else None)
        n = 1
        for d in out.shape[1:]:
            n *= d
        if op.eng == "pe":
            lhs = op.kw.get("lhsT", op.kw.get("in_"))
            passes = 4 if lhs.dtype == F32 else 1
            d = max(n, 64) * passes / 2.0e3 + 0.03
            return d, d + 0.1
        d = n / 0.9e3 + 0.12
        return d, d + 0.25

    def schedule(self, window=WINDOW):
        ops = self.ops
        n = len(ops)
        users = [[] for _ in range(n)]
        for op in ops:
            for d in op.deps:
                users[d].append(op.idx)
        finish = [0.0] * n
        ready = [0.0] * n
        remaining = [0] * n
        scheduled = [False] * n
        free = {e: 0.0 for e in self.ENGS}
        order = []
        bounds = [0] + [f for f in self.fences if f < n] + [n]
        for si in range(len(bounds) - 1):
            lo, hi = bounds[si], bounds[si + 1]
            if lo >= hi:
                continue
            pending = {e: [] for e in self.ENGS}
            for j in range(lo, hi):
                op = ops[j]
                pending[op.eng].append(j)
                r = 0
                rt = 0.0
                for d in op.deps:
                    if scheduled[d]:
                        if finish[d] > rt:
                            rt = finish[d]
                    else:
                        r += 1
                remaining[j] = r
                ready[j] = rt
            left = hi - lo
            while left:
                best = None
                for e in self.ENGS:
                    pend = pending[e]
                    if not pend:
                        continue
                    fe = free[e]
                    lim = min(window, len(pend))
                    dma_seen = False
                    for i in range(lim):
                        j = pend[i]
                        op = ops[j]
                        if op.method is None:
                            if i == 0 and remaining[j] == 0:
                                st = max(fe, ready[j])
                                if best is None or (st, j) < (best[0], best[1]):
                                    best = (st, j, e, i)
                            break
                        if op.is_dma:
                            if dma_seen:
                                continue
                            dma_seen = True
                        if remaining[j] == 0:
                            st = max(fe, ready[j])
                            if best is None or (st, j) < (best[0], best[1]):
                                best = (st, j, e, i)
                st, j, e, i = best
                op = ops[j]
                busy, lat = self._dur(op)
                free[e] = st + busy
                finish[j] = st + lat
                scheduled[j] = True
                pending[e].pop(i)
                order.append(j)
                left -= 1
                fj = finish[j]
                for u in users[j]:
                    if lo <= u < hi:
                        remaining[u] -= 1
                        if ready[u] < fj:
                            ready[u] = fj
            m = max(free.values())
            for e in self.ENGS:
                free[e] = m
        return order

    def emit(self):
        nc = self.nc
        ops = self.ops
        fin = self.add("sp", None)
        fin.deps = self._last()
        self.fences.append(fin.idx)
        order = self.schedule() if SCHEDULE else list(range(len(ops)))
        assert sorted(order) == list(range(len(ops)))
        sched = [ops[j] for j in order]

        def skip(dop, op):
            return dop.eng == "pe" and op.eng == "pe" and not op.is_dma

        for op in ops:
            for d in op.deps:
                dop = ops[d]
                if dop.is_dma or skip(dop, op):
                    continue
                dop.needs_inc = True
        cnt = {e: 0 for e in self.ENGS}
        for op in sched:
            if op.needs_inc:
                cnt[op.eng] += 1
                op.seq = cnt[op.eng]
        sems = {}
        for e in self.ENGS:
            for ph in range(cnt[e] // SEM_ROT + 1):
                sems[f"s_{e}_{ph}"] = self.st.enter_context(nc.semaphore(f"s_{e}_{ph}"))
        for key in self.dma_keys:
            sems["d_" + key] = self.st.enter_context(nc.semaphore("d_" + key))
        per_eng = {e: [op for op in sched if op.eng == e] for e in self.ENGS}
        nw = [0]

        def emit_engine(ename, e):
            waited = {}
            for op in per_eng[ename]:
                need = {}
                for d in op.deps:
                    dop = ops[d]
                    if dop.is_dma:
                        lst = self.dma_keys[dop.key]
                        c = bisect.bisect_left(lst, op.idx)
                        sn = "d_" + dop.key
                        v = 16 * c
                    else:
                        if skip(dop, op):
                            continue
                        ph = (dop.seq - 1) // SEM_ROT
                        sn = f"s_{dop.eng}_{ph}"
                        v = (dop.seq - 1) % SEM_ROT + 1
                    if need.get(sn, 0) < v:
                        need[sn] = v
                for sn, v in need.items():
                    if waited.get(sn, 0) >= v:
                        continue
                    waited[sn] = v
                    e.wait_ge(sems[sn], v)
                    nw[0] += 1
                if op.method is None:
                    continue
                ins = getattr(e, op.method)(*op.args, **op.kw)
                if op.is_dma:
                    ins.then_inc(sems["d_" + op.key], 16)
                elif op.needs_inc:
                    ph = (op.seq - 1) // SEM_ROT
                    ins.then_inc(sems[f"s_{ename}_{ph}"], 1)

        block = self.st.enter_context(nc.Block())

        @block.tensor
        def _(e): emit_engine("pe", e)

        @block.scalar
        def _(e): emit_engine("act", e)

        @block.vector
        def _(e): emit_engine("dve", e)

        @block.gpsimd
        def _(e): emit_engine("pool", e)

        @block.sync
        def _(e): emit_engine("sp", e)

        self.st.close()
        return {"n_ops": len(ops), "n_waits": nw[0], "cnt": cnt}


ARENA_F32 = 27300

W_NAMES = ["w_in", "m_i_bias", "m_f_bias", "m_norm_w", "sb_norm_w", "g_conv_w", "g_A_log",
           "g_dt_bias", "g_norm_w", "w_out", "ln1_g", "ln1_b", "w_router", "b_router", "w_gu",
           "b_gu", "w_down", "b_down", "w_ple_gate", "w_ple_proj", "ln2_g", "ln2_b"]
W_SHAPES = {"w_in": [D, D_IN], "m_i_bias": [4], "m_f_bias": [4], "m_norm_w": [256],
            "sb_norm_w": [256], "g_conv_w": [4, 1536], "g_A_log": [4], "g_dt_bias": [4],
            "g_norm_w": [128], "w_out": [D, D], "ln1_g": [D], "ln1_b": [D],
            "w_router": [D, NE], "b_router": [NE], "w_gu": [NE, D, 2 * D], "b_gu": [NE, 2 * D],
            "w_down": [NE, D, D], "b_down": [NE, D], "w_ple_gate": [D, D],
            "w_ple_proj": [256, D], "ln2_g": [D], "ln2_b": [D]}


def build(layers, first, last, dbg=(), phases=("M", "S", "G", "O", "E")):
    nc = bass.Bass("TRN2", target_bir_lowering=False)

    def din(name, shape, dt=F32):
        return nc.dram_tensor(name, list(shape), dt, kind="ExternalInput").ap()

    x_d = din("x", [S, D])
    p_d = din("p", [len(layers), S, 256])
    ln0g_d = din("ln0_g", [D])
    ln0b_d = din("ln0_b", [D])
    need = set()
    if set(phases) & {"M", "S", "G"}:
        need |= {"w_in", "m_i_bias", "m_f_bias", "m_norm_w", "sb_norm_w", "g_conv_w", "g_A_log",
                 "g_dt_bias", "g_norm_w"}
    if "O" in phases:
        need |= {"w_out", "ln1_g", "ln1_b", "w_router", "b_router", "w_ple_gate", "w_ple_proj",
                 "b_down"}
    if "E" in phases:
        need |= {"w_gu", "b_gu", "w_down", "ln2_g", "ln2_b"}
    nl = len(layers)
    Wfull = {n: din(n, [nl] + W_SHAPES[n]) for n in W_NAMES if n in need}

    class _WL:
        def __getitem__(self, n):
            class _L:
                def __getitem__(s2, l):
                    return Wfull[n][l - layers[0]]
            return _L()
    Wd = _WL()
    cst_d = din("cst", [128, 5, 128])
    sel_d = din("sel", [4, 512])
    out_d = nc.dram_tensor("out", [S, D], F32, kind="ExternalOutput").ap()
    dbg_d = {}
    for name in dbg:
        if name in ("YT",):
            dbg_d[name] = nc.dram_tensor("dbg_" + name, [128, 8, S], BF16, kind="ExternalOutput").ap()
        else:
            dbg_d[name] = nc.dram_tensor("dbg_" + name, [S, D], F32, kind="ExternalOutput").ap()

    P = Prog(nc)
    X = P.sb("X", [128, NT, D], F32)
    XT = P.sb("XT", [128, 8, S], BF16)
    CST = P.sb("CST", [128, 5, 128], F32)
    SEL = P.sb("SEL", [4, 512], F32)
    AR = P.sb("AR", [128, ARENA_F32], F32)
    bX = [P.buf(f"X{i}") for i in range(NT)]
    bXT = [P.buf(f"XT{i}") for i in range(NT)]
    bC = P.buf("cst")
    ident = CST[:, 0, :]
    ones = CST[:, 1, :]
    TRI = CST[:, 2, :]
    UT1 = CST[:, 3, :]
    L1 = CST[:, 4, :]

    banks = [P.ps(f"bank{i}", [128, 512], F32) for i in range(8)]
    bbank = [P.buf(f"bank{i}") for i in range(8)]
    for b_ in bbank:
        b_.excl = True
    bctr = [0]

    def psum():
        i = bctr[0] % 6
        bctr[0] += 1
        return banks[i], bbank[i]

    actr = [0]

    def psacc():
        i = 6 + actr[0] % 2
        actr[0] += 1
        return banks[i], bbank[i]

    aoff = [0]

    def a_reset(off=0):
        aoff[0] = off

    def a_f32(shape, p0=0):
        n = int(np.prod(shape[1:]))
        v = AR[p0:p0 + shape[0], aoff[0]:aoff[0] + n]
        aoff[0] += n
        assert aoff[0] <= ARENA_F32, aoff[0]
        if len(shape) == 3:
            v = v.rearrange("p (a b) -> p a b", a=shape[1])
        return v, P.buf()

    def a_bf16(shape):
        n = int(np.prod(shape[1:]))
        nf = (n + 1) // 2
        v = AR[0:shape[0], aoff[0]:aoff[0] + nf].bitcast(BF16)[:, 0:n]
        aoff[0] += nf
        assert aoff[0] <= ARENA_F32, aoff[0]
        if len(shape) == 3:
            v = v.rearrange("p (a b) -> p a b", a=shape[1])
        return v, P.buf()

    def rsqrt_small(out, in_, scale, eps, b):
        P.act("activation", out=out, in_=in_, func=AF.Ln, scale=scale, bias=eps, R=[b], W=[b])
        P.act("activation", out=out, in_=out, func=AF.Exp, scale=-0.5, R=[b], W=[b])

    def layer_norm_tile(t, G, B, bG, bB, junk, bj, st, bst):
        xt = X[:, t, :]
        P.dve("reduce_sum", out=st[:, 0:1], in_=xt, axis=AX.X, R=[bX[t]], W=[bst])
        P.dve("tensor_scalar", out=st[:, 1:2], in0=st[:, 0:1], scalar1=-1.0 / D, scalar2=None,
              op0=ALU.mult, R=[bst], W=[bst])
        P.act("activation", out=junk, in_=xt, func=AF.Square, bias=st[:, 1:2], scale=1.0,
              accum_out=st[:, 2:3], R=[bX[t], bst], W=[bj, bst])
        rsqrt_small(st[:, 3:4], st[:, 2:3], 1.0 / D, 1e-5, bst)
        P.dve("tensor_scalar", out=xt, in0=xt, scalar1=st[:, 1:2], scalar2=st[:, 3:4],
              op0=ALU.add, op1=ALU.mult, R=[bX[t], bst], W=[bX[t]])
        P.pool("tensor_tensor", out=xt, in0=xt, in1=G, op=ALU.mult, R=[bX[t], bG], W=[bX[t]])
        P.dve("tensor_tensor", out=xt, in0=xt, in1=B, op=ALU.add, R=[bX[t], bB], W=[bX[t]])

    def transpose_tile(t, extra=None):
        for k in range(0, 8, 4):
            pt, pb = psum()
            for j in range(4):
                P.pe("transpose", out=pt[:, j * 128:(j + 1) * 128],
                     in_=X[:, t, (k + j) * 128:(k + j + 1) * 128], identity=ident,
                     R=[bX[t], bC], W=[pb])
            P.act("activation", out=XT[:, k:k + 4, t * 128:(t + 1) * 128],
                  in_=pt[:].rearrange("p (j c) -> p j c", j=4), func=AF.Copy,
                  R=[pb], W=[bXT[t]])
            if extra is not None:
                extra(k, pt, pb)

    wkey = [0]

    def load_w(dst, bdst, src_cols_list, slot):
        for src, c0, n in src_cols_list:
            P.dma(dst[:, :, c0:c0 + n], src.rearrange("(k p) c -> p k c", p=128),
                  W=[bdst], key=f"w{slot}", eng="pool")

    def proj_fm(Wt, bW, c0, ncols, evac, pbase=0, tbs=range(4)):
        for tb in tbs:
            pt, pb = psum()
            for k in range(8):
                P.pe("matmul", pt[pbase:pbase + ncols, :], lhsT=Wt[:, k, c0:c0 + ncols],
                     rhs=XT[:, k, tb * 512:(tb + 1) * 512], start=(k == 0), stop=(k == 7),
                     R=[bW] + bXT[tb * 4:tb * 4 + 4], W=[pb])
            evac(tb, pt[pbase:pbase + ncols, :], pb)

    def proj_tm(Wt, bW, c0, ncols, t):
        pt, pb = psum()
        for k in range(8):
            P.pe("matmul", pt[:, 0:ncols], lhsT=XT[:, k, t * 128:(t + 1) * 128],
                 rhs=Wt[:, k, c0:c0 + ncols], start=(k == 0), stop=(k == 7),
                 R=[bW, bXT[t]], W=[pb])
        return pt, pb

    def bcast_load(dst, bdst, src, key="c"):
        P.dma(dst, src.partition_broadcast(128), W=[bdst], key=key)

    def dump_X(name):
        if name in dbg_d:
            for t in range(NT):
                P.dma(dbg_d[name][t * 128:(t + 1) * 128, :], X[:, t, :], R=[bX[t]], key="dbg")

    P.dma(CST[:], cst_d, W=[bC], key="c")
    P.dma(SEL[:], sel_d, W=[bC], key="c")
    for t in range(NT):
        P.dma(X[:, t, :], x_d[t * 128:(t + 1) * 128, :], W=[bX[t]], key="x")
    a_reset()
    GATES, bGA = a_f32([128, NT, NE])
    AE = aoff[0]
    YT, _ = a_bf16([128, 8, S])
    bYT = [P.buf(f"YT{k}") for k in range(8)]
    A0 = aoff[0]
    if "YT" in dbg_d:
        for k in range(8):
            P.pool("memset", YT[:, k, :], 0.0, W=[bYT[k]])
    G_t, bG = a_f32([128, D])
    B_t, bB = a_f32([128, D])
    junk, bj = a_f32([128, D])
    st, bst = a_f32([128, 8])
    if first:
        bcast_load(G_t, bG, ln0g_d)
        bcast_load(B_t, bB, ln0b_d)
        for t in range(NT):
            layer_norm_tile(t, G_t, B_t, bG, bB, junk, bj, st, bst)
    for t in range(NT):
        transpose_tile(t)
    dump_X("h0")

    for l in layers:
        w_in = Wd["w_in"][l]
        P.barrier()
        a_reset(A0)
        qT, bq = a_bf16([64, S])
        kT, bk = a_bf16([64, S])
        vext, bv = a_bf16([128, NT, 80])
        Ytok, bY = a_f32([128, NT, 128])
        Wm = [a_bf16([128, 8, 256]) for _ in range(2)]
        wTt = [a_bf16([128, 512]) for _ in range(2)]
        oacc, boacc = a_f32([128, 4, 65])
        Gtok, bGtok = a_f32([128, NT, 4])
        emtok, bem = a_f32([128, NT, 4])
        sm, bsm = a_f32([128, 16])
        hh, bhh = a_f32([128, 64])
        osig, bos = a_f32([128, 64])
        NWm, bNWm = a_f32([128, 256])
        NWs, bNWs = a_f32([128, 256])
        gb4, bgb4 = a_f32([4, 4])
        A1 = aoff[0]
        gA, bgA = a_f32([4, S])
        gB, bgB = a_f32([4, S])
        gC, bgC = a_f32([4, S])
        MGrow, bMG = a_f32([128, S])
        DT = [a_f32([128, 512]) for _ in range(2)]
        a_reset(A1)
        e_t = [a_f32([128, 512]) for _ in range(2)]
        sp_t = [a_f32([128, 512]) for _ in range(2)]
        arg_t = [a_f32([128, 512]) for _ in range(2)]
        Suf, bSuf = a_f32([128, 512])
        bcast_load(NWm, bNWm, Wd["m_norm_w"][l])
        bcast_load(NWs, bNWs, Wd["sb_norm_w"][l])
        P.dma(gb4[:, 0:1], Wd["m_i_bias"][l].rearrange("(h o) -> h o", o=1), W=[bgb4], key="c")
        P.dma(gb4[:, 1:2], Wd["m_f_bias"][l].rearrange("(h o) -> h o", o=1), W=[bgb4], key="c")
        P.dve("tensor_scalar", out=gb4[:, 2:3], in0=gb4[:, 1:2], scalar1=-1.0, scalar2=None,
              op0=ALU.mult, R=[bgb4], W=[bgb4])
        wslot = [0]

        def next_w():
            i = wslot[0] % 2
            wslot[0] += 1
            return Wm[i][0], Wm[i][1], i

        if "M" in phases:
            Wt, bW, sl = next_w()
            load_w(Wt, bW, [(w_in[:, 1024:1032], 0, 8)], sl)

            def ev_i(tb, ps, pb):
                P.act("activation", out=gA[:, tb * 512:(tb + 1) * 512], in_=ps, func=AF.Identity,
                      bias=gb4[:, 0:1], scale=1.0, R=[pb, bgb4], W=[bgA])
            proj_fm(Wt, bW, 0, 4, ev_i)

            def ev_f(tb, ps, pb):
                P.act("activation", out=gB[:, tb * 512:(tb + 1) * 512], in_=ps, func=AF.Exp,
                      bias=gb4[:, 2:3], scale=-1.0, R=[pb, bgb4], W=[bgB])
            proj_fm(Wt, bW, 4, 4, ev_f)
            P.act("activation", out=gB, in_=gB, func=AF.Ln, bias=1.0, scale=1.0, R=[bgB], W=[bgB])
            P.dve("tensor_tensor_scan", out=gC, data0=gB, data1=gB, initial=0.0,
                  op0=ALU.add, op1=ALU.max, R=[bgB], W=[bgC])
            P.dve("tensor_tensor", out=gA, in0=gA, in1=gC, op=ALU.add, R=[bgA, bgC], W=[bgA])
            P.dve("tensor_tensor_scan", out=gB, data0=gA, data1=gA, initial=-1e30,
                  op0=ALU.max, op1=ALU.max, R=[bgA], W=[bgB])
            P.dve("tensor_tensor", out=gC, in0=gC, in1=gB, op=ALU.subtract, R=[bgC, bgB], W=[bgC])
            pt, pb = psum()
            for j in range(NT):
                P.pe("transpose", out=pt[:, j * 4:(j + 1) * 4], in_=gA[:, j * 128:(j + 1) * 128],
                     identity=ident[0:4, 0:4], R=[bgA, bC], W=[pb])
                P.pe("transpose", out=pt[:, 64 + j * 4:64 + (j + 1) * 4],
                     in_=gC[:, j * 128:(j + 1) * 128], identity=ident[0:4, 0:4],
                     R=[bgC, bC], W=[pb])
            P.dve("tensor_copy", out=Gtok, in_=pt[:, 0:64].rearrange("p (a b) -> p a b", a=NT),
                  R=[pb], W=[bGtok])
            P.act("activation", out=emtok, in_=pt[:, 64:128].rearrange("p (a b) -> p a b", a=NT),
                  func=AF.Exp, R=[pb], W=[bem])
            P.dve("memset", vext[:, :, 64:66], 1.0, W=[bv])

        def epilogue(qb, pacc, pab, h, hslot, width, NW, bNW, is_m):
            P.act("activation", out=oacc[:, :, 0:width],
                  in_=pacc[:, 0:4 * width].rearrange("p (a b) -> p a b", a=4), func=AF.Copy,
                  R=[pab], W=[boacc])
            for tt in range(4):
                t = 4 * qb + tt
                num = oacc[:, tt, 0:64]
                if is_m:
                    den = oacc[:, tt, 64:65]
                    P.dve("tensor_scalar", out=sm[:, 0:1], in0=den, scalar1=-1.0, scalar2=None,
                          op0=ALU.mult, R=[boacc], W=[bsm])
                    P.dve("tensor_tensor", out=sm[:, 1:2], in0=sm[:, 0:1], in1=den, op=ALU.max,
                          R=[bsm, boacc], W=[bsm])
                    P.dve("tensor_tensor", out=sm[:, 2:3], in0=sm[:, 1:2], in1=emtok[:, t, h:h + 1],
                          op=ALU.max, R=[bsm, bem], W=[bsm])
                    P.dve("reciprocal", out=sm[:, 3:4], in_=sm[:, 2:3], R=[bsm], W=[bsm])
                    P.dve("tensor_scalar", out=hh, in0=num, scalar1=sm[:, 3:4], scalar2=None,
                          op0=ALU.mult, R=[boacc, bsm], W=[bhh])
                    src, bsrc = hh, bhh
                else:
                    src, bsrc = num, boacc
                P.act("activation", out=osig, in_=src, func=AF.Square, accum_out=sm[:, 4:5],
                      R=[bsrc], W=[bos, bsm])
                rsqrt_small(sm[:, 5:6], sm[:, 4:5], 1.0 / 64, 1e-6, bsm)
                dst = Ytok[:, t, hslot * 64:(hslot + 1) * 64]
                P.dve("scalar_tensor_tensor", out=dst, in0=src, scalar=sm[:, 5:6],
                      in1=NW[:, h * 64:(h + 1) * 64], op0=ALU.mult, op1=ALU.mult,
                      R=[bsrc, bsm, bNW], W=[bY])
                if is_m:
                    pt, pb = proj_tm(Wcur[0], Wcur[1], 192, 64, t)
                    P.act("activation", out=osig, in_=pt[:, 0:64], func=AF.Sigmoid, R=[pb], W=[bos])
                    P.pool("tensor_tensor", out=dst, in0=dst, in1=osig, op=ALU.mult,
                           R=[bY, bos], W=[bY])

        def flush_Y(chunk, Ytok=Ytok, bY=bY):
            for t0 in range(0, NT, 4):
                pt, pb = psum()
                for j in range(4):
                    P.pe("transpose", out=pt[:, j * 128:(j + 1) * 128], in_=Ytok[:, t0 + j, :],
                         identity=ident, R=[bY, bC], W=[pb])
                P.act("activation", out=YT[:, chunk, t0 * 128:(t0 + 4) * 128], in_=pt[:],
                      func=AF.Copy, R=[pb], W=[bYT[chunk]])

        Wcur = [None, None]
        if "M" in phases:
            for h in range(4):
                Wt, bW, sl = next_w()
                Wcur[0], Wcur[1] = Wt, bW
                load_w(Wt, bW, [(w_in[:, h * 64:(h + 1) * 64], 0, 64),
                                (w_in[:, 256 + h * 64:256 + (h + 1) * 64], 64, 64),
                                (w_in[:, 512 + h * 64:512 + (h + 1) * 64], 128, 64),
                                (w_in[:, 768 + h * 64:768 + (h + 1) * 64], 192, 64)], sl)

                def ev_q(tb, ps, pb):
                    P.act("activation", out=qT[:, tb * 512:(tb + 1) * 512], in_=ps, func=AF.Copy,
                          R=[pb], W=[bq])
                proj_fm(Wt, bW, 0, 64, ev_q)

                def ev_k(tb, ps, pb):
                    P.act("activation", out=kT[:, tb * 512:(tb + 1) * 512], in_=ps, func=AF.Copy,
                          scale=0.125, R=[pb], W=[bk])
                proj_fm(Wt, bW, 64, 64, ev_k)
                for t in range(NT):
                    pt, pb = proj_tm(Wt, bW, 128, 64, t)
                    P.dve("tensor_copy", out=vext[:, t, 0:64], in_=pt[:, 0:64], R=[pb], W=[bv])
                for tb in range(4):
                    pt, pb = psum()
                    P.pe("matmul", pt[:, :], lhsT=SEL[0:4, h * 128:(h + 1) * 128],
                         rhs=gB[:, tb * 512:(tb + 1) * 512], start=True, stop=True,
                         R=[bC, bgB], W=[pb])
                    P.dve("tensor_copy", out=MGrow[:, tb * 512:(tb + 1) * 512], in_=pt[:, :],
                          R=[pb], W=[bMG])
                pi = 0
                for qb in range(4):
                    pacc, pab = psacc()
                    for kb in range(4 * qb + 4):
                        r = kb - 4 * qb
                        c0 = max(r, 0) * 128
                        n = 512 - c0
                        q0 = qb * 512 + c0
                        pz, pzb = psum()
                        P.pe("matmul", pz[:, 0:n], lhsT=kT[:, kb * 128:(kb + 1) * 128],
                             rhs=qT[:, q0:q0 + n], start=True, stop=True, R=[bk, bq], W=[pzb])
                        dt_, bdt = DT[pi % 2]
                        w_, bw_ = wTt[pi % 2]
                        pi += 1
                        P.act("activation", out=dt_[:, 0:n], in_=MGrow[:, q0:q0 + n], func=AF.Exp,
                              scale=-1.0, bias=Gtok[:, kb, h:h + 1], R=[bMG, bGtok], W=[bdt])
                        if r >= 0:
                            P.pool("tensor_tensor", out=dt_[:, 0:128], in0=dt_[:, 0:128], in1=TRI,
                                   op=ALU.mult, R=[bdt, bC], W=[bdt])
                        P.dve("tensor_tensor", out=w_[:, 0:n], in0=pz[:, 0:n], in1=dt_[:, 0:n],
                              op=ALU.mult, R=[pzb, bdt], W=[bw_])
                        for tt in range(max(r, 0), 4):
                            cc = (tt - max(r, 0)) * 128
                            P.pe("matmul", pacc[:, tt * 65:(tt + 1) * 65], lhsT=w_[:, cc:cc + 128],
                                 rhs=vext[:, kb, 0:65], start=(kb == 0 and tt == 0), stop=(kb == 4 * qb + 3 and tt == 3),
                                 R=[bw_, bv], W=[pab])
                    epilogue(qb, pacc, pab, h, h % 2, 65, NWm, bNWm, True)
                if h % 2 == 1:
                    flush_Y(h // 2)

        if "S" in phases:
            P.barrier()
            for h in range(4):
                Wt, bW, sl = next_w()
                load_w(Wt, bW, [(w_in[:, 1032 + h * 64:1032 + (h + 1) * 64], 0, 64),
                                (w_in[:, 1288 + h * 64:1288 + (h + 1) * 64], 64, 64),
                                (w_in[:, 1544 + h * 64:1544 + (h + 1) * 64], 128, 64)], sl)

                def ev_q(tb, ps, pb):
                    P.act("activation", out=qT[:, tb * 512:(tb + 1) * 512], in_=ps, func=AF.Copy,
                          scale=0.125, R=[pb], W=[bq])
                proj_fm(Wt, bW, 0, 64, ev_q)

                def ev_k(tb, ps, pb):
                    P.act("activation", out=kT[:, tb * 512:(tb + 1) * 512], in_=ps, func=AF.Copy,
                          R=[pb], W=[bk])
                proj_fm(Wt, bW, 64, 64, ev_k)
                for t in range(NT):
                    pt, pb = proj_tm(Wt, bW, 128, 64, t)
                    P.dve("tensor_copy", out=vext[:, t, 0:64], in_=pt[:, 0:64], R=[pb], W=[bv])
                pi = 0
                for qb in range(4):
                    pacc, pab = psacc()
                    P.pool("memset", Suf, 0.0, W=[bSuf])
                    for kb in range(4 * qb + 3, -1, -1):
                        r = kb - 4 * qb
                        c0 = max(r, 0) * 128
                        n = 512 - c0
                        q0 = qb * 512 + c0
                        e_, be_ = e_t[pi % 2]
                        s_, bs_ = sp_t[pi % 2]
                        a_, ba_ = arg_t[pi % 2]
                        w_, bw_ = wTt[pi % 2]
                        pi += 1
                        pz, pzb = psum()
                        P.pe("matmul", pz[:, 0:n], lhsT=kT[:, kb * 128:(kb + 1) * 128],
                             rhs=qT[:, q0:q0 + n], start=True, stop=True, R=[bk, bq], W=[pzb])
                        P.act("activation", out=e_[:, 0:n], in_=pz[:, 0:n], func=AF.Exp,
                              R=[pzb], W=[be_])
                        P.act("activation", out=s_[:, 0:n], in_=e_[:, 0:n], func=AF.Ln, bias=1.0,
                              scale=1.0, R=[be_], W=[bs_])
                        if r >= 0:
                            P.pool("tensor_tensor", out=s_[:, 0:128], in0=s_[:, 0:128], in1=UT1,
                                   op=ALU.mult, R=[bs_, bC], W=[bs_])
                        p2, p2b = psum()
                        P.pe("matmul", p2[:, 0:n], lhsT=L1, rhs=s_[:, 0:n], start=True, stop=True,
                             R=[bC, bs_], W=[p2b])
                        p3, p3b = psum()
                        P.pe("matmul", p3[:, 0:n], lhsT=ones, rhs=s_[:, 0:n], start=True, stop=True,
                             R=[bC, bs_], W=[p3b])
                        P.dve("tensor_tensor", out=a_[:, 0:n], in0=pz[:, 0:n], in1=s_[:, 0:n],
                              op=ALU.subtract, R=[pzb, bs_], W=[ba_])
                        P.dve("tensor_tensor", out=a_[:, 0:n], in0=a_[:, 0:n], in1=p2[:, 0:n],
                              op=ALU.subtract, R=[ba_, p2b], W=[ba_])
                        P.pool("tensor_tensor", out=a_[:, 0:n], in0=a_[:, 0:n], in1=Suf[:, c0:512],
                               op=ALU.subtract, R=[ba_, bSuf], W=[ba_])
                        P.act("activation", out=w_[:, 0:n], in_=a_[:, 0:n], func=AF.Exp,
                              R=[ba_], W=[bw_])
                        if r >= 0:
                            P.pool("tensor_tensor", out=w_[:, 0:128], in0=w_[:, 0:128], in1=UT1,
                                   op=ALU.mult, R=[bw_, bC], W=[bw_])
                        P.dve("tensor_tensor", out=Suf[:, c0:512], in0=Suf[:, c0:512],
                              in1=p3[:, 0:n], op=ALU.add, R=[bSuf, p3b], W=[bSuf])
                        for tt in range(max(r, 0), 4):
                            cc = (tt - max(r, 0)) * 128
                            P.pe("matmul", pacc[:, tt * 64:(tt + 1) * 64], lhsT=w_[:, cc:cc + 128],
                                 rhs=vext[:, kb, 0:64], start=(kb == 4 * qb + 3 and tt == 3), stop=(kb == 0 and tt == 3),
                                 R=[bw_, bv], W=[pab])
                    epilogue(qb, pacc, pab, h, h % 2, 64, NWs, bNWs, False)
                if h % 2 == 1:
                    flush_Y(2 + h // 2)


        if "G" in phases:
            P.barrier()
            a_reset(A0)
            cin, bcin = a_f32([128, S + 4])
            acc, bacc = a_f32([128, S])
            V32, bV32 = a_f32([128, S])
            Uv = acc.rearrange("p (a b) -> p a b", a=NT)
            Yv = V32.rearrange("p (a b) -> p a b", a=NT)
            qTn, bqn = a_bf16([128, S])
            kTn, bkn = a_bf16([128, S])
            KDEC, bKD = a_bf16([128, NT, 128])
            QKT, bQK = a_bf16([128, NT, 128])
            WTt, bWT = a_bf16([128, NT, 128])
            Wgs = [a_bf16([128, 8, 128]) for _ in range(3)]
            sqb, bsqb = a_f32([128, 512])
            rn, brn = a_f32([128, 512])
            g2d, bg2 = a_f32([128, 64])
            be2d, bbe = a_f32([128, 64])
            gam, bgam = a_f32([128, 64])
            glast, bgl = a_f32([128, 64])
            egam, beg = a_f32([128, 64])
            kds, bkds = a_f32([128, 64])
            cdv, bcd = a_f32([128, 64])
            bwv, bbw = a_f32([128, 64])
            g3 = g2d.rearrange("p (a b) -> p a b", a=NT)
            be3 = be2d.rearrange("p (a b) -> p a b", a=NT)
            DTB, bDTB = a_f32([128, 4])
            NEA, bNEA = a_f32([128, 4])
            t4, bt4 = a_f32([128, 4])
            convw, bcw = a_f32([128, 12, 4])
            GNW, bGNW = a_f32([128, 128])
            NPB = 2
            TG_l = [a_f32([128, 128]) for _ in range(NPB)]
            dec_l = [a_f32([128, 256]) for _ in range(NPB)]
            Mm_l = [a_f32([128, 128]) for _ in range(NPB)]
            MT_l = [a_f32([128, 128]) for _ in range(NPB)]
            Pm_l = [a_f32([128, 128]) for _ in range(NPB)]
            Xs_l = [[a_f32([128, 128]) for _ in range(2)] for _ in range(NPB)]
            XTs_l = [[a_f32([128, 128]) for _ in range(2)] for _ in range(NPB)]
            RHS_l = [a_f32([128, 256]) for _ in range(NPB)]
            S_f, bSf = a_f32([128, 128])
            S_b, bSb = a_bf16([128, 128])
            vnb, bvn = a_bf16([128, 128])
            otmp, bot = a_f32([128, 128])
            zs, bzs = a_f32([128, 128])
            sm2, bsm2 = a_f32([128, 8])
            gslot = [0]

            def next_g():
                i = gslot[0] % 3
                gslot[0] += 1
                return Wgs[i][0], Wgs[i][1], 4 + i

            bcast_load(DTB, bDTB, Wd["g_dt_bias"][l])
            bcast_load(NEA, bNEA, Wd["g_A_log"][l])
            bcast_load(GNW, bGNW, Wd["g_norm_w"][l])
            P.act("activation", out=NEA, in_=NEA, func=AF.Exp, R=[bNEA], W=[bNEA])
            P.dve("tensor_scalar", out=NEA, in0=NEA, scalar1=-1.0, scalar2=None, op0=ALU.mult,
                  R=[bNEA], W=[bNEA])
            P.dma(acc[0:4, 0:1536], Wd["g_conv_w"][l], W=[bacc], key="c")
            pt, pb = psum()
            for c in range(12):
                P.pe("transpose", out=pt[:, c * 4:(c + 1) * 4], in_=acc[0:4, c * 128:(c + 1) * 128],
                     identity=ident[0:4, 0:4], R=[bacc, bC], W=[pb])
            P.dve("tensor_copy", out=convw, in_=pt[:, 0:48].rearrange("p (a b) -> p a b", a=12),
                  R=[pb], W=[bcw])
            P.dve("memset", cin[:, 0:3], 0.0, W=[bcin])
            Wt, bW, sl = next_g()
            load_w(Wt, bW, [(w_in[:, 3848:3856], 0, 8)], sl)
            for t in range(NT):
                pt, pb = proj_tm(Wt, bW, 0, 8, t)
                P.dve("tensor_tensor", out=t4, in0=pt[:, 0:4], in1=DTB, op=ALU.add, R=[pb, bDTB], W=[bt4])
                P.act("activation", out=t4, in_=t4, func=AF.Exp, R=[bt4], W=[bt4])
                P.act("activation", out=t4, in_=t4, func=AF.Ln, bias=1.0, scale=1.0, R=[bt4], W=[bt4])
                P.dve("tensor_tensor", out=g3[:, t, :], in0=t4, in1=NEA, op=ALU.mult, R=[bt4, bNEA], W=[bg2])
                P.act("activation", out=be3[:, t, :], in_=pt[:, 4:8], func=AF.Sigmoid, R=[pb], W=[bbe])
            pt, pb = psum()
            P.pe("matmul", pt[:, 0:64], lhsT=TRI, rhs=g2d, start=True, stop=True, R=[bC, bg2], W=[pb])
            P.pe("matmul", pt[:, 64:128], lhsT=ones, rhs=g2d, start=True, stop=True, R=[bC, bg2], W=[pb])
            P.dve("tensor_copy", out=gam, in_=pt[:, 0:64], R=[pb], W=[bgam])
            P.dve("tensor_copy", out=glast, in_=pt[:, 64:128], R=[pb], W=[bgl])
            P.act("activation", out=egam, in_=gam, func=AF.Exp, R=[bgam], W=[beg])
            P.act("activation", out=cdv, in_=glast, func=AF.Exp, R=[bgl], W=[bcd])
            P.dve("tensor_tensor", out=kds, in0=glast, in1=gam, op=ALU.subtract, R=[bgl, bgam], W=[bkds])
            P.act("activation", out=kds, in_=kds, func=AF.Exp, R=[bkds], W=[bkds])
            P.dve("tensor_tensor", out=bwv, in0=be2d, in1=egam, op=ALU.mult, R=[bbe, beg], W=[bbw])

            def l2n(tb, dst_fn):
                tsl = slice(tb * 512, (tb + 1) * 512)
                P.pool("tensor_tensor", out=sqb, in0=acc[:, tsl], in1=acc[:, tsl], op=ALU.mult,
                       R=[bacc], W=[bsqb])
                pt, pb = psum()
                P.pe("matmul", pt[:, :], lhsT=ones, rhs=sqb, start=True, stop=True, R=[bC, bsqb], W=[pb])
                P.act("activation", out=rn, in_=pt[:, :], func=AF.Ln, bias=1e-6, scale=1.0, R=[pb], W=[brn])
                P.act("activation", out=rn, in_=rn, func=AF.Exp, scale=-0.5, R=[brn], W=[brn])
                dst_fn(tsl)

            for h in range(4):
                for nm, col0, cc in (("q", 1800 + h * 128, h), ("v", 2824 + h * 128, 8 + h),
                                     ("k", 2312 + h * 128, 4 + h)):
                    Wt, bW, sl = next_g()
                    load_w(Wt, bW, [(w_in[:, col0:col0 + 128], 0, 128)], sl)

                    def ev_c(tb, ps, pb):
                        P.act("activation", out=cin[:, 3 + tb * 512:3 + (tb + 1) * 512], in_=ps,
                              func=AF.Copy, R=[pb], W=[bcin])
                    proj_fm(Wt, bW, 0, 128, ev_c)
                    P.dve("tensor_scalar", out=acc, in0=cin[:, 3:3 + S], scalar1=convw[:, cc, 3:4],
                          scalar2=None, op0=ALU.mult, R=[bcin, bcw], W=[bacc])
                    for j in (2, 1, 0):
                        P.dve("scalar_tensor_tensor", out=acc, in0=cin[:, j:j + S],
                              scalar=convw[:, cc, j:j + 1], in1=acc, op0=ALU.mult, op1=ALU.add,
                              R=[bcin, bcw, bacc], W=[bacc])
                    P.act("activation", out=acc, in_=acc, func=AF.Silu, R=[bacc], W=[bacc])
                    if nm == "q":
                        for tb in range(4):
                            l2n(tb, lambda tsl: P.dve(
                                "scalar_tensor_tensor", out=qTn[:, tsl], in0=acc[:, tsl],
                                scalar=128 ** -0.5, in1=rn, op0=ALU.mult, op1=ALU.mult,
                                R=[bacc, brn], W=[bqn]))
                    elif nm == "v":
                        P.pool("tensor_copy", out=V32, in_=acc, R=[bacc], W=[bV32])
                    else:
                        for tb in range(4):
                            def kdst(tsl, tb=tb):
                                P.dve("tensor_tensor", out=cin[:, 3 + tb * 512:3 + (tb + 1) * 512],
                                      in0=acc[:, tsl], in1=rn, op=ALU.mult, R=[bacc, brn], W=[bcin])
                                P.act("activation", out=kTn[:, tsl], in_=cin[:, 3 + tb * 512:3 + (tb + 1) * 512],
                                      func=AF.Copy, R=[bcin], W=[bkn])
                            l2n(tb, kdst)
                for c in range(NT):
                    csl = slice(c * 128, (c + 1) * 128)
                    ch = slice(c * 4 + h, c * 4 + h + 1)
                    TG, bTG = TG_l[c % NPB]
                    dec, bdec = dec_l[c % NPB]
                    Mm, bMm = Mm_l[c % NPB]
                    MT, bMT = MT_l[c % NPB]
                    Pm, bPm = Pm_l[c % NPB]
                    Xs = Xs_l[c % NPB]
                    XTs = XTs_l[c % NPB]
                    RHS, bRHS = RHS_l[c % NPB]
                    pk, pkb = psum()
                    P.pe("transpose", out=pk[:, 0:128], in_=cin[:, 3 + c * 128:3 + (c + 1) * 128],
                         identity=ident, R=[bcin, bC], W=[pkb])
                    P.pe("transpose", out=pk[:, 128:256], in_=V32[:, csl], identity=ident,
                         R=[bV32, bC], W=[pkb])
                    P.dve("tensor_scalar", out=RHS[:, 0:128], in0=pk[:, 128:256], scalar1=be2d[:, ch],
                          scalar2=None, op0=ALU.mult, R=[pkb, bbe], W=[bRHS])
                    P.act("activation", out=RHS[:, 128:256], in_=pk[:, 0:128], func=AF.Copy,
                          scale=bwv[:, ch], R=[pkb, bbw], W=[bRHS])
                    P.dve("tensor_scalar", out=KDEC[:, c, :], in0=pk[:, 0:128], scalar1=kds[:, ch],
                          scalar2=None, op0=ALU.mult, R=[pkb, bkds], W=[bKD])
                    P.dve("tensor_scalar", out=TG, in0=TRI, scalar1=g2d[:, ch], scalar2=None,
                          op0=ALU.mult, R=[bC, bg2], W=[bTG])
                    pa, pab_ = psum()
                    P.pe("matmul", pa[:, 0:128], lhsT=TG, rhs=L1, start=True, stop=True, R=[bTG, bC], W=[pab_])
                    P.pe("matmul", pa[:, 128:256], lhsT=L1, rhs=TG, start=True, stop=True, R=[bTG, bC], W=[pab_])
                    P.act("activation", out=dec, in_=pa[:, 0:256], func=AF.Exp, R=[pab_], W=[bdec])
                    P.pool("tensor_tensor", out=dec[:, 0:128], in0=dec[:, 0:128], in1=L1, op=ALU.mult,
                           R=[bdec, bC], W=[bdec])
                    P.pool("tensor_tensor", out=dec[:, 128:256], in0=dec[:, 128:256], in1=TRI, op=ALU.mult,
                           R=[bdec, bC], W=[bdec])
                    pkk, pkkb = psum()
                    P.pe("matmul", pkk[:, 0:128], lhsT=kTn[:, csl], rhs=kTn[:, csl], start=True, stop=True,
                         R=[bkn], W=[pkkb])
                    P.pe("matmul", pkk[:, 128:256], lhsT=kTn[:, csl], rhs=qTn[:, csl], start=True, stop=True,
                         R=[bkn, bqn], W=[pkkb])
                    P.dve("scalar_tensor_tensor", out=Mm, in0=pkk[:, 0:128], scalar=be2d[:, ch],
                          in1=dec[:, 0:128], op0=ALU.mult, op1=ALU.mult, R=[pkkb, bbe, bdec], W=[bMm])
                    P.dve("tensor_tensor", out=QKT[:, c, :], in0=pkk[:, 128:256], in1=dec[:, 128:256],
                          op=ALU.mult, R=[pkkb, bdec], W=[bQK])
                    pm, pmb = psum()
                    P.pe("transpose", out=pm[:, 0:128], in_=Mm, identity=ident, R=[bMm, bC], W=[pmb])
                    P.act("activation", out=MT, in_=pm[:, 0:128], func=AF.Copy, R=[pmb], W=[bMT])
                    P.dve("tensor_tensor", out=Pm, in0=ident, in1=pm[:, 0:128], op=ALU.subtract,
                          R=[bC, pmb], W=[bPm])
                    Xc, bXc, XcT, bXcT = Mm, bMm, MT, bMT
                    for lvl in range(1, 7):
                        px, pxb = psum()
                        P.pe("matmul", px[:, 0:128], lhsT=XcT, rhs=Xc, start=True, stop=True,
                             R=[bXc, bXcT], W=[pxb])
                        if lvl < 6:
                            P.pe("matmul", px[:, 128:256], lhsT=Xc, rhs=XcT, start=True, stop=True,
                                 R=[bXc, bXcT], W=[pxb])
                        Xn, bXn = Xs[lvl % 2]
                        XnT, bXnT = XTs[lvl % 2]
                        P.act("activation", out=Xn, in_=px[:, 0:128], func=AF.Copy, R=[pxb], W=[bXn])
                        if lvl < 6:
                            P.dve("tensor_copy", out=XnT, in_=px[:, 128:256], R=[pxb], W=[bXnT])
                        pp, ppb = psum()
                        P.pe("matmul", pp[:, 0:128], lhsT=Xn, rhs=Pm, start=True, stop=True,
                             R=[bXn, bPm], W=[ppb])
                        P.dve("tensor_tensor", out=Pm, in0=Pm, in1=pp[:, 0:128], op=ALU.add,
                              R=[bPm, ppb], W=[bPm])
                        Xc, bXc, XcT, bXcT = Xn, bXn, XnT, bXnT
                    pu, pub = psum()
                    P.pe("matmul", pu[:, 0:128], lhsT=Pm, rhs=RHS[:, 0:128], start=True, stop=True,
                         R=[bPm, bRHS], W=[pub])
                    P.pe("matmul", pu[:, 128:256], lhsT=RHS[:, 128:256], rhs=Pm, start=True, stop=True,
                         R=[bPm, bRHS], W=[pub])
                    P.act("activation", out=Uv[:, c, :], in_=pu[:, 0:128], func=AF.Copy, R=[pub], W=[bacc])
                    P.dve("tensor_copy", out=WTt[:, c, :], in_=pu[:, 128:256], R=[pub], W=[bWT])
                Wt, bW, sl = next_g()
                load_w(Wt, bW, [(w_in[:, 3336 + h * 128:3336 + (h + 1) * 128], 0, 128)], sl)
                P.dve("memset", S_f, 0.0, W=[bSf])
                P.dve("memset", S_b, 0.0, W=[bSb])
                for c in range(NT):
                    csl = slice(c * 128, (c + 1) * 128)
                    ch = slice(c * 4 + h, c * 4 + h + 1)
                    p1, p1b = psum()
                    P.pe("matmul", p1[:, 0:128], lhsT=WTt[:, c, :], rhs=S_b, start=True, stop=True,
                         R=[bWT, bSb], W=[p1b])
                    P.pe("matmul", p1[:, 128:256], lhsT=qTn[:, csl], rhs=S_b, start=True, stop=True,
                         R=[bqn, bSb], W=[p1b])
                    P.dve("tensor_tensor", out=vnb, in0=Uv[:, c, :], in1=p1[:, 0:128], op=ALU.subtract,
                          R=[bacc, p1b], W=[bvn])
                    p2, p2b = psum()
                    P.pe("matmul", p2[:, 0:128], lhsT=QKT[:, c, :], rhs=vnb, start=True, stop=True,
                         R=[bQK, bvn], W=[p2b])
                    P.pe("matmul", p2[:, 128:256], lhsT=KDEC[:, c, :], rhs=vnb, start=True, stop=True,
                         R=[bKD, bvn], W=[p2b])
                    P.dve("scalar_tensor_tensor", out=S_f, in0=S_f, scalar=cdv[:, ch], in1=p2[:, 128:256],
                          op0=ALU.mult, op1=ALU.add, R=[bSf, bcd, p2b], W=[bSf])
                    P.act("activation", out=S_b, in_=S_f, func=AF.Copy, R=[bSf], W=[bSb])
                    P.act("activation", out=otmp, in_=p1[:, 128:256], func=AF.Copy, scale=egam[:, ch],
                          R=[p1b, beg], W=[bot])
                    P.dve("tensor_tensor", out=otmp, in0=otmp, in1=p2[:, 0:128], op=ALU.add,
                          R=[bot, p2b], W=[bot])
                    P.act("activation", out=zs, in_=otmp, func=AF.Square, accum_out=sm2[:, 0:1],
                          R=[bot], W=[bzs, bsm2])
                    rsqrt_small(sm2[:, 1:2], sm2[:, 0:1], 1.0 / 128, 1e-6, bsm2)
                    pz, pzb = proj_tm(Wt, bW, 0, 128, c)
                    P.act("activation", out=zs, in_=pz[:, 0:128], func=AF.Silu, R=[pzb], W=[bzs])
                    P.dve("scalar_tensor_tensor", out=Yv[:, c, :], in0=otmp, scalar=sm2[:, 1:2], in1=GNW,
                          op0=ALU.mult, op1=ALU.mult, R=[bot, bsm2, bGNW], W=[bV32])
                    P.pool("tensor_tensor", out=Yv[:, c, :], in0=Yv[:, c, :], in1=zs, op=ALU.mult,
                           R=[bV32, bzs], W=[bV32])
                flush_Y(4 + h, Yv, bV32)

        if "YT" in dbg_d:
            P.dma(dbg_d["YT"], YT, R=bYT, key="dbg")

        if "O" in phases:
            P.barrier()
            a_reset(A0)
            Wo, bWo = a_bf16([128, 8, D])
            Wg, bWg = a_bf16([128, 8, D])
            Wp, bWp = a_bf16([128, 2, D])
            G1, bG1 = a_f32([128, D])
            B1, bB1 = a_f32([128, D])
            junk1, bj1 = a_f32([128, D])
            st1, bst1 = a_f32([128, 8])
            WR, bWR = a_f32([128, 8, NE])
            BR, bBR = a_f32([128, NE])
            BD, bBD = a_f32([32, D])
            hT32, bh32 = a_f32([128, 8, 128])
            ptile, bpt = a_f32([128, 256])
            pT, bpT = a_bf16([128, 2, 128])
            lg, blg = a_f32([128, NE])
            msk, bmsk = a_f32([128, NE])
            mx8, bmx = a_f32([128, 16])
            gT, bgT = a_f32([32, 128])
            sig = [a_f32([128, 512]) for _ in range(2)]
            P.dma(Wo, Wd["w_out"][l].rearrange("(k p) c -> p k c", p=128), W=[bWo], key="wo", eng="pool")
            P.dma(Wg, Wd["w_ple_gate"][l].rearrange("(k p) c -> p k c", p=128), W=[bWg], key="wo", eng="pool")
            P.dma(Wp, Wd["w_ple_proj"][l].rearrange("(k p) c -> p k c", p=128), W=[bWp], key="wo", eng="pool")
            P.dma(WR, Wd["w_router"][l].rearrange("(k p) c -> p k c", p=128), W=[bWR], key="c")
            P.dma(BD, Wd["b_down"][l], W=[bBD], key="c")
            bcast_load(BR, bBR, Wd["b_router"][l])
            bcast_load(G1, bG1, Wd["ln1_g"][l])
            bcast_load(B1, bB1, Wd["ln1_b"][l])
            for t in range(NT):
                tsl = slice(t * 128, (t + 1) * 128)
                for nb in range(2):
                    nsl = slice(nb * 512, (nb + 1) * 512)
                    pt, pb = psum()
                    for k in range(8):
                        P.pe("matmul", pt[:, :], lhsT=YT[:, k, tsl], rhs=Wo[:, k, nsl], start=(k == 0),
                             stop=(k == 7), R=[bYT[k], bWo], W=[pb])
                    P.dve("scalar_tensor_tensor", out=X[:, t, nsl], in0=X[:, t, nsl], scalar=ALPHA,
                          in1=pt[:, :], op0=ALU.mult, op1=ALU.add, R=[bX[t], pb], W=[bX[t]])
                layer_norm_tile(t, G1, B1, bG1, bB1, junk1, bj1, st1, bst1)
                if "h1" in dbg_d:
                    P.dma(dbg_d["h1"][tsl, :], X[:, t, :], R=[bX[t]], key="dbg")

                def extra(k, pt, pb):
                    P.dve("tensor_copy", out=hT32[:, k:k + 4, :],
                          in_=pt[:].rearrange("p (j c) -> p j c", j=4), R=[pb], W=[bh32])
                transpose_tile(t, extra)
                pt, pb = psum()
                for k in range(8):
                    P.pe("matmul", pt[:, 0:NE], lhsT=hT32[:, k, :], rhs=WR[:, k, :], start=(k == 0),
                         stop=(k == 7), R=[bh32, bWR], W=[pb])
                P.dve("tensor_tensor", out=lg, in0=pt[:, 0:NE], in1=BR, op=ALU.add, R=[pb, bBR], W=[blg])
                P.dve("max", out=mx8[:, 0:8], in_=lg, R=[blg], W=[bmx])
                P.dve("tensor_scalar", out=msk, in0=lg, scalar1=mx8[:, 3:4], scalar2=None,
                      op0=ALU.is_ge, R=[blg, bmx], W=[bmsk])
                P.dve("tensor_scalar", out=mx8[:, 8:9], in0=mx8[:, 0:1], scalar1=-1.0, scalar2=None,
                      op0=ALU.mult, R=[bmx], W=[bmx])
                P.act("activation", out=lg, in_=lg, func=AF.Exp, bias=mx8[:, 8:9], scale=1.0,
                      R=[blg, bmx], W=[blg])
                P.dve("tensor_tensor", out=lg, in0=lg, in1=msk, op=ALU.mult, R=[blg, bmsk], W=[blg])
                P.dve("reduce_sum", out=mx8[:, 9:10], in_=lg, axis=AX.X, R=[blg], W=[bmx])
                P.dve("reciprocal", out=mx8[:, 10:11], in_=mx8[:, 9:10], R=[bmx], W=[bmx])
                P.dve("tensor_scalar", out=GATES[:, t, :], in0=lg, scalar1=mx8[:, 10:11], scalar2=None,
                      op0=ALU.mult, R=[blg, bmx], W=[bGA])
                pt, pb = psum()
                P.pe("transpose", out=pt[0:NE, 0:128], in_=GATES[:, t, :], identity=ident,
                     R=[bGA, bC], W=[pb])
                P.act("activation", out=gT, in_=pt[0:NE, 0:128], func=AF.Copy, R=[pb], W=[bgT])
                P.dma(ptile, p_d[l - layers[0], tsl, :], W=[bpt], key="pt")
                pt, pb = psum()
                for j in range(2):
                    P.pe("transpose", out=pt[:, j * 128:(j + 1) * 128], in_=ptile[:, j * 128:(j + 1) * 128],
                         identity=ident, R=[bpt, bC], W=[pb])
                P.act("activation", out=pT, in_=pt[:, 0:256].rearrange("p (j c) -> p j c", j=2),
                      func=AF.Copy, R=[pb], W=[bpT])
                for nb in range(2):
                    nsl = slice(nb * 512, (nb + 1) * 512)
                    sg_, bsg_ = sig[nb]
                    pg, pgb = psum()
                    for k in range(8):
                        P.pe("matmul", pg[:, :], lhsT=XT[:, k, tsl], rhs=Wg[:, k, nsl], start=(k == 0),
                             stop=(k == 7), R=[bXT[t], bWg], W=[pgb])
                    pp, ppb = psum()
                    for k in range(2):
                        P.pe("matmul", pp[:, :], lhsT=pT[:, k, :], rhs=Wp[:, k, nsl], start=(k == 0),
                             stop=(k == 1), R=[bpT, bWp], W=[ppb])
                    pbd, pbdb = psum()
                    P.pe("matmul", pbd[:, :], lhsT=gT, rhs=BD[:, nsl], start=True, stop=True,
                         R=[bgT, bBD], W=[pbdb])
                    P.act("activation", out=sg_, in_=pg[:, :], func=AF.Sigmoid, R=[pgb], W=[bsg_])
                    P.dve("tensor_tensor", out=sg_, in0=sg_, in1=pp[:, :], op=ALU.mult, R=[bsg_, ppb], W=[bsg_])
                    P.dve("scalar_tensor_tensor", out=X[:, t, nsl], in0=X[:, t, nsl], scalar=ALPHA,
                          in1=sg_, op0=ALU.mult, op1=ALU.add, R=[bX[t], bsg_], W=[bX[t]])
                    P.dve("tensor_tensor", out=X[:, t, nsl], in0=X[:, t, nsl], in1=pbd[:, :], op=ALU.add,
                          R=[bX[t], pbdb], W=[bX[t]])

        if "E" in phases:
            P.barrier()
            a_reset(AE)
            WG = [a_bf16([128, 8, 1024]) for _ in range(2)]
            WDn = [a_bf16([128, 4, 1024]) for _ in range(2)]
            actb = [a_bf16([128, 4, 512]) for _ in range(3)]
            gm_t = [a_f32([128, 512]) for _ in range(2)]
            sg_t = [a_f32([128, 512]) for _ in range(2)]
            um_t = [a_f32([128, 512]) for _ in range(2)]
            BGU, bBGU = a_f32([32, 2 * D])
            bguT, bbguT = a_f32([128, 16, NE])
            bgu7, _ = a_f32([128, 16, NE])
            G2, bG2 = a_f32([128, D])
            B2, bB2 = a_f32([128, D])
            junk2, bj2 = a_f32([128, D])
            st2, bst2 = a_f32([128, 8])
            P.dma(BGU, Wd["b_gu"][l], W=[bBGU], key="c")
            bcast_load(G2, bG2, Wd["ln2_g"][l])
            bcast_load(B2, bB2, Wd["ln2_b"][l])
            pt, pb = psum()
            for c in range(16):
                P.pe("transpose", out=pt[:, c * NE:(c + 1) * NE], in_=BGU[:, c * 128:(c + 1) * 128],
                     identity=ident[0:NE, 0:NE], R=[bBGU, bC], W=[pb])
            P.dve("tensor_copy", out=bguT, in_=pt[:, :].rearrange("p (a b) -> p a b", a=16), R=[pb], W=[bbguT])
            P.dve("tensor_scalar", out=bgu7, in0=bguT, scalar1=7.0, scalar2=None, op0=ALU.add,
                  R=[bbguT], W=[bbguT])
            ei = [0]

            def emit_gu(e, g, tb, wg_, bwg_, a_, ba_):
                for fc in range(4):
                    gm, bgm = gm_t[ei[0] % 2]
                    sg, bsg = sg_t[ei[0] % 2]
                    um, bum = um_t[ei[0] % 2]
                    ei[0] += 1
                    jg = g * 4 + fc
                    ju = 8 + g * 4 + fc
                    pg, pgb = psum()
                    for k in range(8):
                        P.pe("matmul", pg[:, :], lhsT=wg_[:, k, fc * 128:(fc + 1) * 128],
                             rhs=XT[:, k, tb * 512:(tb + 1) * 512], start=(k == 0), stop=(k == 7),
                             R=[bwg_] + bXT[tb * 4:tb * 4 + 4], W=[pgb])
                    pu, pub = psum()
                    for k in range(8):
                        P.pe("matmul", pu[:, :], lhsT=wg_[:, k, 512 + fc * 128:512 + (fc + 1) * 128],
                             rhs=XT[:, k, tb * 512:(tb + 1) * 512], start=(k == 0), stop=(k == 7),
                             R=[bwg_] + bXT[tb * 4:tb * 4 + 4], W=[pub])
                    P.dve("tensor_scalar", out=gm, in0=pg[:, :], scalar1=bguT[:, jg, e:e + 1], scalar2=7.0,
                          op0=ALU.add, op1=ALU.min, R=[pgb, bbguT], W=[bgm])
                    P.act("activation", out=sg, in_=gm, func=AF.Sigmoid, scale=1.702, R=[bgm], W=[bsg])
                    P.act("activation", out=um, in_=pu[:, :], func=AF.Relu, bias=bgu7[:, ju, e:e + 1],
                          scale=1.0, R=[pub, bbguT], W=[bum])
                    P.pool("tensor_tensor", out=sg, in0=sg, in1=gm, op=ALU.mult, R=[bsg, bgm], W=[bsg])
                    P.dve("tensor_scalar", out=um, in0=um, scalar1=14.0, scalar2=-6.0,
                          op0=ALU.min, op1=ALU.add, R=[bum], W=[bum])
                    P.pool("tensor_tensor", out=a_[:, fc, :], in0=um, in1=sg, op=ALU.mult, R=[bum, bsg], W=[ba_])

            def emit_down(e, g, tb, wd_, bwd_, a_, ba_):
                for tt in range(4):
                    t = tb * 4 + tt
                    for nb in range(2):
                        nsl = slice(nb * 512, (nb + 1) * 512)
                        py, pyb = psum()
                        for fc in range(4):
                            P.pe("matmul", py[:, :], lhsT=a_[:, fc, tt * 128:(tt + 1) * 128],
                                 rhs=wd_[:, fc, nsl], start=(fc == 0), stop=(fc == 3),
                                 R=[ba_, bwd_], W=[pyb])
                        P.dve("scalar_tensor_tensor", out=X[:, t, nsl], in0=py[:, :],
                              scalar=GATES[:, t, e:e + 1], in1=X[:, t, nsl], op0=ALU.mult,
                              op1=ALU.add, R=[pyb, bGA, bX[t]], W=[bX[t]])

            hi = 0
            ti = 0
            prev = None
            for e in range(NE):
                for g in range(2):
                    wg_, bwg_ = WG[hi % 2]
                    wd_, bwd_ = WDn[hi % 2]
                    sl = hi % 2
                    hi += 1
                    wgu = Wd["w_gu"][l][e]
                    P.dma(wg_[:, :, 0:512], wgu[:, g * 512:(g + 1) * 512].rearrange("(k p) c -> p k c", p=128),
                          W=[bwg_], key=f"e{sl}", eng="pool")
                    P.dma(wg_[:, :, 512:1024], wgu[:, D + g * 512:D + (g + 1) * 512].rearrange("(k p) c -> p k c", p=128),
                          W=[bwg_], key=f"e{sl}", eng="pool")
                    P.dma(wd_, Wd["w_down"][l][e][g * 512:(g + 1) * 512, :].rearrange("(k p) c -> p k c", p=128),
                          W=[bwd_], key=f"e{sl}", eng="pool")
                    for tb in range(4):
                        a_, ba_ = actb[ti % 3]
                        ti += 1
                        emit_gu(e, g, tb, wg_, bwg_, a_, ba_)
                        if prev is not None:
                            emit_down(*prev)
                        prev = (e, g, tb, wd_, bwd_, a_, ba_)
            emit_down(*prev)
            for t in range(NT):
                layer_norm_tile(t, G2, B2, bG2, bB2, junk2, bj2, st2, bst2)
                if l != layers[-1] or not last:
                    transpose_tile(t)
            dump_X("h2")

    for t in range(NT):
        P.dma(out_d[t * 128:(t + 1) * 128, :], X[:, t, :], R=[bX[t]], key="out")
    info = P.emit()
    return nc, info


def make_consts():
    j = np.arange(128)[:, None]
    t = np.arange(128)[None, :]
    cst = np.zeros((128, 5, 128), np.float32)
    cst[:, 0, :] = np.eye(128)
    cst[:, 1, :] = 1.0
    cst[:, 2, :] = (j <= t)
    cst[:, 3, :] = (j < t)
    cst[:, 4, :] = (j > t)
    sel = np.zeros((4, 512), np.float32)
    for h in range(4):
        sel[h, h * 128:(h + 1) * 128] = 1.0
    return cst, sel


_CACHE = {}


def run_prog(inputs, xs, layers, first, last, dbg=(), phases=("M", "S", "G", "O", "E"), trace=False, wl0=0):
    key = (tuple(layers), first, last, tuple(dbg), tuple(phases))
    if key not in _CACHE:
        _CACHE[key] = build(layers, first, last, dbg, phases)
    nc, info = _CACHE[key]
    cst, sel = make_consts()
    l0, l1 = layers[0] + wl0, layers[-1] + 1 + wl0
    names = [n for n in W_NAMES]
    in_maps = []
    import concourse.bass as _b
    declared = set(t for t in ["w_in", "m_i_bias", "m_f_bias", "m_norm_w", "sb_norm_w", "g_conv_w",
                               "g_A_log", "g_dt_bias", "g_norm_w"] if set(phases) & {"M", "S", "G"})
    if "O" in phases:
        declared |= {"w_out", "ln1_g", "ln1_b", "w_router", "b_router", "w_ple_gate", "w_ple_proj",
                     "b_down"}
    if "E" in phases:
        declared |= {"w_gu", "b_gu", "w_down", "ln2_g", "ln2_b"}
    wsl = {n: np.ascontiguousarray(np.asarray(inputs[n])[l0:l1]) for n in declared}
    for b in range(8):
        m = {"x": np.ascontiguousarray(xs[b]),
             "p": np.ascontiguousarray(np.asarray(inputs["p"])[l0:l1, b]),
             "ln0_g": np.asarray(inputs["ln0_g"]), "ln0_b": np.asarray(inputs["ln0_b"]),
             "cst": cst, "sel": sel}
        m.update(wsl)
        in_maps.append(m)
    res = run_bass_kernel_spmd(nc, in_maps, core_ids=list(range(8)), trace=trace)
    return res


FUSED = True


def kernel(**inputs):
    xs = np.asarray(inputs["x"], dtype=np.float32)
    if FUSED:
        res = run_prog(inputs, xs, list(range(DEPTH)), True, True)
        return np.stack([np.asarray(r["out"]) for r in res.results], axis=0).astype(np.float32)
    for l in range(DEPTH):
        res = run_prog(inputs, xs, [0], l == 0, True, wl0=l)
        xs = np.stack([np.asarray(r["out"]) for r in res.results], axis=0).astype(np.float32)
    return xs
```

```python
import bisect
from contextlib import ExitStack

import numpy as np
import concourse.bass as bass
import concourse.mybir as mybir
from concourse.bass_utils import run_bass_kernel_spmd

F32 = mybir.dt.float32
BF16 = mybir.dt.bfloat16
AF = mybir.ActivationFunctionType
ALU = mybir.AluOpType
AX = mybir.AxisListType

S = 2048
D = 1024
NT = 16
DEPTH = 4
D_IN = 3856
NE = 32
ALPHA = (2 * DEPTH) ** 0.25
SEM_ROT = 30000
SCHEDULE = True
WINDOW = 32


class Buf:
    __slots__ = ("name", "w", "r", "excl")

    def __init__(self, name):
        self.name = name
        self.w = None
        self.r = {}
        self.excl = False


class Op:
    __slots__ = ("idx", "eng", "method", "args", "kw", "deps", "is_dma", "key",
                 "needs_inc", "seq")


class Prog:
    ENGS = ("pe", "act", "dve", "pool", "sp")

    def __init__(self, nc):
        self.nc = nc
        self.ops = []
        self.dma_keys = {}
        self.st = ExitStack()
        self.nbuf = 0
        self.fences = []

    def sb(self, name, shape, dtype):
        return self.st.enter_context(self.nc.sbuf_tensor(name, list(shape), dtype))

    def ps(self, name, shape, dtype):
        return self.st.enter_context(self.nc.psum_tensor(name, list(shape), dtype))

    def buf(self, name=None):
        self.nbuf += 1
        return Buf(name or f"b{self.nbuf}")

    def add(self, eng, method, *args, R=(), W=(), key=None, **kw):
        op = Op()
        op.idx = len(self.ops)
        op.eng = eng
        op.method = method
        op.args = args
        op.kw = kw
        op.is_dma = method == "dma_start"
        op.key = key
        op.needs_inc = False
        op.seq = None
        deps = set()
        W = list(W) + [b for b in R if b.excl and b not in W]
        for b in R:
            if b.w is not None:
                deps.add(b.w)
        for b in W:
            if b.w is not None:
                deps.add(b.w)
            for ridx in b.r.values():
                deps.add(ridx)
        for d in list(deps):
            dop = self.ops[d]
            if dop.is_dma:
                deps.add(self.dma_keys[dop.key][-1])
        op.deps = deps
        rk = ("dma:" + key) if op.is_dma else eng
        for b in R:
            b.r[rk] = op.idx
        for b in W:
            b.w = op.idx
            b.r = {}
        if op.is_dma:
            self.dma_keys.setdefault(key, []).append(op.idx)
        self.ops.append(op)
        return op

    def pe(self, m, *a, **k): return self.add("pe", m, *a, **k)
    def act(self, m, *a, **k): return self.add("act", m, *a, **k)
    def dve(self, m, *a, **k): return self.add("dve", m, *a, **k)
    def pool(self, m, *a, **k): return self.add("pool", m, *a, **k)

    def dma(self, out, in_, R=(), W=(), key=None, eng="sp"):
        return self.add(eng, "dma_start", R=R, W=W, key=key, out=out, in_=in_)

    def _last(self):
        last = {}
        for op in self.ops:
            if op.is_dma:
                last["dma:" + op.key] = op.idx
            elif op.method is not None:
                last[op.eng] = op.idx
        return set(last.values())

    def barrier(self):
        deps = self._last()
        for e in self.ENGS:
            op = self.add(e, None)
            op.deps = set(deps)
        self.fences.append(len(self.ops))

    @staticmethod
    def _dur(op):
        if op.method is None:
            return 0.0, 0.0
        if op.is_dma:
            out = op.kw["out"]
            n = 1
            for d in out.shape:
                n *= d
            return 0.1, 2.0 + n * 4 / 150e3
        out = op.kw.get("out", op.args[0] if op.args else None)
        n = 1
        for d in out.shape[1:]:
            n *= d
        if op.eng == "pe":
            lhs = op.kw.get("lhsT", op.kw.get("in_"))
            passes = 4 if lhs.dtype == F32 else 1
            d = max(n, 64) * passes / 2.0e3 + 0.03
            return d, d + 0.1
        d = n / 0.9e3 + 0.12
        return d, d + 0.25

    def schedule(self, window=WINDOW):
        ops = self.ops
        n = len(ops)
        users = [[] for _ in range(n)]
        for op in ops:
            for d in op.deps:
                users[d].append(op.idx)
        finish = [0.0] * n
        ready = [0.0] * n
        remaining = [0] * n
        scheduled = [False] * n
        free = {e: 0.0 for e in self.ENGS}
        order = []
        bounds = [0] + [f for f in self.fences if f < n] + [n]
        for si in range(len(bounds) - 1):
            lo, hi = bounds[si], bounds[si + 1]
            if lo >= hi:
                continue
            pending = {e: [] for e in self.ENGS}
            for j in range(lo, hi):
                op = ops[j]
                pending[op.eng].append(j)
                r = 0
                rt = 0.0
                for d in op.deps:
                    if scheduled[d]:
                        if finish[d] > rt:
                            rt = finish[d]
                    else:
                        r += 1
                remaining[j] = r
                ready[j] = rt
            left = hi - lo
            while left:
                best = None
                for e in self.ENGS:
                    pend = pending[e]
                    if not pend:
                        continue
                    fe = free[e]
                    lim = min(window, len(pend))
                    dma_seen = False
                    for i in range(lim):
                        j = pend[i]
                        op = ops[j]
                        if op.method is None:
                            if i == 0 and remaining[j] == 0:
                                st = max(fe, ready[j])
                                if best is None or (st, j) < (best[0], best[1]):
                                    best = (st, j, e, i)
                            break
                        if op.is_dma:
                            if dma_seen:
                                continue
                            dma_seen = True
                        if remaining[j] == 0:
                            st = max(fe, ready[j])
                            if best is None or (st, j) < (best[0], best[1]):
                                best = (st, j, e, i)
                st, j, e, i = best
                op = ops[j]
                busy, lat = self._dur(op)
                free[e] = st + busy
                finish[j] = st + lat
                scheduled[j] = True
                pending[e].pop(i)
                order.append(j)
                left -= 1
                fj = finish[j]
                for u in users[j]:
                    if lo <= u < hi:
                        remaining[u] -= 1
                        if ready[u] < fj:
                            ready[u] = fj
            m = max(free.values())
            for e in self.ENGS:
                free[e] = m
        return order

    def emit(self):
        nc = self.nc
        ops = self.ops
        fin = self.add("sp", None)
        fin.deps = self._last()
        self.fences.append(fin.idx)
        order = self.schedule() if SCHEDULE else list(range(len(ops)))
        assert sorted(order) == list(range(len(ops)))
        sched = [ops[j] for j in order]

        def skip(dop, op):
            return dop.eng == "pe" and op.eng == "pe" and not op.is_dma

        for op in ops:
            for d in op.deps:
                dop = ops[d]
                if dop.is_dma or skip(dop, op):
                    continue
                dop.needs_inc = True
        cnt = {e: 0 for e in self.ENGS}
        for op in sched:
            if op.needs_inc:
                cnt[op.eng] += 1
                op.seq = cnt[op.eng]
        sems = {}
        for e in self.ENGS:
            for ph in range(cnt[e] // SEM_ROT + 1):
                sems[f"s_{e}_{ph}"] = self.st.enter_context(nc.semaphore(f"s_{e}_{ph}"))
        for key in self.dma_keys:
            sems["d_" + key] = self.st.enter_context(nc.semaphore("d_" + key))
        per_eng = {e: [op for op in sched if op.eng == e] for e in self.ENGS}
        nw = [0]

        def emit_engine(ename, e):
            waited = {}
            for op in per_eng[ename]:
                need = {}
                for d in op.deps:
                    dop = ops[d]
                    if dop.is_dma:
                        lst = self.dma_keys[dop.key]
                        c = bisect.bisect_left(lst, op.idx)
                        sn = "d_" + dop.key
                        v = 16 * c
                    else:
                        if skip(dop, op):
                            continue
                        ph = (dop.seq - 1) // SEM_ROT
                        sn = f"s_{dop.eng}_{ph}"
                        v = (dop.seq - 1) % SEM_ROT + 1
                    if need.get(sn, 0) < v:
                        need[sn] = v
                for sn, v in need.items():
                    if waited.get(sn, 0) >= v:
                        continue
                    waited[sn] = v
                    e.wait_ge(sems[sn], v)
                    nw[0] += 1
                if op.method is None:
                    continue
                ins = getattr(e, op.method)(*op.args, **op.kw)
                if op.is_dma:
                    ins.then_inc(sems["d_" + op.key], 16)
                elif op.needs_inc:
                    ph = (op.seq - 1) // SEM_ROT
                    ins.then_inc(sems[f"s_{ename}_{ph}"], 1)

        block = self.st.enter_context(nc.Block())

        @block.tensor
        def _(e): emit_engine("pe", e)

        @block.scalar
        def _(e): emit_engine("act", e)

        @block.vector
        def _(e): emit_engine("dve", e)

        @block.gpsimd
        def _(e): emit_engine("pool", e)

        @block.sync
        def _(e): emit_engine("sp", e)

        self.st.close()
        return {"n_ops": len(ops), "n_waits": nw[0], "cnt": cnt}


ARENA_F32 = 27300

W_NAMES = ["w_in", "m_i_bias", "m_f_bias", "m_norm_w", "sb_norm_w", "g_conv_w", "g_A_log",
           "g_dt_bias", "g_norm_w", "w_out", "ln1_g", "ln1_b", "w_router", "b_router", "w_gu",
           "b_gu", "w_down", "b_down", "w_ple_gate", "w_ple_proj", "ln2_g", "ln2_b"]
W_SHAPES = {"w_in": [D, D_IN], "m_i_bias": [4], "m_f_bias": [4], "m_norm_w": [256],
            "sb_norm_w": [256], "g_conv_w": [4, 1536], "g_A_log": [4], "g_dt_bias": [4],
            "g_norm_w": [128], "w_out": [D, D], "ln1_g": [D], "ln1_b": [D],
            "w_router": [D, NE], "b_router": [NE], "w_gu": [NE, D, 2 * D], "b_gu": [NE, 2 * D],
            "w_down": [NE, D, D], "b_down": [NE, D], "w_ple_gate": [D, D],
            "w_ple_proj": [256, D], "ln2_g": [D], "ln2_b": [D]}


def build(layers, first, last, dbg=(), phases=("M", "S", "G", "O", "E")):
    nc = bass.Bass("TRN2", target_bir_lowering=False)

    def din(name, shape, dt=F32):
        return nc.dram_tensor(name, list(shape), dt, kind="ExternalInput").ap()

    x_d = din("x", [S, D])
    p_d = din("p", [len(layers), S, 256])
    ln0g_d = din("ln0_g", [D])
    ln0b_d = din("ln0_b", [D])
    need = set()
    if set(phases) & {"M", "S", "G"}:
        need |= {"w_in", "m_i_bias", "m_f_bias", "m_norm_w", "sb_norm_w", "g_conv_w", "g_A_log",
                 "g_dt_bias", "g_norm_w"}
    if "O" in phases:
        need |= {"w_out", "ln1_g", "ln1_b", "w_router", "b_router", "w_ple_gate", "w_ple_proj",
                 "b_down"}
    if "E" in phases:
        need |= {"w_gu", "b_gu", "w_down", "ln2_g", "ln2_b"}
    nl = len(layers)
    Wfull = {n: din(n, [nl] + W_SHAPES[n]) for n in W_NAMES if n in need}

    class _WL:
        def __getitem__(self, n):
            class _L:
                def __getitem__(s2, l):
                    return Wfull[n][l - layers[0]]
            return _L()
    Wd = _WL()
    cst_d = din("cst", [128, 5, 128])
    sel_d = din("sel", [4, 512])
    out_d = nc.dram_tensor("out", [S, D], F32, kind="ExternalOutput").ap()
    dbg_d = {}
    for name in dbg:
        if name in ("YT",):
            dbg_d[name] = nc.dram_tensor("dbg_" + name, [128, 8, S], BF16, kind="ExternalOutput").ap()
        else:
            dbg_d[name] = nc.dram_tensor("dbg_" + name, [S, D], F32, kind="ExternalOutput").ap()

    P = Prog(nc)
    X = P.sb("X", [128, NT, D], F32)
    XT = P.sb("XT", [128, 8, S], BF16)
    CST = P.sb("CST", [128, 5, 128], F32)
    SEL = P.sb("SEL", [4, 512], F32)
    AR = P.sb("AR", [128, ARENA_F32], F32)
    bX = [P.buf(f"X{i}") for i in range(NT)]
    bXT = [P.buf(f"XT{i}") for i in range(NT)]
    bC = P.buf("cst")
    ident = CST[:, 0, :]
    ones = CST[:, 1, :]
    TRI = CST[:, 2, :]
    UT1 = CST[:, 3, :]
    L1 = CST[:, 4, :]

    banks = [P.ps(f"bank{i}", [128, 512], F32) for i in range(8)]
    bbank = [P.buf(f"bank{i}") for i in range(8)]
    for b_ in bbank:
        b_.excl = True
    bctr = [0]

    def psum():
        i = bctr[0] % 6
        bctr[0] += 1
        return banks[i], bbank[i]

    actr = [0]

    def psacc():
        i = 6 + actr[0] % 2
        actr[0] += 1
        return banks[i], bbank[i]

    aoff = [0]

    def a_reset(off=0):
        aoff[0] = off

    def a_f32(shape, p0=0):
        n = int(np.prod(shape[1:]))
        v = AR[p0:p0 + shape[0], aoff[0]:aoff[0] + n]
        aoff[0] += n
        assert aoff[0] <= ARENA_F32, aoff[0]
        if len(shape) == 3:
            v = v.rearrange("p (a b) -> p a b", a=shape[1])
        return v, P.buf()

    def a_bf16(shape):
        n = int(np.prod(shape[1:]))
        nf = (n + 1) // 2
        v = AR[0:shape[0], aoff[0]:aoff[0] + nf].bitcast(BF16)[:, 0:n]
        aoff[0] += nf
        assert aoff[0] <= ARENA_F32, aoff[0]
        if len(shape) == 3:
            v = v.rearrange("p (a b) -> p a b", a=shape[1])
        return v, P.buf()

    def rsqrt_small(out, in_, scale, eps, b):
        P.act("activation", out=out, in_=in_, func=AF.Ln, scale=scale, bias=eps, R=[b], W=[b])
        P.act("activation", out=out, in_=out, func=AF.Exp, scale=-0.5, R=[b], W=[b])

    def layer_norm_tile(t, G, B, bG, bB, junk, bj, st, bst):
        xt = X[:, t, :]
        P.dve("reduce_sum", out=st[:, 0:1], in_=xt, axis=AX.X, R=[bX[t]], W=[bst])
        P.dve("tensor_scalar", out=st[:, 1:2], in0=st[:, 0:1], scalar1=-1.0 / D, scalar2=None,
              op0=ALU.mult, R=[bst], W=[bst])
        P.act("activation", out=junk, in_=xt, func=AF.Square, bias=st[:, 1:2], scale=1.0,
              accum_out=st[:, 2:3], R=[bX[t], bst], W=[bj, bst])
        rsqrt_small(st[:, 3:4], st[:, 2:3], 1.0 / D, 1e-5, bst)
        P.dve("tensor_scalar", out=xt, in0=xt, scalar1=st[:, 1:2], scalar2=st[:, 3:4],
              op0=ALU.add, op1=ALU.mult, R=[bX[t], bst], W=[bX[t]])
        P.pool("tensor_tensor", out=xt, in0=xt, in1=G, op=ALU.mult, R=[bX[t], bG], W=[bX[t]])
        P.dve("tensor_tensor", out=xt, in0=xt, in1=B, op=ALU.add, R=[bX[t], bB], W=[bX[t]])

    def transpose_tile(t, extra=None):
        for k in range(0, 8, 4):
            pt, pb = psum()
            for j in range(4):
                P.pe("transpose", out=pt[:, j * 128:(j + 1) * 128],
                     in_=X[:, t, (k + j) * 128:(k + j + 1) * 128], identity=ident,
                     R=[bX[t], bC], W=[pb])
            P.act("activation", out=XT[:, k:k + 4, t * 128:(t + 1) * 128],
                  in_=pt[:].rearrange("p (j c) -> p j c", j=4), func=AF.Copy,
                  R=[pb], W=[bXT[t]])
            if extra is not None:
                extra(k, pt, pb)

    wkey = [0]

    def load_w(dst, bdst, src_cols_list, slot):
        for src, c0, n in src_cols_list:
            P.dma(dst[:, :, c0:c0 + n], src.rearrange("(k p) c -> p k c", p=128),
                  W=[bdst], key=f"w{slot}", eng="pool")

    def proj_fm(Wt, bW, c0, ncols, evac, pbase=0, tbs=range(4)):
        for tb in tbs:
            pt, pb = psum()
            for k in range(8):
                P.pe("matmul", pt[pbase:pbase + ncols, :], lhsT=Wt[:, k, c0:c0 + ncols],
                     rhs=XT[:, k, tb * 512:(tb + 1) * 512], start=(k == 0), stop=(k == 7),
                     R=[bW] + bXT[tb * 4:tb * 4 + 4], W=[pb])
            evac(tb, pt[pbase:pbase + ncols, :], pb)

    def proj_tm(Wt, bW, c0, ncols, t):
        pt, pb = psum()
        for k in range(8):
            P.pe("matmul", pt[:, 0:ncols], lhsT=XT[:, k, t * 128:(t + 1) * 128],
                 rhs=Wt[:, k, c0:c0 + ncols], start=(k == 0), stop=(k == 7),
                 R=[bW, bXT[t]], W=[pb])
        return pt, pb

    def bcast_load(dst, bdst, src, key="c"):
        P.dma(dst, src.partition_broadcast(128), W=[bdst], key=key)

    def dump_X(name):
        if name in dbg_d:
            for t in range(NT):
                P.dma(dbg_d[name][t * 128:(t + 1) * 128, :], X[:, t, :], R=[bX[t]], key="dbg")

    P.dma(CST[:], cst_d, W=[bC], key="c")
    P.dma(SEL[:], sel_d, W=[bC], key="c")
    for t in range(NT):
        P.dma(X[:, t, :], x_d[t * 128:(t + 1) * 128, :], W=[bX[t]], key="x")
    a_reset()
    GATES, bGA = a_f32([128, NT, NE])
    AE = aoff[0]
    YT, _ = a_bf16([128, 8, S])
    bYT = [P.buf(f"YT{k}") for k in range(8)]
    A0 = aoff[0]
    if "YT" in dbg_d:
        for k in range(8):
            P.pool("memset", YT[:, k, :], 0.0, W=[bYT[k]])
    G_t, bG = a_f32([128, D])
    B_t, bB = a_f32([128, D])
    junk, bj = a_f32([128, D])
    st, bst = a_f32([128, 8])
    if first:
        bcast_load(G_t, bG, ln0g_d)
        bcast_load(B_t, bB, ln0b_d)
        for t in range(NT):
            layer_norm_tile(t, G_t, B_t, bG, bB, junk, bj, st, bst)
    for t in range(NT):
        transpose_tile(t)
    dump_X("h0")

    for l in layers:
        w_in = Wd["w_in"][l]
        P.barrier()
        a_reset(A0)
        qT, bq = a_bf16([64, S])
        kT, bk = a_bf16([64, S])
        vext, bv = a_bf16([128, NT, 80])
        Ytok, bY = a_f32([128, NT, 128])
        Wm = [a_bf16([128, 8, 256]) for _ in range(2)]
        wTt = [a_bf16([128, 512]) for _ in range(2)]
        oacc, boacc = a_f32([128, 4, 65])
        Gtok, bGtok = a_f32([128, NT, 4])
        emtok, bem = a_f32([128, NT, 4])
        sm, bsm = a_f32([128, 16])
        hh, bhh = a_f32([128, 64])
        osig, bos = a_f32([128, 64])
        NWm, bNWm = a_f32([128, 256])
        NWs, bNWs = a_f32([128, 256])
        gb4, bgb4 = a_f32([4, 4])
        A1 = aoff[0]
        gA, bgA = a_f32([4, S])
        gB, bgB = a_f32([4, S])
        gC, bgC = a_f32([4, S])
        MGrow, bMG = a_f32([128, S])
        DT = [a_f32([128, 512]) for _ in range(2)]
        a_reset(A1)
        e_t = [a_f32([128, 512]) for _ in range(2)]
        sp_t = [a_f32([128, 512]) for _ in range(2)]
        arg_t = [a_f32([128, 512]) for _ in range(2)]
        Suf, bSuf = a_f32([128, 512])
        bcast_load(NWm, bNWm, Wd["m_norm_w"][l])
        bcast_load(NWs, bNWs, Wd["sb_norm_w"][l])
        P.dma(gb4[:, 0:1], Wd["m_i_bias"][l].rearrange("(h o) -> h o", o=1), W=[bgb4], key="c")
        P.dma(gb4[:, 1:2], Wd["m_f_bias"][l].rearrange("(h o) -> h o", o=1), W=[bgb4], key="c")
        P.dve("tensor_scalar", out=gb4[:, 2:3], in0=gb4[:, 1:2], scalar1=-1.0, scalar2=None,
              op0=ALU.mult, R=[bgb4], W=[bgb4])
        wslot = [0]

        def next_w():
            i = wslot[0] % 2
            wslot[0] += 1
            return Wm[i][0], Wm[i][1], i

        if "M" in phases:
            Wt, bW, sl = next_w()
            load_w(Wt, bW, [(w_in[:, 1024:1032], 0, 8)], sl)

            def ev_i(tb, ps, pb):
                P.act("activation", out=gA[:, tb * 512:(tb + 1) * 512], in_=ps, func=AF.Identity,
                      bias=gb4[:, 0:1], scale=1.0, R=[pb, bgb4], W=[bgA])
            proj_fm(Wt, bW, 0, 4, ev_i)

            def ev_f(tb, ps, pb):
                P.act("activation", out=gB[:, tb * 512:(tb + 1) * 512], in_=ps, func=AF.Exp,
                      bias=gb4[:, 2:3], scale=-1.0, R=[pb, bgb4], W=[bgB])
            proj_fm(Wt, bW, 4, 4, ev_f)
            P.act("activation", out=gB, in_=gB, func=AF.Ln, bias=1.0, scale=1.0, R=[bgB], W=[bgB])
            P.dve("tensor_tensor_scan", out=gC, data0=gB, data1=gB, initial=0.0,
                  op0=ALU.add, op1=ALU.max, R=[bgB], W=[bgC])
            P.dve("tensor_tensor", out=gA, in0=gA, in1=gC, op=ALU.add, R=[bgA, bgC], W=[bgA])
            P.dve("tensor_tensor_scan", out=gB, data0=gA, data1=gA, initial=-1e30,
                  op0=ALU.max, op1=ALU.max, R=[bgA], W=[bgB])
            P.dve("tensor_tensor", out=gC, in0=gC, in1=gB, op=ALU.subtract, R=[bgC, bgB], W=[bgC])
            pt, pb = psum()
            for j in range(NT):
                P.pe("transpose", out=pt[:, j * 4:(j + 1) * 4], in_=gA[:, j * 128:(j + 1) * 128],
                     identity=ident[0:4, 0:4], R=[bgA, bC], W=[pb])
                P.pe("transpose", out=pt[:, 64 + j * 4:64 + (j + 1) * 4],
                     in_=gC[:, j * 128:(j + 1) * 128], identity=ident[0:4, 0:4],
                     R=[bgC, bC], W=[pb])
            P.dve("tensor_copy", out=Gtok, in_=pt[:, 0:64].rearrange("p (a b) -> p a b", a=NT),
                  R=[pb], W=[bGtok])
            P.act("activation", out=emtok, in_=pt[:, 64:128].rearrange("p (a b) -> p a b", a=NT),
                  func=AF.Exp, R=[pb], W=[bem])
            P.dve("memset", vext[:, :, 64:66], 1.0, W=[bv])

        def epilogue(qb, pacc, pab, h, hslot, width, NW, bNW, is_m):
            P.act("activation", out=oacc[:, :, 0:width],
                  in_=pacc[:, 0:4 * width].rearrange("p (a b) -> p a b", a=4), func=AF.Copy,
                  R=[pab], W=[boacc])
            for tt in range(4):
                t = 4 * qb + tt
                num = oacc[:, tt, 0:64]
                if is_m:
                    den = oacc[:, tt, 64:65]
                    P.dve("tensor_scalar", out=sm[:, 0:1], in0=den, scalar1=-1.0, scalar2=None,
                          op0=ALU.mult, R=[boacc], W=[bsm])
                    P.dve("tensor_tensor", out=sm[:, 1:2], in0=sm[:, 0:1], in1=den, op=ALU.max,
                          R=[bsm, boacc], W=[bsm])
                    P.dve("tensor_tensor", out=sm[:, 2:3], in0=sm[:, 1:2], in1=emtok[:, t, h:h + 1],
                          op=ALU.max, R=[bsm, bem], W=[bsm])
                    P.dve("reciprocal", out=sm[:, 3:4], in_=sm[:, 2:3], R=[bsm], W=[bsm])
                    P.dve("tensor_scalar", out=hh, in0=num, scalar1=sm[:, 3:4], scalar2=None,
                          op0=ALU.mult, R=[boacc, bsm], W=[bhh])
                    src, bsrc = hh, bhh
                else:
                    src, bsrc = num, boacc
                P.act("activation", out=osig, in_=src, func=AF.Square, accum_out=sm[:, 4:5],
                      R=[bsrc], W=[bos, bsm])
                rsqrt_small(sm[:, 5:6], sm[:, 4:5], 1.0 / 64, 1e-6, bsm)
                dst = Ytok[:, t, hslot * 64:(hslot + 1) * 64]
                P.dve("scalar_tensor_tensor", out=dst, in0=src, scalar=sm[:, 5:6],
                      in1=NW[:, h * 64:(h + 1) * 64], op0=ALU.mult, op1=ALU.mult,
                      R=[bsrc, bsm, bNW], W=[bY])
                if is_m:
                    pt, pb = proj_tm(Wcur[0], Wcur[1], 192, 64, t)
                    P.act("activation", out=osig, in_=pt[:, 0:64], func=AF.Exp, scale=-1.0, R=[pb], W=[bos])
                    P.dve("tensor_scalar", out=osig, in0=osig, scalar1=1.0, scalar2=None, op0=ALU.add,
                          R=[bos], W=[bos])
                    P.dve("reciprocal", out=osig, in_=osig, R=[bos], W=[bos])
                    P.pool("tensor_tensor", out=dst, in0=dst, in1=osig, op=ALU.mult,
                           R=[bY, bos], W=[bY])

        def flush_Y(chunk, Ytok=Ytok, bY=bY):
            for t0 in range(0, NT, 4):
                pt, pb = psum()
                for j in range(4):
                    P.pe("transpose", out=pt[:, j * 128:(j + 1) * 128], in_=Ytok[:, t0 + j, :],
                         identity=ident, R=[bY, bC], W=[pb])
                P.act("activation", out=YT[:, chunk, t0 * 128:(t0 + 4) * 128], in_=pt[:],
                      func=AF.Copy, R=[pb], W=[bYT[chunk]])

        Wcur = [None, None]
        if "M" in phases:
            for h in range(4):
                Wt, bW, sl = next_w()
                Wcur[0], Wcur[1] = Wt, bW
                load_w(Wt, bW, [(w_in[:, h * 64:(h + 1) * 64], 0, 64),
                                (w_in[:, 256 + h * 64:256 + (h + 1) * 64], 64, 64),
                                (w_in[:, 512 + h * 64:512 + (h + 1) * 64], 128, 64),
                                (w_in[:, 768 + h * 64:768 + (h + 1) * 64], 192, 64)], sl)

                def ev_q(tb, ps, pb):
                    P.act("activation", out=qT[:, tb * 512:(tb + 1) * 512], in_=ps, func=AF.Copy,
                          R=[pb], W=[bq])
                proj_fm(Wt, bW, 0, 64, ev_q)

                def ev_k(tb, ps, pb):
                    P.act("activation", out=kT[:, tb * 512:(tb + 1) * 512], in_=ps, func=AF.Copy,
                          scale=0.125, R=[pb], W=[bk])
                proj_fm(Wt, bW, 64, 64, ev_k)
                for t in range(NT):
                    pt, pb = proj_tm(Wt, bW, 128, 64, t)
                    P.dve("tensor_copy", out=vext[:, t, 0:64], in_=pt[:, 0:64], R=[pb], W=[bv])
                for tb in range(4):
                    pt, pb = psum()
                    P.pe("matmul", pt[:, :], lhsT=SEL[0:4, h * 128:(h + 1) * 128],
                         rhs=gB[:, tb * 512:(tb + 1) * 512], start=True, stop=True,
                         R=[bC, bgB], W=[pb])
                    P.dve("tensor_copy", out=MGrow[:, tb * 512:(tb + 1) * 512], in_=pt[:, :],
                          R=[pb], W=[bMG])
                pi = 0
                for qb in range(4):
                    pacc, pab = psacc()
                    for kb in range(4 * qb + 4):
                        r = kb - 4 * qb
                        c0 = max(r, 0) * 128
                        n = 512 - c0
                        q0 = qb * 512 + c0
                        pz, pzb = psum()
                        P.pe("matmul", pz[:, 0:n], lhsT=kT[:, kb * 128:(kb + 1) * 128],
                             rhs=qT[:, q0:q0 + n], start=True, stop=True, R=[bk, bq], W=[pzb])
                        dt_, bdt = DT[pi % 2]
                        w_, bw_ = wTt[pi % 2]
                        pi += 1
                        P.act("activation", out=dt_[:, 0:n], in_=MGrow[:, q0:q0 + n], func=AF.Exp,
                              scale=-1.0, bias=Gtok[:, kb, h:h + 1], R=[bMG, bGtok], W=[bdt])
                        if r >= 0:
                            P.pool("tensor_tensor", out=dt_[:, 0:128], in0=dt_[:, 0:128], in1=TRI,
                                   op=ALU.mult, R=[bdt, bC], W=[bdt])
                        P.dve("tensor_tensor", out=w_[:, 0:n], in0=pz[:, 0:n], in1=dt_[:, 0:n],
                              op=ALU.mult, R=[pzb, bdt], W=[bw_])
                        for tt in range(max(r, 0), 4):
                            cc = (tt - max(r, 0)) * 128
                            P.pe("matmul", pacc[:, tt * 65:(tt + 1) * 65], lhsT=w_[:, cc:cc + 128],
                                 rhs=vext[:, kb, 0:65], start=(kb == 0 and tt == 0), stop=(kb == 4 * qb + 3 and tt == 3),
                                 R=[bw_, bv], W=[pab])
                    epilogue(qb, pacc, pab, h, h % 2, 65, NWm, bNWm, True)
                if h % 2 == 1:
                    flush_Y(h // 2)

        if "S" in phases:
            P.barrier()
            for h in range(4):
                Wt, bW, sl = next_w()
                load_w(Wt, bW, [(w_in[:, 1032 + h * 64:1032 + (h + 1) * 64], 0, 64),
                                (w_in[:, 1288 + h * 64:1288 + (h + 1) * 64], 64, 64),
                                (w_in[:, 1544 + h * 64:1544 + (h + 1) * 64], 128, 64)], sl)

                def ev_q(tb, ps, pb):
                    P.act("activation", out=qT[:, tb * 512:(tb + 1) * 512], in_=ps, func=AF.Copy,
                          scale=0.125, R=[pb], W=[bq])
                proj_fm(Wt, bW, 0, 64, ev_q)

                def ev_k(tb, ps, pb):
                    P.act("activation", out=kT[:, tb * 512:(tb + 1) * 512], in_=ps, func=AF.Copy,
                          R=[pb], W=[bk])
                proj_fm(Wt, bW, 64, 64, ev_k)
                for t in range(NT):
                    pt, pb = proj_tm(Wt, bW, 128, 64, t)
                    P.dve("tensor_copy", out=vext[:, t, 0:64], in_=pt[:, 0:64], R=[pb], W=[bv])
                pairs = [(qb, kb) for qb in range(4) for kb in range(4 * qb + 3, -1, -1)]
                stt = {}
                cur = {}

                def stageA(i):
                    qb, kb = pairs[i]
                    r = kb - 4 * qb
                    c0 = max(r, 0) * 128
                    n = 512 - c0
                    q0 = qb * 512 + c0
                    e_, be_ = e_t[i % 2]
                    s_, bs_ = sp_t[i % 2]
                    pz, pzb = psum()
                    P.pe("matmul", pz[:, 0:n], lhsT=kT[:, kb * 128:(kb + 1) * 128],
                         rhs=qT[:, q0:q0 + n], start=True, stop=True, R=[bk, bq], W=[pzb])
                    P.act("activation", out=e_[:, 0:n], in_=pz[:, 0:n], func=AF.Exp,
                          R=[pzb], W=[be_])
                    P.act("activation", out=s_[:, 0:n], in_=e_[:, 0:n], func=AF.Ln, bias=1.0,
                          scale=1.0, R=[be_], W=[bs_])
                    if r >= 0:
                        P.pool("tensor_tensor", out=s_[:, 0:128], in0=s_[:, 0:128], in1=UT1,
                               op=ALU.mult, R=[bs_, bC], W=[bs_])
                    stt[i] = (pz, pzb, s_, bs_, r, c0, n)

                def stageBC(i):
                    qb, kb = pairs[i]
                    pz, pzb, s_, bs_, r, c0, n = stt.pop(i)
                    a_, ba_ = arg_t[i % 2]
                    w_, bw_ = wTt[i % 2]
                    if kb == 4 * qb + 3:
                        cur["pacc"], cur["pab"] = psacc()
                        P.pool("memset", Suf, 0.0, W=[bSuf])
                    pacc, pab = cur["pacc"], cur["pab"]
                    p2, p2b = psum()
                    P.pe("matmul", p2[:, 0:n], lhsT=L1, rhs=s_[:, 0:n], start=True, stop=True,
                         R=[bC, bs_], W=[p2b])
                    p3, p3b = psum()
                    P.pe("matmul", p3[:, 0:n], lhsT=ones, rhs=s_[:, 0:n], start=True, stop=True,
                         R=[bC, bs_], W=[p3b])
                    P.dve("tensor_tensor", out=a_[:, 0:n], in0=pz[:, 0:n], in1=s_[:, 0:n],
                          op=ALU.subtract, R=[pzb, bs_], W=[ba_])
                    P.dve("tensor_tensor", out=a_[:, 0:n], in0=a_[:, 0:n], in1=p2[:, 0:n],
                          op=ALU.subtract, R=[ba_, p2b], W=[ba_])
                    P.pool("tensor_tensor", out=a_[:, 0:n], in0=a_[:, 0:n], in1=Suf[:, c0:512],
                           op=ALU.subtract, R=[ba_, bSuf], W=[ba_])
                    P.act("activation", out=w_[:, 0:n], in_=a_[:, 0:n], func=AF.Exp,
                          R=[ba_], W=[bw_])
                    if r >= 0:
                        P.pool("tensor_tensor", out=w_[:, 0:128], in0=w_[:, 0:128], in1=UT1,
                               op=ALU.mult, R=[bw_, bC], W=[bw_])
                    P.dve("tensor_tensor", out=Suf[:, c0:512], in0=Suf[:, c0:512],
                          in1=p3[:, 0:n], op=ALU.add, R=[bSuf, p3b], W=[bSuf])
                    for tt in range(max(r, 0), 4):
                        cc = (tt - max(r, 0)) * 128
                        P.pe("matmul", pacc[:, tt * 64:(tt + 1) * 64], lhsT=w_[:, cc:cc + 128],
                             rhs=vext[:, kb, 0:64], start=(kb == 4 * qb + 3 and tt == 3),
                             stop=(kb == 0 and tt == 3), R=[bw_, bv], W=[pab])
                    if kb == 0:
                        epilogue(qb, pacc, pab, h, h % 2, 64, NWs, bNWs, False)

                stageA(0)
                for i in range(len(pairs)):
                    if i + 1 < len(pairs):
                        stageA(i + 1)
                    stageBC(i)
                if h % 2 == 1:
                    flush_Y(2 + h // 2)


        if "G" in phases:
            P.barrier()
            a_reset(A0)
            cin, bcin = a_f32([128, S + 4])
            acc, bacc = a_f32([128, S])
            V32, bV32 = a_f32([128, S])
            Uv = acc.rearrange("p (a b) -> p a b", a=NT)
            Yv = V32.rearrange("p (a b) -> p a b", a=NT)
            qTn, bqn = a_bf16([128, S])
            kTn, bkn = a_bf16([128, S])
            KDEC, bKD = a_bf16([128, NT, 128])
            QKT, bQK = a_bf16([128, NT, 128])
            WTt, bWT = a_bf16([128, NT, 128])
            Wgs = [a_bf16([128, 8, 128]) for _ in range(3)]
            sqb, bsqb = a_f32([128, 512])
            rn, brn = a_f32([128, 512])
            g2d, bg2 = a_f32([128, 64])
            be2d, bbe = a_f32([128, 64])
            gam, bgam = a_f32([128, 64])
            glast, bgl = a_f32([128, 64])
            egam, beg = a_f32([128, 64])
            kds, bkds = a_f32([128, 64])
            cdv, bcd = a_f32([128, 64])
            bwv, bbw = a_f32([128, 64])
            g3 = g2d.rearrange("p (a b) -> p a b", a=NT)
            be3 = be2d.rearrange("p (a b) -> p a b", a=NT)
            DTB, bDTB = a_f32([128, 4])
            NEA, bNEA = a_f32([128, 4])
            t4, bt4 = a_f32([128, 4])
            convw, bcw = a_f32([128, 12, 4])
            GNW, bGNW = a_f32([128, 128])
            NPB = 2
            TG_l = [a_f32([128, 128]) for _ in range(NPB)]
            dec_l = [a_f32([128, 256]) for _ in range(NPB)]
            Mm_l = [a_f32([128, 128]) for _ in range(NPB)]
            MT_l = [a_f32([128, 128]) for _ in range(NPB)]
            Pm_l = [a_f32([128, 128]) for _ in range(NPB)]
            Xs_l = [[a_f32([128, 128]) for _ in range(2)] for _ in range(NPB)]
            XTs_l = [[a_f32([128, 128]) for _ in range(2)] for _ in range(NPB)]
            RHS_l = [a_f32([128, 256]) for _ in range(NPB)]
            S_f, bSf = a_f32([128, 128])
            S_b, bSb = a_bf16([128, 128])
            vnb, bvn = a_bf16([128, 128])
            otmp, bot = a_f32([128, 128])
            zs, bzs = a_f32([128, 128])
            sm2, bsm2 = a_f32([128, 8])
            gslot = [0]

            def next_g():
                i = gslot[0] % 3
                gslot[0] += 1
                return Wgs[i][0], Wgs[i][1], 4 + i

            bcast_load(DTB, bDTB, Wd["g_dt_bias"][l])
            bcast_load(NEA, bNEA, Wd["g_A_log"][l])
            bcast_load(GNW, bGNW, Wd["g_norm_w"][l])
            P.act("activation", out=NEA, in_=NEA, func=AF.Exp, R=[bNEA], W=[bNEA])
            P.dve("tensor_scalar", out=NEA, in0=NEA, scalar1=-1.0, scalar2=None, op0=ALU.mult,
                  R=[bNEA], W=[bNEA])
            P.dma(acc[0:4, 0:1536], Wd["g_conv_w"][l], W=[bacc], key="c")
            pt, pb = psum()
            for c in range(12):
                P.pe("transpose", out=pt[:, c * 4:(c + 1) * 4], in_=acc[0:4, c * 128:(c + 1) * 128],
                     identity=ident[0:4, 0:4], R=[bacc, bC], W=[pb])
            P.dve("tensor_copy", out=convw, in_=pt[:, 0:48].rearrange("p (a b) -> p a b", a=12),
                  R=[pb], W=[bcw])
            P.dve("memset", cin[:, 0:3], 0.0, W=[bcin])
            Wt, bW, sl = next_g()
            load_w(Wt, bW, [(w_in[:, 3848:3856], 0, 8)], sl)
            for t in range(NT):
                pt, pb = proj_tm(Wt, bW, 0, 8, t)
                P.dve("tensor_tensor", out=t4, in0=pt[:, 0:4], in1=DTB, op=ALU.add, R=[pb, bDTB], W=[bt4])
                P.act("activation", out=t4, in_=t4, func=AF.Exp, R=[bt4], W=[bt4])
                P.act("activation", out=t4, in_=t4, func=AF.Ln, bias=1.0, scale=1.0, R=[bt4], W=[bt4])
                P.dve("tensor_tensor", out=g3[:, t, :], in0=t4, in1=NEA, op=ALU.mult, R=[bt4, bNEA], W=[bg2])
                P.act("activation", out=be3[:, t, :], in_=pt[:, 4:8], func=AF.Sigmoid, R=[pb], W=[bbe])
            pt, pb = psum()
            P.pe("matmul", pt[:, 0:64], lhsT=TRI, rhs=g2d, start=True, stop=True, R=[bC, bg2], W=[pb])
            P.pe("matmul", pt[:, 64:128], lhsT=ones, rhs=g2d, start=True, stop=True, R=[bC, bg2], W=[pb])
            P.dve("tensor_copy", out=gam, in_=pt[:, 0:64], R=[pb], W=[bgam])
            P.dve("tensor_copy", out=glast, in_=pt[:, 64:128], R=[pb], W=[bgl])
            P.act("activation", out=egam, in_=gam, func=AF.Exp, R=[bgam], W=[beg])
            P.act("activation", out=cdv, in_=glast, func=AF.Exp, R=[bgl], W=[bcd])
            P.dve("tensor_tensor", out=kds, in0=glast, in1=gam, op=ALU.subtract, R=[bgl, bgam], W=[bkds])
            P.act("activation", out=kds, in_=kds, func=AF.Exp, R=[bkds], W=[bkds])
            P.dve("tensor_tensor", out=bwv, in0=be2d, in1=egam, op=ALU.mult, R=[bbe, beg], W=[bbw])

            def l2n(tb, dst_fn):
                tsl = slice(tb * 512, (tb + 1) * 512)
                P.pool("tensor_tensor", out=sqb, in0=acc[:, tsl], in1=acc[:, tsl], op=ALU.mult,
                       R=[bacc], W=[bsqb])
                pt, pb = psum()
                P.pe("matmul", pt[:, :], lhsT=ones, rhs=sqb, start=True, stop=True, R=[bC, bsqb], W=[pb])
                P.act("activation", out=rn, in_=pt[:, :], func=AF.Ln, bias=1e-6, scale=1.0, R=[pb], W=[brn])
                P.act("activation", out=rn, in_=rn, func=AF.Exp, scale=-0.5, R=[brn], W=[brn])
                dst_fn(tsl)

            for h in range(4):
                for nm, col0, cc in (("q", 1800 + h * 128, h), ("v", 2824 + h * 128, 8 + h),
                                     ("k", 2312 + h * 128, 4 + h)):
                    Wt, bW, sl = next_g()
                    load_w(Wt, bW, [(w_in[:, col0:col0 + 128], 0, 128)], sl)

                    def ev_c(tb, ps, pb):
                        P.act("activation", out=cin[:, 3 + tb * 512:3 + (tb + 1) * 512], in_=ps,
                              func=AF.Copy, R=[pb], W=[bcin])
                    proj_fm(Wt, bW, 0, 128, ev_c)
                    P.dve("tensor_scalar", out=acc, in0=cin[:, 3:3 + S], scalar1=convw[:, cc, 3:4],
                          scalar2=None, op0=ALU.mult, R=[bcin, bcw], W=[bacc])
                    for j in (2, 1, 0):
                        P.dve("scalar_tensor_tensor", out=acc, in0=cin[:, j:j + S],
                              scalar=convw[:, cc, j:j + 1], in1=acc, op0=ALU.mult, op1=ALU.add,
                              R=[bcin, bcw, bacc], W=[bacc])
                    P.act("activation", out=acc, in_=acc, func=AF.Silu, R=[bacc], W=[bacc])
                    if nm == "q":
                        for tb in range(4):
                            l2n(tb, lambda tsl: P.dve(
                                "scalar_tensor_tensor", out=qTn[:, tsl], in0=acc[:, tsl],
                                scalar=128 ** -0.5, in1=rn, op0=ALU.mult, op1=ALU.mult,
                                R=[bacc, brn], W=[bqn]))
                    elif nm == "v":
                        P.pool("tensor_copy", out=V32, in_=acc, R=[bacc], W=[bV32])
                    else:
                        for tb in range(4):
                            def kdst(tsl, tb=tb):
                                P.dve("tensor_tensor", out=cin[:, 3 + tb * 512:3 + (tb + 1) * 512],
                                      in0=acc[:, tsl], in1=rn, op=ALU.mult, R=[bacc, brn], W=[bcin])
                                P.act("activation", out=kTn[:, tsl], in_=cin[:, 3 + tb * 512:3 + (tb + 1) * 512],
                                      func=AF.Copy, R=[bcin], W=[bkn])
                            l2n(tb, kdst)
                for c in range(NT):
                    csl = slice(c * 128, (c + 1) * 128)
                    ch = slice(c * 4 + h, c * 4 + h + 1)
                    TG, bTG = TG_l[c % NPB]
                    dec, bdec = dec_l[c % NPB]
                    Mm, bMm = Mm_l[c % NPB]
                    MT, bMT = MT_l[c % NPB]
                    Pm, bPm = Pm_l[c % NPB]
                    Xs = Xs_l[c % NPB]
                    XTs = XTs_l[c % NPB]
                    RHS, bRHS = RHS_l[c % NPB]
                    pk, pkb = psum()
                    P.pe("transpose", out=pk[:, 0:128], in_=cin[:, 3 + c * 128:3 + (c + 1) * 128],
                         identity=ident, R=[bcin, bC], W=[pkb])
                    P.pe("transpose", out=pk[:, 128:256], in_=V32[:, csl], identity=ident,
                         R=[bV32, bC], W=[pkb])
                    P.dve("tensor_scalar", out=RHS[:, 0:128], in0=pk[:, 128:256], scalar1=be2d[:, ch],
                          scalar2=None, op0=ALU.mult, R=[pkb, bbe], W=[bRHS])
                    P.act("activation", out=RHS[:, 128:256], in_=pk[:, 0:128], func=AF.Copy,
                          scale=bwv[:, ch], R=[pkb, bbw], W=[bRHS])
                    P.dve("tensor_scalar", out=KDEC[:, c, :], in0=pk[:, 0:128], scalar1=kds[:, ch],
                          scalar2=None, op0=ALU.mult, R=[pkb, bkds], W=[bKD])
                    P.dve("tensor_scalar", out=TG, in0=TRI, scalar1=g2d[:, ch], scalar2=None,
                          op0=ALU.mult, R=[bC, bg2], W=[bTG])
                    pa, pab_ = psum()
                    P.pe("matmul", pa[:, 0:128], lhsT=TG, rhs=L1, start=True, stop=True, R=[bTG, bC], W=[pab_])
                    P.pe("matmul", pa[:, 128:256], lhsT=L1, rhs=TG, start=True, stop=True, R=[bTG, bC], W=[pab_])
                    P.act("activation", out=dec, in_=pa[:, 0:256], func=AF.Exp, R=[pab_], W=[bdec])
                    P.pool("tensor_tensor", out=dec[:, 0:128], in0=dec[:, 0:128], in1=L1, op=ALU.mult,
                           R=[bdec, bC], W=[bdec])
                    P.pool("tensor_tensor", out=dec[:, 128:256], in0=dec[:, 128:256], in1=TRI, op=ALU.mult,
                           R=[bdec, bC], W=[bdec])
                    pkk, pkkb = psum()
                    P.pe("matmul", pkk[:, 0:128], lhsT=kTn[:, csl], rhs=kTn[:, csl], start=True, stop=True,
                         R=[bkn], W=[pkkb])
                    P.pe("matmul", pkk[:, 128:256], lhsT=kTn[:, csl], rhs=qTn[:, csl], start=True, stop=True,
                         R=[bkn, bqn], W=[pkkb])
                    P.dve("scalar_tensor_tensor", out=Mm, in0=pkk[:, 0:128], scalar=be2d[:, ch],
                          in1=dec[:, 0:128], op0=ALU.mult, op1=ALU.mult, R=[pkkb, bbe, bdec], W=[bMm])
                    P.dve("tensor_tensor", out=QKT[:, c, :], in0=pkk[:, 128:256], in1=dec[:, 128:256],
                          op=ALU.mult, R=[pkkb, bdec], W=[bQK])
                    pm, pmb = psum()
                    P.pe("transpose", out=pm[:, 0:128], in_=Mm, identity=ident, R=[bMm, bC], W=[pmb])
                    P.act("activation", out=MT, in_=pm[:, 0:128], func=AF.Copy, R=[pmb], W=[bMT])
                    P.dve("tensor_tensor", out=Pm, in0=ident, in1=pm[:, 0:128], op=ALU.subtract,
                          R=[bC, pmb], W=[bPm])
                    Xc, bXc, XcT, bXcT = Mm, bMm, MT, bMT
                    for lvl in range(1, 7):
                        px, pxb = psum()
                        P.pe("matmul", px[:, 0:128], lhsT=XcT, rhs=Xc, start=True, stop=True,
                             R=[bXc, bXcT], W=[pxb])
                        if lvl < 6:
                            P.pe("matmul", px[:, 128:256], lhsT=Xc, rhs=XcT, start=True, stop=True,
                                 R=[bXc, bXcT], W=[pxb])
                        Xn, bXn = Xs[lvl % 2]
                        XnT, bXnT = XTs[lvl % 2]
                        P.act("activation", out=Xn, in_=px[:, 0:128], func=AF.Copy, R=[pxb], W=[bXn])
                        if lvl < 6:
                            P.dve("tensor_copy", out=XnT, in_=px[:, 128:256], R=[pxb], W=[bXnT])
                        pp, ppb = psum()
                        P.pe("matmul", pp[:, 0:128], lhsT=Xn, rhs=Pm, start=True, stop=True,
                             R=[bXn, bPm], W=[ppb])
                        P.dve("tensor_tensor", out=Pm, in0=Pm, in1=pp[:, 0:128], op=ALU.add,
                              R=[bPm, ppb], W=[bPm])
                        Xc, bXc, XcT, bXcT = Xn, bXn, XnT, bXnT
                    pu, pub = psum()
                    P.pe("matmul", pu[:, 0:128], lhsT=Pm, rhs=RHS[:, 0:128], start=True, stop=True,
                         R=[bPm, bRHS], W=[pub])
                    P.pe("matmul", pu[:, 128:256], lhsT=RHS[:, 128:256], rhs=Pm, start=True, stop=True,
                         R=[bPm, bRHS], W=[pub])
                    P.act("activation", out=Uv[:, c, :], in_=pu[:, 0:128], func=AF.Copy, R=[pub], W=[bacc])
                    P.dve("tensor_copy", out=WTt[:, c, :], in_=pu[:, 128:256], R=[pub], W=[bWT])
                Wt, bW, sl = next_g()
                load_w(Wt, bW, [(w_in[:, 3336 + h * 128:3336 + (h + 1) * 128], 0, 128)], sl)
                P.dve("memset", S_f, 0.0, W=[bSf])
                P.dve("memset", S_b, 0.0, W=[bSb])
                for c in range(NT):
                    csl = slice(c * 128, (c + 1) * 128)
                    ch = slice(c * 4 + h, c * 4 + h + 1)
                    p1, p1b = psum()
                    P.pe("matmul", p1[:, 0:128], lhsT=WTt[:, c, :], rhs=S_b, start=True, stop=True,
                         R=[bWT, bSb], W=[p1b])
                    P.pe("matmul", p1[:, 128:256], lhsT=qTn[:, csl], rhs=S_b, start=True, stop=True,
                         R=[bqn, bSb], W=[p1b])
                    P.dve("tensor_tensor", out=vnb, in0=Uv[:, c, :], in1=p1[:, 0:128], op=ALU.subtract,
                          R=[bacc, p1b], W=[bvn])
                    p2, p2b = psum()
                    P.pe("matmul", p2[:, 0:128], lhsT=QKT[:, c, :], rhs=vnb, start=True, stop=True,
                         R=[bQK, bvn], W=[p2b])
                    P.pe("matmul", p2[:, 128:256], lhsT=KDEC[:, c, :], rhs=vnb, start=True, stop=True,
                         R=[bKD, bvn], W=[p2b])
                    P.dve("scalar_tensor_tensor", out=S_f, in0=S_f, scalar=cdv[:, ch], in1=p2[:, 128:256],
                          op0=ALU.mult, op1=ALU.add, R=[bSf, bcd, p2b], W=[bSf])
                    P.act("activation", out=S_b, in_=S_f, func=AF.Copy, R=[bSf], W=[bSb])
                    P.act("activation", out=otmp, in_=p1[:, 128:256], func=AF.Copy, scale=egam[:, ch],
                          R=[p1b, beg], W=[bot])
                    P.dve("tensor_tensor", out=otmp, in0=otmp, in1=p2[:, 0:128], op=ALU.add,
                          R=[bot, p2b], W=[bot])
                    P.act("activation", out=zs, in_=otmp, func=AF.Square, accum_out=sm2[:, 0:1],
                          R=[bot], W=[bzs, bsm2])
                    rsqrt_small(sm2[:, 1:2], sm2[:, 0:1], 1.0 / 128, 1e-6, bsm2)
                    pz, pzb = proj_tm(Wt, bW, 0, 128, c)
                    P.act("activation", out=zs, in_=pz[:, 0:128], func=AF.Exp, scale=-1.0, R=[pzb], W=[bzs])
                    P.dve("tensor_scalar", out=zs, in0=zs, scalar1=1.0, scalar2=None, op0=ALU.add,
                          R=[bzs], W=[bzs])
                    P.dve("reciprocal", out=zs, in_=zs, R=[bzs], W=[bzs])
                    P.dve("scalar_tensor_tensor", out=Yv[:, c, :], in0=otmp, scalar=sm2[:, 1:2], in1=GNW,
                          op0=ALU.mult, op1=ALU.mult, R=[bot, bsm2, bGNW], W=[bV32])
                    P.pool("tensor_tensor", out=Yv[:, c, :], in0=Yv[:, c, :], in1=zs, op=ALU.mult,
                           R=[bV32, bzs], W=[bV32])
                    P.dve("tensor_tensor", out=Yv[:, c, :], in0=Yv[:, c, :], in1=pz[:, 0:128], op=ALU.mult,
                          R=[bV32, pzb], W=[bV32])
                flush_Y(4 + h, Yv, bV32)

        if "YT" in dbg_d:
            P.dma(dbg_d["YT"], YT, R=bYT, key="dbg")

        if "O" in phases:
            P.barrier()
            a_reset(A0)
            Wo, bWo = a_bf16([128, 8, D])
            Wg, bWg = a_bf16([128, 8, D])
            Wp, bWp = a_bf16([128, 2, D])
            G1, bG1 = a_f32([128, D])
            B1, bB1 = a_f32([128, D])
            junk1, bj1 = a_f32([128, D])
            st1, bst1 = a_f32([128, 8])
            WR, bWR = a_f32([128, 8, NE])
            BR, bBR = a_f32([128, NE])
            BD, bBD = a_f32([32, D])
            hT32, bh32 = a_f32([128, 8, 128])
            ptile, bpt = a_f32([128, 256])
            pT, bpT = a_bf16([128, 2, 128])
            lg, blg = a_f32([128, NE])
            msk, bmsk = a_f32([128, NE])
            mx8, bmx = a_f32([128, 16])
            gT, bgT = a_f32([32, 128])
            sig = [a_f32([128, 512]) for _ in range(2)]
            P.dma(Wo, Wd["w_out"][l].rearrange("(k p) c -> p k c", p=128), W=[bWo], key="wo", eng="pool")
            P.dma(Wg, Wd["w_ple_gate"][l].rearrange("(k p) c -> p k c", p=128), W=[bWg], key="wo", eng="pool")
            P.dma(Wp, Wd["w_ple_proj"][l].rearrange("(k p) c -> p k c", p=128), W=[bWp], key="wo", eng="pool")
            P.dma(WR, Wd["w_router"][l].rearrange("(k p) c -> p k c", p=128), W=[bWR], key="c")
            P.dma(BD, Wd["b_down"][l], W=[bBD], key="c")
            bcast_load(BR, bBR, Wd["b_router"][l])
            bcast_load(G1, bG1, Wd["ln1_g"][l])
            bcast_load(B1, bB1, Wd["ln1_b"][l])
            for t in range(NT):
                tsl = slice(t * 128, (t + 1) * 128)
                for nb in range(2):
                    nsl = slice(nb * 512, (nb + 1) * 512)
                    pt, pb = psum()
                    for k in range(8):
                        P.pe("matmul", pt[:, :], lhsT=YT[:, k, tsl], rhs=Wo[:, k, nsl], start=(k == 0),
                             stop=(k == 7), R=[bYT[k], bWo], W=[pb])
                    P.dve("scalar_tensor_tensor", out=X[:, t, nsl], in0=X[:, t, nsl], scalar=ALPHA,
                          in1=pt[:, :], op0=ALU.mult, op1=ALU.add, R=[bX[t], pb], W=[bX[t]])
                layer_norm_tile(t, G1, B1, bG1, bB1, junk1, bj1, st1, bst1)
                if "h1" in dbg_d:
                    P.dma(dbg_d["h1"][tsl, :], X[:, t, :], R=[bX[t]], key="dbg")

                def extra(k, pt, pb):
                    P.dve("tensor_copy", out=hT32[:, k:k + 4, :],
                          in_=pt[:].rearrange("p (j c) -> p j c", j=4), R=[pb], W=[bh32])
                transpose_tile(t, extra)
                pt, pb = psum()
                for k in range(8):
                    P.pe("matmul", pt[:, 0:NE], lhsT=hT32[:, k, :], rhs=WR[:, k, :], start=(k == 0),
                         stop=(k == 7), R=[bh32, bWR], W=[pb])
                P.dve("tensor_tensor", out=lg, in0=pt[:, 0:NE], in1=BR, op=ALU.add, R=[pb, bBR], W=[blg])
                P.dve("max", out=mx8[:, 0:8], in_=lg, R=[blg], W=[bmx])
                P.dve("tensor_scalar", out=msk, in0=lg, scalar1=mx8[:, 3:4], scalar2=None,
                      op0=ALU.is_ge, R=[blg, bmx], W=[bmsk])
                P.dve("tensor_scalar", out=mx8[:, 8:9], in0=mx8[:, 0:1], scalar1=-1.0, scalar2=None,
                      op0=ALU.mult, R=[bmx], W=[bmx])
                P.act("activation", out=lg, in_=lg, func=AF.Exp, bias=mx8[:, 8:9], scale=1.0,
                      R=[blg, bmx], W=[blg])
                P.dve("tensor_tensor", out=lg, in0=lg, in1=msk, op=ALU.mult, R=[blg, bmsk], W=[blg])
                P.dve("reduce_sum", out=mx8[:, 9:10], in_=lg, axis=AX.X, R=[blg], W=[bmx])
                P.dve("reciprocal", out=mx8[:, 10:11], in_=mx8[:, 9:10], R=[bmx], W=[bmx])
                P.dve("tensor_scalar", out=GATES[:, t, :], in0=lg, scalar1=mx8[:, 10:11], scalar2=None,
                      op0=ALU.mult, R=[blg, bmx], W=[bGA])
                pt, pb = psum()
                P.pe("transpose", out=pt[0:NE, 0:128], in_=GATES[:, t, :], identity=ident,
                     R=[bGA, bC], W=[pb])
                P.act("activation", out=gT, in_=pt[0:NE, 0:128], func=AF.Copy, R=[pb], W=[bgT])
                P.dma(ptile, p_d[l - layers[0], tsl, :], W=[bpt], key="pt")
                pt, pb = psum()
                for j in range(2):
                    P.pe("transpose", out=pt[:, j * 128:(j + 1) * 128], in_=ptile[:, j * 128:(j + 1) * 128],
                         identity=ident, R=[bpt, bC], W=[pb])
                P.act("activation", out=pT, in_=pt[:, 0:256].rearrange("p (j c) -> p j c", j=2),
                      func=AF.Copy, R=[pb], W=[bpT])
                for nb in range(2):
                    nsl = slice(nb * 512, (nb + 1) * 512)
                    sg_, bsg_ = sig[nb]
                    pg, pgb = psum()
                    for k in range(8):
                        P.pe("matmul", pg[:, :], lhsT=XT[:, k, tsl], rhs=Wg[:, k, nsl], start=(k == 0),
                             stop=(k == 7), R=[bXT[t], bWg], W=[pgb])
                    pp, ppb = psum()
                    for k in range(2):
                        P.pe("matmul", pp[:, :], lhsT=pT[:, k, :], rhs=Wp[:, k, nsl], start=(k == 0),
                             stop=(k == 1), R=[bpT, bWp], W=[ppb])
                    pbd, pbdb = psum()
                    P.pe("matmul", pbd[:, :], lhsT=gT, rhs=BD[:, nsl], start=True, stop=True,
                         R=[bgT, bBD], W=[pbdb])
                    P.act("activation", out=sg_, in_=pg[:, :], func=AF.Sigmoid, R=[pgb], W=[bsg_])
                    P.dve("tensor_tensor", out=sg_, in0=sg_, in1=pp[:, :], op=ALU.mult, R=[bsg_, ppb], W=[bsg_])
                    P.dve("scalar_tensor_tensor", out=X[:, t, nsl], in0=X[:, t, nsl], scalar=ALPHA,
                          in1=sg_, op0=ALU.mult, op1=ALU.add, R=[bX[t], bsg_], W=[bX[t]])
                    P.dve("tensor_tensor", out=X[:, t, nsl], in0=X[:, t, nsl], in1=pbd[:, :], op=ALU.add,
                          R=[bX[t], pbdb], W=[bX[t]])

        if "E" in phases:
            P.barrier()
            a_reset(AE)
            WG = [a_bf16([128, 8, 1024]) for _ in range(2)]
            WDn = [a_bf16([128, 4, 1024]) for _ in range(2)]
            actb = [a_bf16([128, 4, 512]) for _ in range(3)]
            gm_t = [a_f32([128, 512]) for _ in range(2)]
            sg_t = [a_f32([128, 512]) for _ in range(2)]
            um_t = [a_f32([128, 512]) for _ in range(2)]
            BGU, bBGU = a_f32([32, 2 * D])
            bguT, bbguT = a_f32([128, 16, NE])
            bgu7, _ = a_f32([128, 16, NE])
            G2, bG2 = a_f32([128, D])
            B2, bB2 = a_f32([128, D])
            junk2, bj2 = a_f32([128, D])
            st2, bst2 = a_f32([128, 8])
            P.dma(BGU, Wd["b_gu"][l], W=[bBGU], key="c")
            bcast_load(G2, bG2, Wd["ln2_g"][l])
            bcast_load(B2, bB2, Wd["ln2_b"][l])
            pt, pb = psum()
            for c in range(16):
                P.pe("transpose", out=pt[:, c * NE:(c + 1) * NE], in_=BGU[:, c * 128:(c + 1) * 128],
                     identity=ident[0:NE, 0:NE], R=[bBGU, bC], W=[pb])
            P.dve("tensor_copy", out=bguT, in_=pt[:, :].rearrange("p (a b) -> p a b", a=16), R=[pb], W=[bbguT])
            P.dve("tensor_scalar", out=bgu7, in0=bguT, scalar1=7.0, scalar2=None, op0=ALU.add,
                  R=[bbguT], W=[bbguT])
            ei = [0]

            def emit_gu(e, g, tb, wg_, bwg_, a_, ba_):
                for fc in range(4):
                    gm, bgm = gm_t[ei[0] % 2]
                    sg, bsg = sg_t[ei[0] % 2]
                    um, bum = um_t[ei[0] % 2]
                    ei[0] += 1
                    jg = g * 4 + fc
                    ju = 8 + g * 4 + fc
                    pg, pgb = psum()
                    for k in range(8):
                        P.pe("matmul", pg[:, :], lhsT=wg_[:, k, fc * 128:(fc + 1) * 128],
                             rhs=XT[:, k, tb * 512:(tb + 1) * 512], start=(k == 0), stop=(k == 7),
                             R=[bwg_] + bXT[tb * 4:tb * 4 + 4], W=[pgb])
                    pu, pub = psum()
                    for k in range(8):
                        P.pe("matmul", pu[:, :], lhsT=wg_[:, k, 512 + fc * 128:512 + (fc + 1) * 128],
                             rhs=XT[:, k, tb * 512:(tb + 1) * 512], start=(k == 0), stop=(k == 7),
                             R=[bwg_] + bXT[tb * 4:tb * 4 + 4], W=[pub])
                    P.dve("tensor_scalar", out=gm, in0=pg[:, :], scalar1=bguT[:, jg, e:e + 1], scalar2=7.0,
                          op0=ALU.add, op1=ALU.min, R=[pgb, bbguT], W=[bgm])
                    P.act("activation", out=sg, in_=gm, func=AF.Sigmoid, scale=1.702, R=[bgm], W=[bsg])
                    P.act("activation", out=um, in_=pu[:, :], func=AF.Relu, bias=bgu7[:, ju, e:e + 1],
                          scale=1.0, R=[pub, bbguT], W=[bum])
                    P.pool("tensor_tensor", out=sg, in0=sg, in1=gm, op=ALU.mult, R=[bsg, bgm], W=[bsg])
                    P.dve("tensor_scalar", out=um, in0=um, scalar1=14.0, scalar2=-6.0,
                          op0=ALU.min, op1=ALU.add, R=[bum], W=[bum])
                    P.pool("tensor_tensor", out=a_[:, fc, :], in0=um, in1=sg, op=ALU.mult, R=[bum, bsg], W=[ba_])

            def emit_down(e, g, tb, wd_, bwd_, a_, ba_):
                for tt in range(4):
                    t = tb * 4 + tt
                    for nb in range(2):
                        nsl = slice(nb * 512, (nb + 1) * 512)
                        py, pyb = psum()
                        for fc in range(4):
                            P.pe("matmul", py[:, :], lhsT=a_[:, fc, tt * 128:(tt + 1) * 128],
                                 rhs=wd_[:, fc, nsl], start=(fc == 0), stop=(fc == 3),
                                 R=[ba_, bwd_], W=[pyb])
                        P.dve("scalar_tensor_tensor", out=X[:, t, nsl], in0=py[:, :],
                              scalar=GATES[:, t, e:e + 1], in1=X[:, t, nsl], op0=ALU.mult,
                              op1=ALU.add, R=[pyb, bGA, bX[t]], W=[bX[t]])

            hi = 0
            ti = 0
            prev = None
            for e in range(NE):
                for g in range(2):
                    wg_, bwg_ = WG[hi % 2]
                    wd_, bwd_ = WDn[hi % 2]
                    sl = hi % 2
                    hi += 1
                    wgu = Wd["w_gu"][l][e]
                    P.dma(wg_[:, :, 0:512], wgu[:, g * 512:(g + 1) * 512].rearrange("(k p) c -> p k c", p=128),
                          W=[bwg_], key=f"e{sl}", eng="pool")
                    P.dma(wg_[:, :, 512:1024], wgu[:, D + g * 512:D + (g + 1) * 512].rearrange("(k p) c -> p k c", p=128),
                          W=[bwg_], key=f"e{sl}", eng="pool")
                    P.dma(wd_, Wd["w_down"][l][e][g * 512:(g + 1) * 512, :].rearrange("(k p) c -> p k c", p=128),
                          W=[bwd_], key=f"e{sl}", eng="pool")
                    for tb in range(4):
                        a_, ba_ = actb[ti % 3]
                        ti += 1
                        emit_gu(e, g, tb, wg_, bwg_, a_, ba_)
                        if prev is not None:
                            emit_down(*prev)
                        prev = (e, g, tb, wd_, bwd_, a_, ba_)
            emit_down(*prev)
            for t in range(NT):
                layer_norm_tile(t, G2, B2, bG2, bB2, junk2, bj2, st2, bst2)
                if l != layers[-1] or not last:
                    transpose_tile(t)
            dump_X("h2")

    for t in range(NT):
        P.dma(out_d[t * 128:(t + 1) * 128, :], X[:, t, :], R=[bX[t]], key="out")
    info = P.emit()
    return nc, info


def make_consts():
    j = np.arange(128)[:, None]
    t = np.arange(128)[None, :]
    cst = np.zeros((128, 5, 128), np.float32)
    cst[:, 0, :] = np.eye(128)
    cst[:, 1, :] = 1.0
    cst[:, 2, :] = (j <= t)
    cst[:, 3, :] = (j < t)
    cst[:, 4, :] = (j > t)
    sel = np.zeros((4, 512), np.float32)
    for h in range(4):
        sel[h, h * 128:(h + 1) * 128] = 1.0
    return cst, sel


_CACHE = {}


def run_prog(inputs, xs, layers, first, last, dbg=(), phases=("M", "S", "G", "O", "E"), trace=False, wl0=0):
    key = (tuple(layers), first, last, tuple(dbg), tuple(phases))
    if key not in _CACHE:
        _CACHE[key] = build(layers, first, last, dbg, phases)
    nc, info = _CACHE[key]
    cst, sel = make_consts()
    l0, l1 = layers[0] + wl0, layers[-1] + 1 + wl0
    names = [n for n in W_NAMES]
    in_maps = []
    import concourse.bass as _b
    declared = set(t for t in ["w_in", "m_i_bias", "m_f_bias", "m_norm_w", "sb_norm_w", "g_conv_w",
                               "g_A_log", "g_dt_bias", "g_norm_w"] if set(phases) & {"M", "S", "G"})
    if "O" in phases:
        declared |= {"w_out", "ln1_g", "ln1_b", "w_router", "b_router", "w_ple_gate", "w_ple_proj",
                     "b_down"}
    if "E" in phases:
        declared |= {"w_gu", "b_gu", "w_down", "ln2_g", "ln2_b"}
    wsl = {n: np.ascontiguousarray(np.asarray(inputs[n])[l0:l1]) for n in declared}
    for b in range(8):
        m = {"x": np.ascontiguousarray(xs[b]),
             "p": np.ascontiguousarray(np.asarray(inputs["p"])[l0:l1, b]),
             "ln0_g": np.asarray(inputs["ln0_g"]), "ln0_b": np.asarray(inputs["ln0_b"]),
             "cst": cst, "sel": sel}
        m.update(wsl)
        in_maps.append(m)
    res = run_bass_kernel_spmd(nc, in_maps, core_ids=list(range(8)), trace=trace)
    return res


FUSED = True


def kernel(**inputs):
    xs = np.asarray(inputs["x"], dtype=np.float32)
    if FUSED:
        res = run_prog(inputs, xs, list(range(DEPTH)), True, True)
        return np.stack([np.asarray(r["out"]) for r in res.results], axis=0).astype(np.float32)
    for l in range(DEPTH):
        res = run_prog(inputs, xs, [0], l == 0, True, wl0=l)
        xs = np.stack([np.asarray(r["out"]) for r in res.results], axis=0).astype(np.float32)
    return xs
```
